# Optimizing a Trainium2 kernel written in Bass

```python
import math
import jax, jax.numpy as jnp
from jax import lax
import numpy as np

D_MODEL = 1024
BATCH = 8
SEQ = 4096
DEPTH = 1

PLE_DIM = 256
SB_HEADS = 8
SB_HEAD_DIM = 64
SB_WIDTH = SB_HEADS * SB_HEAD_DIM
SB_BLOCK_Q = 128
SB_SCALE = 1.0 / math.sqrt(SB_HEAD_DIM)
ML_HEADS = 4
ML_HEAD_DIM = 128
ML_WIDTH = ML_HEADS * ML_HEAD_DIM
ML_CHUNK = 64
CONV_WIDTH = 4
D_FF = 4 * D_MODEL
RMS_EPS = 1e-6
IN_SIZES = (SB_WIDTH, SB_WIDTH, SB_WIDTH, ML_WIDTH, ML_WIDTH, ML_WIDTH, ML_WIDTH, ML_HEADS, ML_HEADS, D_MODEL, D_MODEL)
IN_COLS = sum(IN_SIZES)
IN_OFFSETS = tuple(int(v) for v in np.cumsum(IN_SIZES)[:-1])

kernel_name = "hybrid_stickbreak_mlstm_gated_block"


def rmsnorm(x, g):
    xf = x.astype(jnp.float32)
    y = xf * lax.rsqrt(jnp.mean(xf * xf, axis=-1, keepdims=True) + RMS_EPS)
    return (y * g.astype(jnp.float32)).astype(x.dtype)


def causal_depthwise_conv(u, w, b):
    c = u.shape[-1]
    y = lax.conv_general_dilated(u, w[:, None, :].astype(u.dtype), window_strides=(1,),
                                 padding=[(CONV_WIDTH - 1, 0)],
                                 dimension_numbers=('NWC', 'WIO', 'NWC'),
                                 feature_group_count=c)
    return y + b.astype(y.dtype)


def split_heads(u, n_heads):
    bsz, seq, _ = u.shape
    return u.reshape(bsz, seq, n_heads, -1).transpose(0, 2, 1, 3)


def merge_heads(u):
    bsz, nh, seq, dh = u.shape
    return u.transpose(0, 2, 1, 3).reshape(bsz, seq, nh * dh)


def stick_breaking_attention(q, k, v):
    seq = q.shape[2]
    outs = []
    for blk in range(seq // SB_BLOCK_Q):
        t0 = blk * SB_BLOCK_Q
        kv_len = t0 + SB_BLOCK_Q
        z = jnp.einsum('bhqd,bhkd->bhqk', q[:, :, t0:kv_len], k[:, :, :kv_len]) * SB_SCALE
        mask = jnp.arange(kv_len)[None, :] < (t0 + jnp.arange(SB_BLOCK_Q))[:, None]
        log_beta = jax.nn.log_sigmoid(z)
        log_keep = jnp.where(mask, log_beta - z, 0.0)
        log_after = lax.cumsum(log_keep, axis=3, reverse=True) - log_keep
        a = jnp.where(mask, jnp.exp(log_beta + log_after), 0.0)
        outs.append(jnp.einsum('bhqk,bhkd->bhqd', a, v[:, :, :kv_len]))
    return jnp.concatenate(outs, axis=2)


def mlstm_chunkwise(q, k, v, i_pre, log_f):
    bsz, nh, seq, dh = q.shape
    nc = seq // ML_CHUNK
    k = k * (dh ** -0.5)

    def to_chunks(a):
        a = a.reshape(bsz, nh, nc, ML_CHUNK, *a.shape[3:])
        return jnp.moveaxis(a, 2, 0)

    causal = jnp.tril(jnp.ones((ML_CHUNK, ML_CHUNK), dtype=bool))

    def step(carry, inp):
        c_prev, n_prev, m_prev = carry
        qc, kc, vc, ic, fc = inp
        b = jnp.cumsum(fc, axis=-1)
        d = b[..., :, None] - b[..., None, :] + ic[..., None, :]
        d = jnp.where(causal, d, -jnp.inf)
        m_inter = b + m_prev[..., None]
        m_t = jnp.maximum(m_inter, jnp.max(d, axis=-1))
        s_mat = jnp.einsum('bhtd,bhsd->bhts', qc, kc) * jnp.exp(d - m_t[..., None])
        inter = jnp.exp(m_inter - m_t)
        num = jnp.einsum('bhts,bhsd->bhtd', s_mat, vc) + inter[..., None] * jnp.einsum('bhvk,bhtk->bhtv', c_prev, qc)
        den = jnp.sum(s_mat, axis=-1) + inter * jnp.einsum('bhk,bhtk->bht', n_prev, qc)
        h = num / jnp.maximum(jnp.abs(den), jnp.exp(-m_t))[..., None]
        b_end = b[..., -1]
        g = b_end[..., None] - b + ic
        m_new = jnp.maximum(b_end + m_prev, jnp.max(g, axis=-1))
        w_s = jnp.exp(g - m_new[..., None])
        decay = jnp.exp(b_end + m_prev - m_new)
        c_new = decay[..., None, None] * c_prev + jnp.einsum('bhs,bhsv,bhsk->bhvk', w_s, vc, kc)
        n_new = decay[..., None] * n_prev + jnp.einsum('bhs,bhsk->bhk', w_s, kc)
        return (c_new, n_new, m_new), h

    init = (jnp.zeros((bsz, nh, dh, dh), jnp.float32),
            jnp.zeros((bsz, nh, dh), jnp.float32),
            jnp.zeros((bsz, nh), jnp.float32))
    xs = (to_chunks(q), to_chunks(k), to_chunks(v), to_chunks(i_pre), to_chunks(log_f))
    _, h = lax.scan(step, init, xs)
    return jnp.moveaxis(h, 0, 2).reshape(bsz, nh, seq, dh)


def setup_inputs(seed: int = 0) -> dict:
    key = jax.random.key(seed)
    ks = jax.random.split(key, 24)

    def nrm(k, shape, scale):
        return jax.random.normal(k, shape, jnp.float32) * scale

    def gain(k, n):
        return 1.0 + 0.05 * jax.random.normal(k, (DEPTH, n), jnp.float32)

    return {
        "x": nrm(ks[0], (BATCH, SEQ, D_MODEL), 1.0),
        "p": nrm(ks[1], (DEPTH, BATCH, SEQ, PLE_DIM), 1.0),
        "g_mix_pre": gain(ks[2], D_MODEL),
        "w_in": nrm(ks[3], (DEPTH, D_MODEL, IN_COLS), D_MODEL ** -0.5),
        "b_igate": nrm(ks[4], (DEPTH, ML_HEADS), 0.1),
        "b_fgate": 3.0 + nrm(ks[5], (DEPTH, ML_HEADS), 0.5),
        "w_conv": nrm(ks[6], (DEPTH, CONV_WIDTH, 2 * ML_WIDTH), CONV_WIDTH ** -0.5),
        "b_conv": nrm(ks[7], (DEPTH, 2 * ML_WIDTH), 0.02),
        "g_mlstm_head": gain(ks[8], ML_WIDTH),
        "w_branch_sb": nrm(ks[9], (DEPTH, SB_WIDTH, D_MODEL), SB_WIDTH ** -0.5),
        "w_branch_ml": nrm(ks[10], (DEPTH, ML_WIDTH, D_MODEL), ML_WIDTH ** -0.5),
        "w_out": nrm(ks[11], (DEPTH, D_MODEL, D_MODEL), D_MODEL ** -0.5),
        "g_mix_post": gain(ks[12], D_MODEL),
        "g_mlp_pre": gain(ks[13], D_MODEL),
        "w_mlp_up": nrm(ks[14], (DEPTH, D_MODEL, D_FF), D_MODEL ** -0.5),
        "w_mlp_down": nrm(ks[15], (DEPTH, D_FF, D_MODEL), D_FF ** -0.5),
        "g_mlp_post": gain(ks[16], D_MODEL),
        "g_ple_pre": gain(ks[17], D_MODEL),
        "w_ple_gate": nrm(ks[18], (DEPTH, D_MODEL, D_MODEL), D_MODEL ** -0.5),
        "w_ple_proj": nrm(ks[19], (DEPTH, PLE_DIM, D_MODEL), PLE_DIM ** -0.5),
        "g_ple_post": gain(ks[20], D_MODEL),
    }


def reference(x, p, g_mix_pre, w_in, b_igate, b_fgate, w_conv, b_conv, g_mlstm_head,
              w_branch_sb, w_branch_ml, w_out, g_mix_post, g_mlp_pre, w_mlp_up, w_mlp_down,
              g_mlp_post, g_ple_pre, w_ple_gate, w_ple_proj, g_ple_post):
    f32 = jnp.float32
    bsz, seq, _ = x.shape
    for layer in range(DEPTH):
        h = rmsnorm(x, g_mix_pre[layer])
        z = h @ w_in[layer]
        (q_sb, k_sb, v_sb, q_ml, k_ml, v_ml, o_ml, i_ml, f_ml,
         gate_sb, gate_ml) = jnp.split(z, IN_OFFSETS, axis=-1)

        y_sb = stick_breaking_attention(split_heads(q_sb.astype(f32), SB_HEADS),
                                        split_heads(k_sb.astype(f32), SB_HEADS),
                                        split_heads(v_sb.astype(f32), SB_HEADS))
        y_sb = merge_heads(y_sb).astype(x.dtype)

        qk = jax.nn.silu(causal_depthwise_conv(jnp.concatenate([q_ml, k_ml], axis=-1),
                                               w_conv[layer], b_conv[layer]))
        q_c, k_c = jnp.split(qk, 2, axis=-1)
        i_pre = (i_ml + b_igate[layer]).astype(f32).transpose(0, 2, 1)
        log_f = jax.nn.log_sigmoid((f_ml + b_fgate[layer]).astype(f32)).transpose(0, 2, 1)
        h_ml = mlstm_chunkwise(split_heads(q_c.astype(f32), ML_HEADS),
                               split_heads(k_c.astype(f32), ML_HEADS),
                               split_heads(v_ml.astype(f32), ML_HEADS), i_pre, log_f)
        h_ml = h_ml.transpose(0, 2, 1, 3)
        h_ml = h_ml * lax.rsqrt(jnp.mean(h_ml * h_ml, axis=-1, keepdims=True) + RMS_EPS)
        h_ml = h_ml.reshape(bsz, seq, ML_WIDTH) * g_mlstm_head[layer].astype(f32)
        y_ml = (h_ml * jax.nn.sigmoid(o_ml.astype(f32))).astype(x.dtype)

        merged = (jax.nn.sigmoid(gate_sb) * (y_sb @ w_branch_sb[layer])
                  + jax.nn.sigmoid(gate_ml) * (y_ml @ w_branch_ml[layer]))
        x = x + rmsnorm(merged @ w_out[layer], g_mix_post[layer])

        u = jax.nn.relu(rmsnorm(x, g_mlp_pre[layer]) @ w_mlp_up[layer])
        x = x + rmsnorm((u * u) @ w_mlp_down[layer], g_mlp_post[layer])

        gate = jax.nn.sigmoid(rmsnorm(x, g_ple_pre[layer]) @ w_ple_gate[layer])
        x = x + rmsnorm(gate * (p[layer] @ w_ple_proj[layer]), g_ple_post[layer])
    return x
```

```python
import os
import math
from contextlib import ExitStack
import numpy as np
import ml_dtypes
import concourse.bass as bass
import concourse.mybir as mybir
from concourse.bass_utils import run_bass_kernel_spmd

F32 = mybir.dt.float32
BF16 = mybir.dt.bfloat16
AF = mybir.ActivationFunctionType
ALU = mybir.AluOpType
AX = mybir.AxisListType

S = 4096
D = 1024
NCOL = 5640
NG = 8
NT = 32
EPS = 1e-6
SB_SCALE = 1.0 / 8.0
LN_C = -0.5 * math.log(128.0)
NEG = -30000.0


class Trk:
    SAME_ENGINE_SYNC = True
    NDMA = 8

    def __init__(self, nc):
        self.nc = nc
        self.eng = {'pe': nc.tensor, 'act': nc.scalar, 'dve': nc.vector, 'pool': nc.gpsimd, 'sp': nc.sync}
        self.sem, self.cnt = {}, {}
        for e in ('pe', 'act', 'dve', 'pool'):
            self.sem[e] = nc.alloc_semaphore('c_' + e)
            self.cnt[e] = 0
        self.dsem, self.dcnt = {}, {}
        for q in ('sp', 'act', 'pool'):
            self.dsem[q] = [nc.alloc_semaphore('d_%s%d' % (q, i)) for i in range(self.NDMA)]
            self.dcnt[q] = 0
        self.waited, self.lastw, self.readers, self.semobj = {}, {}, {}, {}
        for e, s in self.sem.items():
            self.semobj[('c', e)] = s
        for q, l in self.dsem.items():
            for i, s in enumerate(l):
                self.semobj[('d', q, i)] = s
        self.nops = 0

    def _wait(self, e, tok):
        if tok is None:
            return
        sk, val = tok
        if sk[0] == 'c' and sk[1] == e:
            if e == 'pe' or not self.SAME_ENGINE_SYNC:
                return
        if self.waited.get((e, sk), 0) >= val:
            return
        self.eng[e].wait_ge(self.semobj[sk], val)
        self.waited[(e, sk)] = val

    def _deps(self, e, reads, writes):
        need = {}

        def add(tok):
            if tok is not None and need.get(tok[0], 0) < tok[1]:
                need[tok[0]] = tok[1]
        for b in reads:
            add(self.lastw.get(b))
        for b in writes:
            add(self.lastw.get(b))
            for r in self.readers.get(b, ()):
                add(r)
        for sk, val in need.items():
            self._wait(e, (sk, val))

    def _commit(self, tok, reads, writes):
        for b in reads:
            lst = self.readers.setdefault(b, [])
            for k_, r in enumerate(lst):
                if r[0] == tok[0]:
                    lst[k_] = tok
                    break
            else:
                lst.append(tok)
        for b in writes:
            self.lastw[b] = tok
            self.readers[b] = []

    def op(self, e, fn, reads=(), writes=()):
        self._deps(e, reads, writes)
        ins = fn(self.eng[e])
        self.cnt[e] += 1
        ins.then_inc(self.sem[e], 1)
        tok = (('c', e), self.cnt[e])
        self._commit(tok, reads, writes)
        self.nops += 1
        return tok

    def dma(self, q, out, in_, reads=(), writes=(), **kw):
        i = self.dcnt[q]
        slot, rnd = i % self.NDMA, i // self.NDMA
        sk = ('d', q, slot)
        if rnd > 0:
            self._wait(q, (sk, 16 * rnd))
        self._deps(q, reads, writes)
        self.eng[q].dma_start(out=out, in_=in_, **kw).then_inc(self.semobj[sk], 16)
        self.dcnt[q] += 1
        tok = (sk, 16 * (rnd + 1))
        self._commit(tok, reads, writes)
        self.nops += 1
        return tok

    def barrier(self):
        toks = [(('c', e), self.cnt[e]) for e in self.sem if self.cnt[e] > 0]
        for q in self.dsem:
            n = self.dcnt[q]
            for s in range(self.NDMA):
                k = (n - s + self.NDMA - 1) // self.NDMA
                if k > 0:
                    toks.append((('d', q, s), 16 * k))
        for e in ('pe', 'act', 'dve', 'pool', 'sp'):
            for sk, val in toks:
                if sk[0] == 'c' and sk[1] == e:
                    continue
                if self.waited.get((e, sk), 0) >= val:
                    continue
                self.eng[e].wait_ge(self.semobj[sk], val)
                self.waited[(e, sk)] = val
        self.lastw.clear()
        self.readers.clear()


def host_consts():
    bf = ml_dtypes.bfloat16
    c = {}
    c['c_identb'] = np.eye(128, dtype=np.float32).astype(bf)
    c['c_identf'] = np.eye(128, dtype=np.float32)
    j = np.arange(128)[:, None]
    s = np.arange(128)[None, :]
    c['c_uneg'] = np.where(j >= s, -1.0, 0.0).astype(np.float32).astype(bf)
    c['c_oneg'] = np.full((128, 128), -1.0, np.float32).astype(bf)
    negm = np.zeros((128, 4, 512), np.float32)
    for i in range(4):
        key = 128 * i + np.arange(128)[:, None]
        qq = np.arange(512)[None, :]
        negm[:, i, :] = np.where(key >= qq, NEG, 0.0)
    c['c_negm'] = negm.astype(bf)
    ss = np.arange(64)[:, None]
    tt = np.arange(64)[None, :]
    m01 = np.where(ss <= tt, 1.0, 0.0).astype(np.float32)
    c['c_m01'] = np.tile(m01, (1, 4)).astype(bf)
    c['c_ones'] = np.ones((128, 64), np.float32)
    return c


def build(debug=False, upto=99):
    nc = bass.Bass("TRN2", target_bir_lowering=False)
    t = Trk(nc)

    def din(name, shape, dt=F32):
        return nc.dram_tensor(name, list(shape), dt, kind="ExternalInput").ap()

    def dscr(name, shape, dt):
        return nc.dram_tensor(name, list(shape), dt, kind=("ExternalOutput" if debug else "Internal")).ap()

    x = din('x', [S, D]); p_in = din('p', [S, 256])
    g_mix_pre = din('g_mix_pre', [D]); w_in = din('w_in', [D, NCOL])
    b_igate = din('b_igate', [4]); b_fgate = din('b_fgate', [4])
    w_conv = din('w_conv', [4, 1024]); b_conv = din('b_conv', [1024])
    g_mlstm_head = din('g_mlstm_head', [512])
    w_branch_sb = din('w_branch_sb', [512, D]); w_branch_ml = din('w_branch_ml', [512, D])
    w_out = din('w_out', [D, D]); g_mix_post = din('g_mix_post', [D]); g_mlp_pre = din('g_mlp_pre', [D])
    w_mlp_up = din('w_mlp_up', [D, 4096]); w_mlp_down = din('w_mlp_down', [4096, D])
    g_mlp_post = din('g_mlp_post', [D]); g_ple_pre = din('g_ple_pre', [D])
    w_ple_gate = din('w_ple_gate', [D, D]); w_ple_proj = din('w_ple_proj', [256, D]); g_ple_post = din('g_ple_post', [D])
    c_identb = din('c_identb', [128, 128], BF16); c_identf = din('c_identf', [128, 128])
    c_uneg = din('c_uneg', [128, 128], BF16); c_oneg = din('c_oneg', [128, 128], BF16)
    c_negm = din('c_negm', [128, 4, 512], BF16); c_m01 = din('c_m01', [64, 256], BF16)
    c_ones = din('c_ones', [128, 64])
    y = nc.dram_tensor('y', [S, D], F32, kind="ExternalOutput").ap()

    qsbT = dscr('qsbT', [512, S], BF16); ksbT = dscr('ksbT', [512, S], BF16); vsb = dscr('vsb', [S, 512], BF16)
    qmlT = dscr('qmlT', [512, S], BF16); kmlT = dscr('kmlT', [512, S], BF16); kml = dscr('kml', [S, 512], BF16)
    vml = dscr('vml', [S, 512], BF16); osig = dscr('osig', [S, 512], BF16); gsig = dscr('gsig', [S, 2048], BF16)
    gifT = dscr('gifT', [8, S], F32)
    gs1 = dscr('gs1', [2, 256], F32); gs2 = dscr('gs2', [256], F32); gs3 = dscr('gs3', [256], F32)
    ysbT = dscr('ysbT', [512, S], BF16); ymlT = dscr('ymlT', [512, S], BF16)
    x1 = dscr('x1', [S, D], F32); x2 = dscr('x2', [S, D], F32)
    Wsb_b = dscr('Wsb_b', [512, D], BF16); Wml_b = dscr('Wml_b', [512, D], BF16); Wo_b = dscr('Wo_b', [D, D], BF16)
    Wup_b = dscr('Wup_b', [D, 4096], BF16); Wdn_b = dscr('Wdn_b', [4096, D], BF16)
    Wg_b = dscr('Wg_b', [D, D], BF16); Wp_b = dscr('Wp_b', [256, D], BF16)
    h2T_d = dscr('h2T_d', [D, S], BF16)

    PS = [nc.alloc_psum_tensor('ps%d' % i, [128, 512], F32) for i in range(8)]
    PSb = [h.bitcast(BF16) for h in PS]
    identb = nc.alloc_sbuf_tensor('identb', [128, 128], BF16)
    identf = nc.alloc_sbuf_tensor('identf', [128, 128], F32)
    t.dma('sp', identb[:, :], c_identb, writes=['identb'])
    t.dma('sp', identf[:, :], c_identf, writes=['identf'])
    t.barrier()

    NC = dict(allow_slow_non_contiguous=True)

    def rms_rstd(src_ap, rd, junk, ssc, key):
        t.op('act', lambda e: e.activation(junk[:, :], src_ap, AF.Square, accum_out=ssc), reads=rd, writes=['junk', key])
        t.op('act', lambda e: e.activation(ssc, ssc, AF.Sqrt, scale=1.0 / 1024, bias=EPS), reads=[key], writes=[key])
        t.op('dve', lambda e: e.reciprocal(ssc, ssc), reads=[key], writes=[key])

    def transpose8(src, srckey, n, dst_ap, dstkey, bank, eng):
        pb = PSb[bank]
        srckeys = srckey if isinstance(srckey, list) else [srckey]
        for kc in range(n):
            t.op('pe', lambda e, kc=kc: e.transpose(pb[:, kc * 128:(kc + 1) * 128], src[:, kc * 128:(kc + 1) * 128], identb[:, :]),
                 reads=srckeys, writes=['ps%d' % bank])
        src_v = pb[:, 0:n * 128].rearrange('p (k t) -> p k t', k=n)
        if eng == 'act':
            t.op('act', lambda e: e.activation(dst_ap, src_v, AF.Copy), reads=['ps%d' % bank], writes=[dstkey])
        else:
            t.op(eng, lambda e: e.tensor_copy(dst_ap, src_v), reads=['ps%d' % bank], writes=[dstkey])

    def cast_weight(es0, Wdst, wsrc, nk, ncols, gvec, piece, name):
        stg = [es0.enter_context(nc.sbuf_tensor('%s_stg%d' % (name, i), [128, piece], F32)) for i in range(3)]
        n = 0
        for kc in range(nk):
            for c0 in range(0, ncols, piece):
                w = min(piece, ncols - c0)
                sl = n % 3
                t.dma('sp', stg[sl][:, 0:w], wsrc[kc * 128:(kc + 1) * 128, c0:c0 + w], writes=[(name, 'stg', sl)])
                dst = Wdst[:, kc, c0:c0 + w]
                if n % 2 == 0:
                    if gvec is None:
                        t.op('dve', lambda e, dst=dst, sl=sl, w=w: e.tensor_copy(dst, stg[sl][:, 0:w]),
                             reads=[(name, 'stg', sl)], writes=[(name, kc, c0)])
                    else:
                        t.op('dve', lambda e, dst=dst, sl=sl, w=w, kc=kc: e.tensor_scalar(dst, stg[sl][:, 0:w], gvec[:, kc:kc + 1], None, ALU.mult),
                             reads=[(name, 'stg', sl)], writes=[(name, kc, c0)])
                else:
                    if gvec is None:
                        t.op('act', lambda e, dst=dst, sl=sl, w=w: e.activation(dst, stg[sl][:, 0:w], AF.Copy),
                             reads=[(name, 'stg', sl)], writes=[(name, kc, c0)])
                    else:
                        t.op('act', lambda e, dst=dst, sl=sl, w=w, kc=kc: e.activation(dst, stg[sl][:, 0:w], AF.Copy, scale=gvec[:, kc:kc + 1]),
                             reads=[(name, 'stg', sl)], writes=[(name, kc, c0)])
                n += 1

    def load_bf16_weight(Wdst, wsrc_b, nk, name):
        for kc in range(nk):
            t.dma('sp', Wdst[:, kc, :], wsrc_b[kc * 128:(kc + 1) * 128, :], writes=[(name, kc)])

    def phase1():
        with ExitStack() as es:
            def sbt(name, shape, dt):
                return es.enter_context(nc.sbuf_tensor(name, shape, dt))
            Wb = sbt('Wb', [128, 8, NCOL], BF16)
            gpre = sbt('gpre', [128, 8], F32)
            wcv = sbt('wcv', [128, 8, 4], F32)
            bcv = sbt('bcv', [128, 8], F32)
            ghb = sbt('ghb', [128, 512], F32)
            t.dma('sp', gpre[:, :], g_mix_pre.rearrange('(k p) -> p k', p=128), writes=['gpre'], **NC)
            for tap in range(4):
                t.dma('sp', wcv[:, :, tap], w_conv[tap, :].rearrange('(j p) -> p j', p=128), writes=[('wcv', tap)], **NC)
            t.dma('sp', bcv[:, :], b_conv.rearrange('(j p) -> p j', p=128), writes=['bcv'], **NC)
            t.dma('sp', ghb[:, :], g_mlstm_head.partition_broadcast(128), writes=['ghb'])
            t.barrier()
            with ExitStack() as es0:
                cast_weight(es0, Wb, w_in, 8, NCOL, gpre, 1880, 'win')
                t.barrier()
            t.barrier()

            xs = [sbt('xs%d' % i, [128, 1024], F32) for i in range(8)]
            hb = [sbt('hb%d' % i, [128, 1024], BF16) for i in range(4)]
            hT = [sbt('hT%d' % i, [128, 8, 512], BF16) for i in range(2)]
            junk = sbt('junk', [128, 1024], BF16)
            ssq = sbt('ssq', [128, 8], F32)
            cb = sbt('cb', [128, 8, 515], F32)
            acc = [sbt('acc%d' % i, [128, 512], F32) for i in range(2)]
            ost = [sbt('ost%d' % i, [128, 512], BF16) for i in range(6)]
            osf = [sbt('osf%d' % i, [128, 512], F32) for i in range(2)]
            gst = [sbt('gst%d' % i, [8, 512], F32) for i in range(2)]
            kst = [sbt('kst%d' % i, [128, 4, 128], BF16) for i in range(2)]
            kbf = [sbt('kbf%d' % i, [128, 512], BF16) for i in range(4)]
            t.op('pool', lambda e: e.memset(cb[:, :, 0:3], 0.0), writes=[('cb', j) for j in range(8)])

            gv_up = sbt('gv_up', [128, 8], F32); gv_g = sbt('gv_g', [128, 8], F32)
            t.dma('sp', gv_up[:, :], g_mlp_pre.rearrange('(k p) -> p k', p=128), writes=['gv_up'], **NC)
            t.dma('sp', gv_g[:, :], g_ple_pre.rearrange('(k p) -> p k', p=128), writes=['gv_g'], **NC)
            wst = [sbt('wst%d' % i, [128, 1024], F32) for i in range(2)]
            wsb16 = [sbt('wsb16_%d' % i, [128, 1024], BF16) for i in range(2)]
            pieces = []
            for (src, dst, nk, ncols, gv) in ((w_branch_sb, Wsb_b, 4, D, None), (w_branch_ml, Wml_b, 4, D, None), (w_out, Wo_b, 8, D, None),
                                              (w_mlp_up, Wup_b, 8, 4096, gv_up), (w_mlp_down, Wdn_b, 32, D, None),
                                              (w_ple_gate, Wg_b, 8, D, gv_g), (w_ple_proj, Wp_b, 2, D, None)):
                for kc in range(nk):
                    for c0 in range(0, ncols, 1024):
                        pieces.append((src, dst, kc, c0, gv))

            def piece_load(pi):
                src, dst, kc, c0, gv = pieces[pi]
                sl = pi % 2
                t.dma('sp', wst[sl][:, :], src[kc * 128:(kc + 1) * 128, c0:c0 + 1024], writes=[('wst', sl)])

            def piece_cast(pi):
                src, dst, kc, c0, gv = pieces[pi]
                sl = pi % 2
                if gv is None:
                    t.op('act', lambda e: e.activation(wsb16[sl][:, :], wst[sl][:, :], AF.Copy), reads=[('wst', sl)], writes=[('wsb16', sl)])
                else:
                    t.op('act', lambda e: e.activation(wsb16[sl][:, :], wst[sl][:, :], AF.Copy, scale=gv[:, kc:kc + 1]),
                         reads=[('wst', sl), 'gv_up', 'gv_g'], writes=[('wsb16', sl)])
                t.dma('act', dst[kc * 128:(kc + 1) * 128, c0:c0 + 1024], wsb16[sl][:, :], reads=[('wsb16', sl)], writes=[('wcast', pi)])

            st = {'bank': 0, 'ost': 0, 'n': 0, 'slot': 0}

            def precast_tick():
                k_ = st['slot']
                st['slot'] += 1
                pi = k_ // 4
                if k_ % 4 == 0 and pi < len(pieces):
                    piece_load(pi)
                elif k_ % 4 == 2 and 0 <= pi - 1 < len(pieces):
                    piece_cast(pi - 1)

            def nbank():
                b = st['bank']
                st['bank'] = (b + 1) % 6
                precast_tick()
                return b

            def nost():
                o = st['ost']
                st['ost'] = (o + 1) % 6
                return o

            def load_x(tt):
                sl = tt % 8
                t.dma('sp', xs[sl][:, :], x[tt * 128:(tt + 1) * 128, :], writes=[('xs', sl)])

            def norm_C(tt):
                sl, hs, g, col, ti = tt % 8, tt % 4, tt // 4, tt % 8, tt % 4
                rms_rstd(xs[sl][:, :], [('xs', sl)], junk, ssq[:, col:col + 1], ('ssq', col))
                t.op('dve', lambda e: e.tensor_scalar(hb[hs][:, :], xs[sl][:, :], ssq[:, col:col + 1], None, ALU.mult),
                     reads=[('xs', sl), ('ssq', col)], writes=[('hb', hs)])

            def norm_P(tt):
                sl, hs, g, col, ti = tt % 8, tt % 4, tt // 4, tt % 8, tt % 4
                transpose8(hb[hs], ('hb', hs), 8, hT[g % 2][:, :, ti * 128:(ti + 1) * 128], ('hT', g % 2, ti), 6 + (tt % 2),
                           'act' if tt % 2 == 0 else 'dve')

            def norm_T(tt):
                norm_C(tt)
                norm_P(tt)

            def evac_copy(dst, src, rd, wr, scale=None):
                n = st['n']
                st['n'] += 1
                if n % 2 == 0:
                    if scale is None:
                        t.op('act', lambda e: e.activation(dst, src, AF.Copy), reads=rd, writes=wr)
                    else:
                        t.op('act', lambda e: e.activation(dst, src, AF.Copy, scale=scale), reads=rd, writes=wr)
                else:
                    if scale is None:
                        t.op('dve', lambda e: e.tensor_copy(dst, src), reads=rd, writes=wr)
                    else:
                        t.op('dve', lambda e: e.tensor_scalar(dst, src, scale, None, ALU.mult), reads=rd, writes=wr)

            def proj_group(g):
                hTg = hT[g % 2]
                rdh = [('hT', g % 2, i) for i in range(4)]
                tok0 = g * 512
                fm = [('qsb', cc, cc * 128) for cc in range(4)] + [('ksb', cc, 512 + cc * 128) for cc in range(4)] + \
                     [('qml', cc, 1536 + cc * 128) for cc in range(4)] + [('kml', cc, 2048 + cc * 128) for cc in range(4)]
                for kind, cc, col0 in fm:
                    b = nbank()
                    bk = 'ps%d' % b
                    for kc in range(8):
                        t.op('pe', lambda e, kc=kc, b=b, col0=col0: e.matmul(PS[b][:, :], lhsT=Wb[:, kc, col0:col0 + 128], rhs=hTg[:, kc, :],
                                                                       start=(kc == 0), stop=(kc == 7)), reads=rdh, writes=[bk])
                    if kind in ('qsb', 'ksb'):
                        o = nost()
                        evac_copy(ost[o][:, :], PS[b][:, :], [bk], [('ost', o)], scale=(SB_SCALE if kind == 'qsb' else None))
                        dst = (qsbT if kind == 'qsb' else ksbT)[cc * 128:(cc + 1) * 128, tok0:tok0 + 512]
                        t.dma('sp', dst, ost[o][:, :], reads=[('ost', o)], writes=[(kind, cc, g)])
                    else:
                        j = cc if kind == 'qml' else 4 + cc
                        ck = ('cb', j)
                        evac_copy(cb[:, j, 3:515], PS[b][:, :], [bk], [ck])
                        a = acc[j % 2]
                        ak = ('acc', j % 2)
                        t.op('dve', lambda e, j=j, a=a: e.tensor_scalar(a[:, :], cb[:, j, 3:515], wcv[:, j, 3:4], None, ALU.mult), reads=[ck], writes=[ak])
                        for tap in (2, 1, 0):
                            t.op('dve', lambda e, j=j, a=a, tap=tap: e.scalar_tensor_tensor(a[:, :], cb[:, j, tap:tap + 512], wcv[:, j, tap:tap + 1], a[:, :],
                                                                                     ALU.mult, ALU.add), reads=[ck, ak], writes=[ak])
                        t.op('pool', lambda e, j=j: e.tensor_copy(cb[:, j, 0:3], cb[:, j, 512:515]), reads=[ck], writes=[ck])
                        if kind == 'qml':
                            o = nost()
                            t.op('act', lambda e, j=j, a=a, o=o: e.activation(ost[o][:, :], a[:, :], AF.Silu, bias=bcv[:, j:j + 1]), reads=[ak], writes=[('ost', o)])
                            t.dma('sp', qmlT[cc * 128:(cc + 1) * 128, tok0:tok0 + 512], ost[o][:, :], reads=[('ost', o)], writes=[('qmlT', cc, g)])
                        else:
                            kb_ = kbf[cc]
                            t.op('act', lambda e, j=j, a=a, kb_=kb_: e.activation(kb_[:, :], a[:, :], AF.Silu, bias=bcv[:, j:j + 1]), reads=[ak], writes=[('kbf', cc)])
                            t.dma('sp', kmlT[cc * 128:(cc + 1) * 128, tok0:tok0 + 512], kb_[:, :], reads=[('kbf', cc)], writes=[('kmlT', cc, g)])

                def k_transposes(cc):
                    tb = 6 + (cc % 2)
                    pb = PSb[tb]
                    for i in range(4):
                        t.op('pe', lambda e, i=i, pb=pb: e.transpose(pb[:, i * 128:(i + 1) * 128], kbf[cc][:, i * 128:(i + 1) * 128], identb[:, :]),
                             reads=[('kbf', cc)], writes=['ps%d' % tb])
                    ks = kst[cc % 2]
                    t.op('dve', lambda e, ks=ks, pb=pb: e.tensor_copy(ks[:, :, :], pb[:, 0:512].rearrange('p (i d) -> p i d', i=4)),
                         reads=['ps%d' % tb], writes=[('kst', cc % 2)])
                    t.dma('sp', kml[tok0:tok0 + 512, cc * 128:(cc + 1) * 128].rearrange('(i p) d -> p i d', p=128), ks[:, :, :],
                          reads=[('kst', cc % 2)], writes=[('kml', cc, g)])

                b = nbank()
                bk = 'ps%d' % b
                for kc in range(8):
                    t.op('pe', lambda e, kc=kc, b=b: e.matmul(PS[b][0:8, :], lhsT=Wb[:, kc, 3584:3592], rhs=hTg[:, kc, :], start=(kc == 0), stop=(kc == 7)),
                         reads=rdh, writes=[bk])
                gs = gst[g % 2]
                t.op('dve', lambda e, b=b, gs=gs: e.tensor_copy(gs[:, :], PS[b][0:8, :]), reads=[bk], writes=[('gst', g % 2)])
                t.dma('sp', gifT[:, tok0:tok0 + 512], gs[:, :], reads=[('gst', g % 2)], writes=[('gifT', g)])
                tmj = [('vsb', 1024, vsb, 0), ('vml', 2560, vml, 0), ('oml', 3072, osig, 0),
                       ('gate', 3592, gsig, 0), ('gate', 4104, gsig, 512), ('gate', 4616, gsig, 1024), ('gate', 5128, gsig, 1536)]
                for ti in range(4):
                    r0 = tok0 + ti * 128
                    if ti == 1:
                        for cc in range(4):
                            k_transposes(cc)
                    if ti == 2 and g + 1 < NG:
                        for tj in range(4):
                            norm_C((g + 1) * 4 + tj)
                    if ti == 3 and g + 1 < NG:
                        for tj in range(4):
                            norm_P((g + 1) * 4 + tj)
                    for kind, col0, dstT, dcol in tmj:
                        b = nbank()
                        bk = 'ps%d' % b
                        for kc in range(8):
                            t.op('pe', lambda e, kc=kc, b=b, col0=col0, ti=ti: e.matmul(PS[b][:, :], lhsT=hTg[:, kc, ti * 128:(ti + 1) * 128],
                                                                                 rhs=Wb[:, kc, col0:col0 + 512], start=(kc == 0), stop=(kc == 7)),
                                 reads=[('hT', g % 2, ti)], writes=[bk])
                        o = nost()
                        if kind in ('vsb', 'vml'):
                            evac_copy(ost[o][:, :], PS[b][:, :], [bk], [('ost', o)])
                        elif kind == 'gate':
                            t.op('act', lambda e, b=b, o=o: e.activation(ost[o][:, :], PS[b][:, :], AF.Sigmoid), reads=[bk], writes=[('ost', o)])
                        else:
                            f = osf[ti % 2]
                            t.op('act', lambda e, b=b, f=f: e.activation(f[:, :], PS[b][:, :], AF.Sigmoid), reads=[bk], writes=[('osf', ti % 2)])
                            t.op('dve', lambda e, f=f, o=o: e.tensor_tensor(ost[o][:, :], f[:, :], ghb[:, :], ALU.mult), reads=[('osf', ti % 2)], writes=[('ost', o)])
                        t.dma('sp', dstT[r0:r0 + 128, dcol:dcol + 512], ost[o][:, :], reads=[('ost', o)], writes=[(kind, col0, g, ti)])

            for tt in range(8):
                load_x(tt)
            for ti in range(4):
                norm_T(ti)
            for g in range(NG):
                proj_group(g)
                if g + 2 < NG:
                    for ti in range(4):
                        load_x((g + 2) * 4 + ti)
            assert st['slot'] >= 4 * len(pieces)
            piece_cast(len(pieces) - 1)
            t.barrier()

    def phase23():
        with ExitStack() as es:
            def sbt(name, shape, dt):
                return es.enter_context(nc.sbuf_tensor(name, shape, dt))
            KT = sbt('KT', [128, 4, S], BF16)
            V = sbt('V', [128, 32, 512], BF16)
            Q = [sbt('Q%d' % i, [128, 4, 512], BF16) for i in range(2)]
            E = [sbt('E%d' % i, [128, 512], F32) for i in range(3)]
            SPt = [sbt('SP%d' % i, [128, 512], BF16) for i in range(3)]
            A = [sbt('A%d' % i, [128, 512], BF16) for i in range(3)]
            R = [sbt('R%d' % i, [128, 512], BF16) for i in range(2)]
            uneg = sbt('uneg', [128, 128], BF16)
            oneg = sbt('oneg', [128, 128], BF16)
            negm = sbt('negm', [128, 4, 512], BF16)
            ostg = [sbt('ostg%d' % i, [64, 512], BF16) for i in range(2)]
            t.dma('sp', uneg[:, :], c_uneg, writes=['uneg'])
            t.dma('sp', oneg[:, :], c_oneg, writes=['oneg'])
            t.dma('sp', negm[:, :, :], c_negm, writes=['negm'])
            for j in range(4):
                t.dma('sp', KT[:, j, :], ksbT[j * 128:(j + 1) * 128, :], writes=[('KT', j)])
            for j in range(4):
                t.dma('sp', V[:, j * 8:(j + 1) * 8, :], vsb[j * 1024:(j + 1) * 1024, :].rearrange('(kb p) c -> p kb c', p=128), writes=[('V', j)])
            GI = sbt('GI', [128, 2, 64], F32); GF = sbt('GF', [128, 2, 64], F32)
            bi_t = sbt('bi_t', [128, 2], F32); bf_t = sbt('bf_t', [128, 2], F32); nbf = sbt('nbf', [128, 2], F32)
            ones = sbt('ones', [128, 64], F32)
            ef = sbt('ef', [128, 2, 64], F32); cs = sbt('cs', [128, 2, 64], F32); aa = sbt('aa', [128, 2, 64], F32)
            amax = sbt('amax', [128, 2], F32); bend = sbt('bend', [128, 2], F32); abe = sbt('abe', [128, 2], F32)
            b4 = sbt('b4', [4, 64], F32); a4 = sbt('a4', [4, 64], F32); mn4 = sbt('mn4', [4, 64], F32); mp4 = sbt('mp4', [4, 64], F32)
            mp = sbt('mp', [128, 2], F32); nM = sbt('nM', [128, 2], F32); thb = sbt('thb', [128, 2], F32); dec = sbt('dec', [128, 2], F32)
            wq = sbt('wq', [128, 2, 64], F32); th = sbt('th', [128, 2, 64], F32)
            wT = sbt('wT', [64, 256], F32); thT = sbt('thT', [64, 256], F32); decb = sbt('decb', [128, 256], F32)
            m01 = sbt('m01', [64, 256], BF16)
            t.dma('sp', ones[:, :], c_ones, writes=['ones'])
            t.dma('sp', m01[:, :], c_m01, writes=['m01'])
            for h in range(4):
                r0, q = (h % 2) * 64, h // 2
                t.dma('sp', GI[r0:r0 + 64, q, :], gifT[h, :].rearrange('(c l) -> c l', l=64), writes=[('GI', h)])
                t.dma('sp', GF[r0:r0 + 64, q, :], gifT[4 + h, :].rearrange('(c l) -> c l', l=64), writes=[('GF', h)])
                t.dma('sp', bi_t[r0:r0 + 64, q:q + 1], b_igate[h:h + 1].partition_broadcast(64), writes=[('bi', h)])
                t.dma('sp', bf_t[r0:r0 + 64, q:q + 1], b_fgate[h:h + 1].partition_broadcast(64), writes=[('bf', h)])
            t.barrier()
            G = ['gp']
            t.op('dve', lambda e: e.tensor_scalar(nbf[:, :], bf_t[:, :], -1.0, None, ALU.mult), reads=G, writes=G)
            for q in range(2):
                t.op('act', lambda e, q=q: e.activation(ef[:, q, :], GF[:, q, :], AF.Exp, bias=nbf[:, q:q + 1], scale=-1.0), reads=G, writes=G)
            t.op('act', lambda e: e.activation(ef[:, :, :], ef[:, :, :], AF.Ln, bias=1.0), reads=G, writes=G)
            for q in range(2):
                t.op('dve', lambda e, q=q: e.tensor_tensor_scan(cs[:, q, :], ones[:, :], ef[:, q, :], 0.0, ALU.mult, ALU.add), reads=G, writes=G)
                t.op('dve', lambda e, q=q: e.scalar_tensor_tensor(aa[:, q, :], GI[:, q, :], bi_t[:, q:q + 1], cs[:, q, :], ALU.add, ALU.add), reads=G, writes=G)
            t.op('dve', lambda e: e.tensor_reduce(amax[:, :], aa[:, :, :], AX.X, ALU.max), reads=G, writes=G)
            t.op('dve', lambda e: e.tensor_scalar(bend[:, :], cs[:, :, 63], -1.0, None, ALU.mult), reads=G, writes=G)
            t.op('dve', lambda e: e.tensor_tensor(abe[:, :], amax[:, :], bend[:, :], ALU.add), reads=G, writes=G)
            t.dma('sp', gs1[0, :].rearrange('(q p) -> p q', p=128), bend[:, :], reads=G, writes=['gs1a'], **NC)
            t.dma('sp', gs1[1, :].rearrange('(q p) -> p q', p=128), abe[:, :], reads=G, writes=['gs1b'], **NC)
            t.dma('sp', b4[:, :], gs1[0, :].rearrange('(h c) -> h c', c=64), reads=['gs1a'], writes=['b4'])
            t.dma('sp', a4[:, :], gs1[1, :].rearrange('(h c) -> h c', c=64), reads=['gs1b'], writes=['a4'])
            t.op('dve', lambda e: e.tensor_tensor_scan(mn4[:, :], b4[:, :], a4[:, :], 0.0, ALU.add, ALU.max), reads=['b4', 'a4'], writes=['mn4'])
            t.op('dve', lambda e: e.memset(mp4[:, 0:1], 0.0), reads=[], writes=['mp4'])
            t.op('dve', lambda e: e.tensor_copy(mp4[:, 1:64], mn4[:, 0:63]), reads=['mn4', 'mp4'], writes=['mp4'])
            t.dma('sp', gs2.rearrange('(h c) -> h c', c=64), mp4[:, :], reads=['mp4'], writes=['gs2'])
            t.dma('sp', mp[:, :], gs2.rearrange('(q p) -> p q', p=128), reads=['gs2'], writes=G, **NC)
            t.op('dve', lambda e: e.tensor_tensor(nM[:, :], mp[:, :], amax[:, :], ALU.max), reads=G, writes=G)
            t.op('dve', lambda e: e.tensor_scalar(nM[:, :], nM[:, :], -1.0, None, ALU.mult), reads=G, writes=G)
            t.op('dve', lambda e: e.tensor_scalar(thb[:, :], nM[:, :], -LN_C, None, ALU.add), reads=G, writes=G)
            t.op('dve', lambda e: e.tensor_tensor(dec[:, :], mp[:, :], nM[:, :], ALU.add), reads=G, writes=G)
            t.op('act', lambda e: e.activation(dec[:, :], dec[:, :], AF.Exp), reads=G, writes=G)
            for q in range(2):
                t.op('act', lambda e, q=q: e.activation(wq[:, q, :], aa[:, q, :], AF.Exp, bias=nM[:, q:q + 1]), reads=G, writes=G)
                t.op('act', lambda e, q=q: e.activation(th[:, q, :], cs[:, q, :], AF.Exp, bias=thb[:, q:q + 1]), reads=G, writes=G)
            t.dma('sp', gs3.rearrange('(q p) -> p q', p=128), dec[:, :], reads=G, writes=['gs3'], **NC)
            t.dma('sp', decb[:, :], gs3.partition_broadcast(128), reads=['gs3'], writes=['decb'])
            for q in range(2):
                t.op('pe', lambda e, q=q: e.transpose(PS[0][0:64, q * 128:(q + 1) * 128], wq[:, q, :], identf[:, :]), reads=G, writes=['ps0'])
                t.op('pe', lambda e, q=q: e.transpose(PS[1][0:64, q * 128:(q + 1) * 128], th[:, q, :], identf[:, :]), reads=G, writes=['ps1'])
            t.op('dve', lambda e: e.tensor_copy(wT[:, :], PS[0][0:64, 0:256]), reads=['ps0'], writes=['wT'])
            t.op('dve', lambda e: e.tensor_copy(thT[:, :], PS[1][0:64, 0:256]), reads=['ps1'], writes=['thT'])
            t.barrier()

            qTs = [sbt('qTs%d' % i, [128, 4, 512], BF16) for i in range(2)]
            kTs = [sbt('kTs%d' % i, [128, 4, 512], BF16) for i in range(2)]
            ktm = [sbt('ktm%d' % i, [64, 8, 512], BF16) for i in range(2)]
            vx = [sbt('vx%d' % i, [64, 8, 4, 129], BF16) for i in range(2)]
            og = [sbt('og%d' % i, [64, 8, 512], BF16) for i in range(2)]
            yst = [sbt('yst%d' % i, [128, 4, 512], BF16) for i in range(2)]
            CT = sbt('CT', [128, 4, 129], F32); CTd = sbt('CTd', [128, 4, 129], F32); CTb = sbt('CTb', [128, 4, 129], BF16)
            SmT = [sbt('SmT%d' % i, [64, 256], BF16) for i in range(2)]
            vw = [sbt('vw%d' % i, [64, 4, 129], BF16) for i in range(2)]
            dd = sbt('dd', [64, 4], F32); rr = sbt('rr', [64, 4], F32); s2 = sbt('s2', [64, 4], F32)
            hh = sbt('hh', [64, 4, 128], F32); sq = sbt('sq', [64, 4, 128], F32); hn = sbt('hn', [64, 4, 128], F32)
            yt = [sbt('yt%d' % i, [64, 512], BF16) for i in range(2)]
            for i in range(2):
                t.op('pool', lambda e, i=i: e.memset(vx[i][:, :, :, :], 1.0), writes=[('vx', i)])
            t.op('dve', lambda e: e.memset(CT[:, :, :], 0.0), writes=['CT'])

            def load_sc(sc):
                sl = sc % 2
                c0 = sc * 512
                t.dma('sp', qTs[sl][:, :, :], qmlT[:, c0:c0 + 512].rearrange('(h p) tk -> p h tk', p=128), writes=[('qTs', sl)])
                t.dma('sp', kTs[sl][:, :, :], kmlT[:, c0:c0 + 512].rearrange('(h p) tk -> p h tk', p=128), writes=[('kTs', sl)])
                t.dma('sp', ktm[sl][:, :, :], kml[c0:c0 + 512, :].rearrange('(c l) d -> l c d', l=64), writes=[('ktm', sl)])
                for ci in range(8):
                    t.dma('sp', vx[sl][:, ci, :, 0:128], vml[c0 + ci * 64:c0 + (ci + 1) * 64, :].rearrange('l (h d) -> l h d', h=4),
                          writes=[('vx', sl)])
                t.dma('sp', og[sl][:, :, :], osig[c0:c0 + 512, :].rearrange('(c l) d -> l c d', l=64), writes=[('og', sl)])

            def XB(j):
                return 4 + j

            def YB(j):
                return 6 + j

            def mstage(c, k):
                sc, ci = c // 8, c % 8
                sl = sc % 2
                tk = slice(ci * 64, (ci + 1) * 64)
                par = c % 2
                if k == 0:
                    if ci == 0 and sc + 1 < 8:
                        load_sc(sc + 1)
                    for h in range(4):
                        j, hl = h // 2, h % 2
                        t.op('pe', lambda e, h=h, j=j, hl=hl: e.matmul(PS[XB(j)][0:64, 258 + hl * 64:258 + (hl + 1) * 64], lhsT=kTs[sl][:, h, tk], rhs=qTs[sl][:, h, tk],
                                                                  start=True, stop=True), reads=[('qTs', sl), ('kTs', sl)], writes=['ps%d' % XB(j)])
                    wbc = wT[:, c:256:64].unsqueeze(2).broadcast_to([64, 4, 129])
                    t.op('pool', lambda e: e.tensor_tensor(vw[par][:, :, :], vx[sl][:, ci, :, :], wbc, ALU.mult), reads=[('vx', sl)], writes=[('vw', par)])
                    dbc = decb[:, c:256:64].unsqueeze(2).broadcast_to([128, 4, 129])
                    t.op('pool', lambda e: e.tensor_tensor(CTd[:, :, :], CT[:, :, :], dbc, ALU.mult), reads=['CT'], writes=['CTd'])
                    t.op('pool', lambda e: e.tensor_copy(CTb[:, :, :], CTd[:, :, :]), reads=['CTd'], writes=['CTb'])
                elif k == 1:
                    for j in range(2):
                        t.op('dve', lambda e, j=j: e.tensor_tensor(SmT[par][:, j * 128:(j + 1) * 128], PS[XB(j)][0:64, 258:386], m01[:, 0:128], ALU.mult),
                             reads=['ps%d' % XB(j)], writes=[('SmT', par, j)])
                elif k == 2:
                    for h in range(4):
                        j, off = h // 2, (h % 2) * 129
                        t.op('pe', lambda e, h=h, j=j, off=off: e.matmul(PS[XB(j)][0:64, off:off + 129], lhsT=SmT[par][:, h * 64:(h + 1) * 64], rhs=vw[par][:, h, :],
                                                                    start=True, stop=False), reads=[('SmT', par, j), ('vw', par)], writes=['ps%d' % XB(j)])
                        t.op('pe', lambda e, h=h, j=j, off=off: e.matmul(PS[XB(j)][0:64, off:off + 129], lhsT=qTs[sl][:, h, tk], rhs=CTb[:, h, :],
                                                                    start=False, stop=True), reads=[('qTs', sl), 'CTb'], writes=['ps%d' % XB(j)])
                    for h in range(4):
                        j, off = h // 2, (h % 2) * 129
                        t.op('pe', lambda e, h=h, j=j, off=off: e.matmul(PS[YB(j)][:, off:off + 129], lhsT=ktm[sl][:, ci, h * 128:(h + 1) * 128], rhs=vw[par][:, h, :],
                                                                    start=True, stop=True), reads=[('ktm', sl), ('vw', par)], writes=['ps%d' % YB(j)])
                elif k == 3:
                    for j in range(2):
                        t.op('dve', lambda e, j=j: e.tensor_tensor(CT[:, 2 * j:2 * j + 2, :], CTd[:, 2 * j:2 * j + 2, :],
                                                                   PS[YB(j)][:, 0:258].rearrange('p (a b) -> p a b', a=2), ALU.add),
                             reads=['CTd', 'ps%d' % YB(j)], writes=['CT'])
                    for j in range(2):
                        den = PS[XB(j)][0:64, 128:258:129]
                        t.op('dve', lambda e, j=j, den=den: e.tensor_tensor(dd[:, 2 * j:2 * j + 2], den, thT[:, 2 * j * 64 + c:2 * j * 64 + c + 65:64], ALU.max),
                             reads=['ps%d' % XB(j)], writes=['dd'])
                        t.op('dve', lambda e, j=j, den=den: e.scalar_tensor_tensor(dd[:, 2 * j:2 * j + 2], den, -1.0, dd[:, 2 * j:2 * j + 2], ALU.mult, ALU.max),
                             reads=['ps%d' % XB(j), 'dd'], writes=['dd'])
                    t.op('dve', lambda e: e.reciprocal(rr[:, :], dd[:, :]), reads=['dd'], writes=['rr'])
                    for j in range(2):
                        t.op('dve', lambda e, j=j: e.tensor_tensor(hh[:, 2 * j:2 * j + 2, :], PS[XB(j)][0:64, 0:258].rearrange('p (a b) -> p a b', a=2)[:, :, 0:128],
                                                                   rr[:, 2 * j:2 * j + 2].unsqueeze(2).broadcast_to([64, 2, 128]), ALU.mult),
                             reads=['rr', 'ps%d' % XB(j)], writes=['hh'])
                    t.op('pool', lambda e: e.tensor_tensor(sq[:, :, :], hh[:, :, :], hh[:, :, :], ALU.mult), reads=['hh'], writes=['sq'])
                elif k == 4:
                    t.op('dve', lambda e: e.tensor_reduce(s2[:, :], sq[:, :, :], AX.X, ALU.add), reads=['sq'], writes=['s2'])
                    t.op('act', lambda e: e.activation(s2[:, :], s2[:, :], AF.Ln, scale=1.0 / 128, bias=EPS), reads=['s2'], writes=['s2'])
                    t.op('act', lambda e: e.activation(s2[:, :], s2[:, :], AF.Exp, scale=-0.5), reads=['s2'], writes=['s2'])
                    t.op('dve', lambda e: e.tensor_tensor(hn[:, :, :], hh[:, :, :], s2[:, :].unsqueeze(2).broadcast_to([64, 4, 128]), ALU.mult),
                         reads=['hh', 's2'], writes=['hn'])
                    t.op('pool', lambda e: e.tensor_tensor(yt[par][:, :], hn[:, :, :].rearrange('p a b -> p (a b)'), og[sl][:, ci, :], ALU.mult),
                         reads=['hn', ('og', sl)], writes=[('yt', par)])
                elif k == 5:
                    for h in range(4):
                        j, hl = h // 2, h % 2
                        t.op('pe', lambda e, h=h, j=j, hl=hl: e.transpose(PSb[YB(j)][:, 516 + hl * 64:516 + (hl + 1) * 64], yt[par][:, h * 128:(h + 1) * 128], identb[0:64, 0:64]),
                             reads=[('yt', par)], writes=['ps%d' % YB(j)])
                    for j in range(2):
                        t.op('dve', lambda e, j=j: e.tensor_copy(yst[sl][:, 2 * j:2 * j + 2, tk], PSb[YB(j)][:, 516:644].rearrange('p (h tk) -> p h tk', h=2)),
                             reads=['ps%d' % YB(j)], writes=[('yst', sl)])
                    if ci == 7:
                        t.dma('sp', ymlT[:, sc * 512:(sc + 1) * 512].rearrange('(h p) tk -> p h tk', p=128), yst[sl][:, :, :],
                              reads=[('yst', sl)], writes=[('ymlT', sc)])

            def load_q(g):
                t.dma('sp', Q[g % 2][:, :, :], qsbT[:, g * 512:(g + 1) * 512].rearrange('(j p) tk -> p j tk', p=128), writes=[('Q', g % 2)])

            units = []
            m = 0
            for g in range(NG):
                for h in range(8):
                    kbs = list(range(4 * g + 3, -1, -1))
                    for n_, kb in enumerate(kbs):
                        units.append(dict(g=g, h=h, kb=kb, first=(n_ == 0), last=(n_ == len(kbs) - 1), m=m, newg=(h == 0 and n_ == 0)))
                    m += 1
            for i_, u in enumerate(units):
                u['i'] = i_

            def c0_of(u):
                return max(0, (u['kb'] - 4 * u['g']) * 128)

            def S1(u):
                g, h, kb, i = u['g'], u['h'], u['kb'], u['i']
                if u['newg'] and g + 1 < NG:
                    load_q(g + 1)
                pb = i % 3
                j, r0 = h // 2, (h % 2) * 64
                diag = kb >= 4 * g
                c0 = c0_of(u)
                t.op('pe', lambda e: e.matmul(PS[pb][:, c0:512], lhsT=KT[r0:r0 + 64, j, kb * 128:(kb + 1) * 128], rhs=Q[g % 2][r0:r0 + 64, j, c0:512],
                                              start=True, stop=(not diag)), reads=[('Q', g % 2)], writes=['ps%d' % pb])
                if diag:
                    di = kb - 4 * g
                    t.op('pe', lambda e: e.matmul(PS[pb][:, c0:c0 + 128], lhsT=identb[:, :], rhs=negm[:, di, c0:c0 + 128], start=False, stop=True,
                                                  skip_group_check=True), reads=[], writes=['ps%d' % pb])

            def S2a(u):
                i = u['i']
                pb, sl = i % 3, i % 3
                c0 = c0_of(u)
                t.op('act', lambda e: e.activation(E[sl][:, c0:512], PS[pb][:, c0:512], AF.Exp), reads=['ps%d' % pb], writes=[('E', sl)])

            def S2b(u):
                i = u['i']
                pb, sl = i % 3, i % 3
                c0 = c0_of(u)
                t.op('act', lambda e: e.activation(SPt[sl][:, c0:512], E[sl][:, c0:512], AF.Ln, bias=1.0), reads=[('E', sl)], writes=[('SP', sl)])

            def S3(u):
                i, m_ = u['i'], u['m']
                pb, sl, rs = i % 3, i % 3, m_ % 2
                c0 = c0_of(u)
                t.op('pe', lambda e: e.matmul(PS[pb][:, c0:512], lhsT=uneg[:, :], rhs=SPt[sl][:, c0:512], start=False, stop=u['first'], skip_group_check=True),
                     reads=[('SP', sl)], writes=['ps%d' % pb])
                if not u['first']:
                    t.op('pe', lambda e: e.matmul(PS[pb][:, c0:512], lhsT=oneg[:, :], rhs=R[rs][:, c0:512], start=False, stop=True, skip_group_check=True),
                         reads=[('R', rs)], writes=['ps%d' % pb])
                if not u['last']:
                    if u['first']:
                        if c0 > 0:
                            t.op('dve', lambda e: e.memset(R[rs][:, 0:c0], 0.0), reads=[], writes=[('R', rs)])
                        t.op('dve', lambda e: e.tensor_copy(R[rs][:, c0:512], SPt[sl][:, c0:512]), reads=[('SP', sl), ('R', rs)], writes=[('R', rs)])
                    else:
                        t.op('dve', lambda e: e.tensor_tensor(R[rs][:, c0:512], R[rs][:, c0:512], SPt[sl][:, c0:512], ALU.add),
                             reads=[('SP', sl), ('R', rs)], writes=[('R', rs)])

            def S4(u):
                i = u['i']
                pb, sl = i % 3, i % 3
                c0 = c0_of(u)
                t.op('act', lambda e: e.activation(A[sl][:, c0:512], PS[pb][:, c0:512], AF.Exp), reads=['ps%d' % pb], writes=[('A', sl)])

            def S5(u):
                g, h, kb, i, m_ = u['g'], u['h'], u['kb'], u['i'], u['m']
                sl = i % 3
                o0 = (m_ % 2) * 64
                ok = ('ps3', m_ % 2)
                c0 = c0_of(u)
                t.op('pe', lambda e: e.matmul(PS[3][o0:o0 + 64, c0:512], lhsT=V[:, kb, h * 64:(h + 1) * 64], rhs=A[sl][:, c0:512], start=u['first'], stop=u['last'],
                                              skip_group_check=True), reads=[('A', sl)], writes=[ok])
                if u['last']:
                    os_ = ostg[m_ % 2]
                    t.op('dve', lambda e: e.tensor_copy(os_[:, :], PS[3][o0:o0 + 64, :]), reads=[ok], writes=[('ostg', m_ % 2)])
                    t.dma('sp', ysbT[h * 64:(h + 1) * 64, g * 512:(g + 1) * 512], os_[:, :], reads=[('ostg', m_ % 2)], writes=[('ysbT', m_)])

            load_q(0)
            load_sc(0)
            n = len(units)
            GAP = 3
            for i in range(n + 2):
                if i < n:
                    S1(units[i])
                    S2a(units[i])
                    S2b(units[i])
                if 0 <= i - 1 < n:
                    S3(units[i - 1])
                    S4(units[i - 1])
                if 0 <= i - 2 < n:
                    S5(units[i - 2])
                if i % GAP == 0:
                    sidx = i // GAP
                    c, k = sidx // 6, sidx % 6
                    if c < 64:
                        mstage(c, k)
            t.barrier()

    def phase4a():
        with ExitStack() as es:
            def sbt(name, shape, dt):
                return es.enter_context(nc.sbuf_tensor(name, shape, dt))
            Wsb = sbt('Wsb', [128, 4, D], BF16); Wml = sbt('Wml', [128, 4, D], BF16); Wo = sbt('Wo', [128, 8, D], BF16)
            gbc = sbt('gbc', [128, D], F32)
            t.dma('sp', gbc[:, :], g_mix_post.partition_broadcast(128), writes=['gbc'])
            load_bf16_weight(Wsb, Wsb_b, 4, 'wsb')
            load_bf16_weight(Wml, Wml_b, 4, 'wml')
            load_bf16_weight(Wo, Wo_b, 8, 'wo')
            t.barrier()
            ysT = [sbt('ysT%d' % i, [128, 4, 512], BF16) for i in range(2)]
            ymT = [sbt('ymT%d' % i, [128, 4, 512], BF16) for i in range(2)]
            gsx = [sbt('gsx%d' % i, [128, 2048], BF16) for i in range(3)]
            xs = [sbt('xa%d' % i, [128, D], F32) for i in range(3)]
            t1 = sbt('t1', [128, D], F32); t2 = sbt('t2', [128, D], F32)
            mg = [sbt('mg%d' % i, [128, D], BF16) for i in range(2)]
            mT = [sbt('mT%d' % i, [128, 8, 128], BF16) for i in range(2)]
            junk = sbt('junk4', [128, 512], BF16)
            ssq = sbt('ssq4', [128, 4], F32)
            tmp = sbt('tmp4', [128, D], F32)
            xo = [sbt('xo%d' % i, [128, D], F32) for i in range(2)]
            junk2 = sbt('junk4b', [128, D], BF16)
            h2 = [sbt('h2_%d' % i, [128, D], BF16) for i in range(2)]
            h2T = [sbt('h2T_%d' % i, [128, 8, 128], BF16) for i in range(2)]

            def tileB2a(tt):
                sl = tt % 2
                rms_rstd(xo[sl][:, :], [('xo', sl, 0), ('xo', sl, 1)], junk2, ssq[:, 3:4], ('ssq4', 3))
                t.op('dve', lambda e: e.tensor_scalar(h2[sl][:, :], xo[sl][:, :], ssq[:, 3:4], None, ALU.mult),
                     reads=[('xo', sl, 0), ('xo', sl, 1), ('ssq4', 3)], writes=[('h2', sl)])

            def tileB2b(tt):
                sl = tt % 2
                transpose8(h2[sl], ('h2', sl), 8, h2T[sl][:, :, :], ('h2T', sl), 6 + sl, 'dve')
                t.dma('sp', h2T_d[:, tt * 128:(tt + 1) * 128].rearrange('(k p) tk -> p k tk', p=128), h2T[sl][:, :, :],
                      reads=[('h2T', sl)], writes=[('h2T_d', tt)])

            def load_g(g):
                sl = g % 2
                t.dma('sp', ysT[sl][:, :, :], ysbT[:, g * 512:(g + 1) * 512].rearrange('(j p) tk -> p j tk', p=128), writes=[('ysT', sl)])
                t.dma('sp', ymT[sl][:, :, :], ymlT[:, g * 512:(g + 1) * 512].rearrange('(j p) tk -> p j tk', p=128), writes=[('ymT', sl)])

            def load_t(tt):
                sl = tt % 3
                t.dma('sp', gsx[sl][:, :], gsig[tt * 128:(tt + 1) * 128, :], writes=[('gsx', sl)])
                t.dma('sp', xs[sl][:, :], x[tt * 128:(tt + 1) * 128, :], writes=[('xa', sl)])

            def tileA1(tt):
                g, ti, sl = tt // 4, tt % 4, tt % 2
                gl = g % 2
                tks = slice(ti * 128, (ti + 1) * 128)
                for half in range(2):
                    cs_ = slice(half * 512, (half + 1) * 512)
                    for (Ysrc, Wsrc, bk, yk) in ((ysT, Wsb, half, 'ysT'), (ymT, Wml, 2 + half, 'ymT')):
                        for j in range(4):
                            t.op('pe', lambda e, Ysrc=Ysrc, Wsrc=Wsrc, bk=bk, j=j, cs_=cs_: e.matmul(PS[bk][:, :], lhsT=Ysrc[gl][:, j, tks], rhs=Wsrc[:, j, cs_],
                                                                                                start=(j == 0), stop=(j == 3)),
                                 reads=[(yk, gl)], writes=['ps%d' % bk])
                for half in range(2):
                    cs_ = slice(half * 512, (half + 1) * 512)
                    t.op('dve', lambda e, half=half, cs_=cs_: e.tensor_tensor(t1[:, cs_], PS[half][:, :], gsx[tt % 3][:, cs_], ALU.mult),
                         reads=['ps%d' % half, ('gsx', tt % 3)], writes=[('t1', half)])
                    t.op('dve', lambda e, half=half, cs_=cs_: e.tensor_tensor(t2[:, cs_], PS[2 + half][:, :], gsx[tt % 3][:, 1024 + half * 512:1024 + (half + 1) * 512], ALU.mult),
                         reads=['ps%d' % (2 + half), ('gsx', tt % 3)], writes=[('t2', half)])
                    t.op('pool', lambda e, cs_=cs_: e.tensor_tensor(mg[sl][:, cs_], t1[:, cs_], t2[:, cs_], ALU.add),
                         reads=[('t1', half), ('t2', half)], writes=[('mg', sl, half)])

            def tileA2(tt):
                sl = tt % 2
                transpose8(mg[sl], [('mg', sl, 0), ('mg', sl, 1)], 8, mT[sl][:, :, :], ('mT', sl), 6 + sl, 'act')

            def tileB(tt):
                sl = tt % 2
                for half in range(2):
                    cs_ = slice(half * 512, (half + 1) * 512)
                    bk = 4 + half
                    for kc in range(8):
                        t.op('pe', lambda e, kc=kc, bk=bk, cs_=cs_: e.matmul(PS[bk][:, :], lhsT=mT[sl][:, kc, :], rhs=Wo[:, kc, cs_], start=(kc == 0), stop=(kc == 7)),
                             reads=[('mT', sl)], writes=['ps%d' % bk])
                for half in range(2):
                    t.op('act', lambda e, half=half: e.activation(junk[:, :], PS[4 + half][:, :], AF.Square, accum_out=ssq[:, half:half + 1]),
                         reads=['ps%d' % (4 + half)], writes=['junk4', ('ssq4', half)])
                t.op('dve', lambda e: e.tensor_tensor(ssq[:, 2:3], ssq[:, 0:1], ssq[:, 1:2], ALU.add), reads=[('ssq4', 0), ('ssq4', 1)], writes=[('ssq4', 2)])
                t.op('act', lambda e: e.activation(ssq[:, 2:3], ssq[:, 2:3], AF.Sqrt, scale=1.0 / 1024, bias=EPS), reads=[('ssq4', 2)], writes=[('ssq4', 2)])
                t.op('dve', lambda e: e.reciprocal(ssq[:, 2:3], ssq[:, 2:3]), reads=[('ssq4', 2)], writes=[('ssq4', 2)])
                for half in range(2):
                    cs_ = slice(half * 512, (half + 1) * 512)
                    t.op('dve', lambda e, half=half, cs_=cs_: e.tensor_tensor(tmp[:, cs_], PS[4 + half][:, :], gbc[:, cs_], ALU.mult),
                         reads=['ps%d' % (4 + half)], writes=[('tmp4', half)])
                    t.op('dve', lambda e, cs_=cs_: e.scalar_tensor_tensor(xo[sl][:, cs_], tmp[:, cs_], ssq[:, 2:3], xs[tt % 3][:, cs_], ALU.mult, ALU.add),
                         reads=[('tmp4', half), ('ssq4', 2), ('xa', tt % 3)], writes=[('xo', sl, half)])
                t.dma('sp', x1[tt * 128:(tt + 1) * 128, :], xo[sl][:, :], reads=[('xo', sl, 0), ('xo', sl, 1)], writes=[('x1', tt)])

            load_g(0)
            load_t(0)
            load_t(1)
            load_t(2)
            tileA1(0)
            tileA2(0)
            for tt in range(NT):
                if (tt + 1) % 4 == 0 and (tt + 1) // 4 + 1 < NG:
                    load_g((tt + 1) // 4 + 1)
                if tt == 0:
                    load_g(1)
                if tt + 1 < NT:
                    tileA1(tt + 1)
                if tt >= 1:
                    tileB2b(tt - 1)
                tileB(tt)
                tileB2a(tt)
                if tt + 3 < NT:
                    load_t(tt + 3)
                if tt + 1 < NT:
                    tileA2(tt + 1)
            tileB2b(NT - 1)
            t.barrier()

    def phase4b():
        with ExitStack() as es:
            def sbt(name, shape, dt):
                return es.enter_context(nc.sbuf_tensor(name, shape, dt))
            Wup = sbt('Wup', [128, 8, 4096], BF16); Wdn = sbt('Wdn', [128, 32, D], BF16)
            gbc = sbt('gbc5', [128, D], F32)
            t.dma('sp', gbc[:, :], g_mlp_post.partition_broadcast(128), writes=['gbc'])
            hT = [sbt('hT5_%d' % i, [128, 8, 512], BF16) for i in range(2)]
            xr = [sbt('xr%d' % i, [128, D], F32) for i in range(3)]

            def load_h(g):
                t.dma('sp', hT[g % 2][:, :, :], h2T_d[:, g * 512:(g + 1) * 512].rearrange('(k p) tk -> p k tk', p=128), writes=[('hT5', g % 2)])

            def load_xr(tt):
                t.dma('sp', xr[tt % 3][:, :], x1[tt * 128:(tt + 1) * 128, :], writes=[('xr', tt % 3)])

            load_h(0)
            load_bf16_weight(Wup, Wup_b, 8, 'wup')
            load_h(1)
            load_bf16_weight(Wdn, Wdn_b, 32, 'wdn')
            for tt in range(3):
                load_xr(tt)
            UT = sbt('UT', [128, 32, 512], BF16)
            rl = [sbt('rl%d' % i, [128, 512], F32) for i in range(2)]
            junk = sbt('junk5', [128, 512], BF16)
            ssq = sbt('ssq5', [128, 8], F32)
            tmp = sbt('tmp5', [128, 512], F32)
            xo = [sbt('xo5_%d' % i, [128, 512], F32) for i in range(2)]
            rdw_up = [('wup', kc) for kc in range(8)]
            rdw_dn = [('wdn', f) for f in range(32)]

            def group(g):
                hTg = hT[g % 2]
                for f in range(32):
                    bk = f % 4
                    for kc in range(8):
                        t.op('pe', lambda e, kc=kc, bk=bk, f=f: e.matmul(PS[bk][:, :], lhsT=Wup[:, kc, f * 128:(f + 1) * 128], rhs=hTg[:, kc, :], start=(kc == 0), stop=(kc == 7)),
                             reads=[('hT5', g % 2)] + (rdw_up if g == 0 else []), writes=['ps%d' % bk])
                    r = rl[f % 2]
                    t.op('act', lambda e, bk=bk, r=r: e.activation(r[:, :], PS[bk][:, :], AF.Relu), reads=['ps%d' % bk], writes=[('rl', f % 2)])
                    t.op('pool', lambda e, f=f, r=r: e.tensor_tensor(UT[:, f, :], r[:, :], r[:, :], ALU.mult), reads=[('rl', f % 2)], writes=[('UT', f)])
                rdu = [('UT', f) for f in range(32)]
                for ti in range(4):
                    tt = g * 4 + ti
                    xs_ = xr[tt % 3]
                    b0 = 4 + 2 * (ti % 2)
                    for half in range(2):
                        bk = b0 + half
                        cs_ = slice(half * 512, (half + 1) * 512)
                        for f in range(32):
                            t.op('pe', lambda e, f=f, bk=bk, cs_=cs_, ti=ti: e.matmul(PS[bk][:, :], lhsT=UT[:, f, ti * 128:(ti + 1) * 128], rhs=Wdn[:, f, cs_],
                                                                                  start=(f == 0), stop=(f == 31)),
                                 reads=rdu + (rdw_dn if (g == 0 and ti == 0) else []), writes=['ps%d' % bk])
                    c0 = 2 * (ti % 2)
                    for half in range(2):
                        t.op('act', lambda e, half=half: e.activation(junk[:, :], PS[b0 + half][:, :], AF.Square, accum_out=ssq[:, c0 + half:c0 + half + 1]),
                             reads=['ps%d' % (b0 + half)], writes=['junk', ('ssq5', c0 + half)])
                    sc_ = ssq[:, 4 + ti % 2:5 + ti % 2]
                    sk_ = ('ssq5', 4 + ti % 2)
                    t.op('dve', lambda e: e.tensor_tensor(sc_, ssq[:, c0:c0 + 1], ssq[:, c0 + 1:c0 + 2], ALU.add), reads=[('ssq5', c0), ('ssq5', c0 + 1)], writes=[sk_])
                    t.op('act', lambda e: e.activation(sc_, sc_, AF.Sqrt, scale=1.0 / 1024, bias=EPS), reads=[sk_], writes=[sk_])
                    t.op('dve', lambda e: e.reciprocal(sc_, sc_), reads=[sk_], writes=[sk_])
                    for half in range(2):
                        cs_ = slice(half * 512, (half + 1) * 512)
                        t.op('dve', lambda e, half=half, cs_=cs_: e.tensor_tensor(tmp[:, :], PS[b0 + half][:, :], gbc[:, cs_], ALU.mult),
                             reads=['ps%d' % (b0 + half), 'gbc'], writes=['tmp5'])
                        t.op('dve', lambda e, half=half, cs_=cs_: e.scalar_tensor_tensor(xo[half][:, :], tmp[:, :], sc_, xs_[:, cs_], ALU.mult, ALU.add),
                             reads=['tmp5', sk_, ('xr', tt % 3)], writes=[('xo5', half)])
                        t.dma('sp', x2[tt * 128:(tt + 1) * 128, cs_], xo[half][:, :], reads=[('xo5', half)], writes=[('x2', tt, half)])
                    if tt + 3 < NT:
                        load_xr(tt + 3)

            for g in range(NG):
                group(g)
                if g + 2 < NG:
                    load_h(g + 2)
            t.barrier()

    def phase4c():
        with ExitStack() as es:
            def sbt(name, shape, dt):
                return es.enter_context(nc.sbuf_tensor(name, shape, dt))
            Wg = sbt('Wg', [128, 8, D], BF16); Wp = sbt('Wp', [128, 2, D], BF16)
            gbc = sbt('gbc6', [128, D], F32)
            t.dma('sp', gbc[:, :], g_ple_post.partition_broadcast(128), writes=['gbc'])
            load_bf16_weight(Wg, Wg_b, 8, 'wg')
            load_bf16_weight(Wp, Wp_b, 2, 'wp')
            t.barrier()
            xs = [sbt('xc%d' % i, [128, D], F32) for i in range(3)]
            pt = [sbt('pt%d' % i, [128, 256], F32) for i in range(3)]
            pbf = [sbt('pbf%d' % i, [128, 256], BF16) for i in range(2)]
            hb = [sbt('hb6_%d' % i, [128, D], BF16) for i in range(2)]
            hT = [sbt('hT6_%d' % i, [128, 8, 128], BF16) for i in range(2)]
            pT = [sbt('pT6_%d' % i, [128, 2, 128], BF16) for i in range(2)]
            sg = sbt('sg6', [128, D], F32); ee = sbt('ee6', [128, D], F32)
            junk = sbt('junk6', [128, D], BF16)
            ssq = sbt('ssq6', [128, 4], F32)
            tmp = sbt('tmp6', [128, D], F32)
            xo = [sbt('xo6_%d' % i, [128, D], F32) for i in range(2)]

            def load_t(tt):
                sl = tt % 3
                t.dma('sp', xs[tt % 3][:, :], x2[tt * 128:(tt + 1) * 128, :], writes=[('xc', tt % 3)])
                t.dma('sp', pt[tt % 3][:, :], p_in[tt * 128:(tt + 1) * 128, :], writes=[('pt', tt % 3)])

            def tileC12(tt):
                sl = tt % 2
                rms_rstd(xs[tt % 3][:, :], [('xc', tt % 3)], junk, ssq[:, 0:1], ('ssq6', 0))
                t.op('dve', lambda e: e.tensor_scalar(hb[sl][:, :], xs[tt % 3][:, :], ssq[:, 0:1], None, ALU.mult), reads=[('xc', tt % 3), ('ssq6', 0)], writes=[('hb6', sl)])
                t.op('pool', lambda e: e.tensor_copy(pbf[sl][:, :], pt[tt % 3][:, :]), reads=[('pt', tt % 3)], writes=[('pbf', sl)])
                transpose8(hb[sl], ('hb6', sl), 8, hT[sl][:, :, :], ('hT6', sl), 6, 'act')
                transpose8(pbf[sl], ('pbf', sl), 2, pT[sl][:, :, :], ('pT6', sl), 7, 'dve')

            def tileC3(tt):
                sl = tt % 2
                for half in range(2):
                    cs_ = slice(half * 512, (half + 1) * 512)
                    for kc in range(8):
                        t.op('pe', lambda e, kc=kc, half=half, cs_=cs_: e.matmul(PS[half][:, :], lhsT=hT[sl][:, kc, :], rhs=Wg[:, kc, cs_], start=(kc == 0), stop=(kc == 7)),
                             reads=[('hT6', sl)], writes=['ps%d' % half])
                    for j in range(2):
                        t.op('pe', lambda e, j=j, half=half, cs_=cs_: e.matmul(PS[2 + half][:, :], lhsT=pT[sl][:, j, :], rhs=Wp[:, j, cs_], start=(j == 0), stop=(j == 1)),
                             reads=[('pT6', sl)], writes=['ps%d' % (2 + half)])

            def tileC4(tt):
                sl = tt % 2
                for half in range(2):
                    cs_ = slice(half * 512, (half + 1) * 512)
                    t.op('act', lambda e, half=half, cs_=cs_: e.activation(sg[:, cs_], PS[half][:, :], AF.Sigmoid), reads=['ps%d' % half], writes=[('sg6', half)])
                    t.op('dve', lambda e, half=half, cs_=cs_: e.tensor_tensor(ee[:, cs_], PS[2 + half][:, :], sg[:, cs_], ALU.mult),
                         reads=['ps%d' % (2 + half), ('sg6', half)], writes=[('ee6', half)])
                rms_rstd(ee[:, :], [('ee6', 0), ('ee6', 1)], junk, ssq[:, 1:2], ('ssq6', 1))
                t.op('pool', lambda e: e.tensor_tensor(tmp[:, :], ee[:, :], gbc[:, :], ALU.mult), reads=[('ee6', 0), ('ee6', 1)], writes=['tmp6'])
                t.op('dve', lambda e: e.scalar_tensor_tensor(xo[sl][:, :], tmp[:, :], ssq[:, 1:2], xs[tt % 3][:, :], ALU.mult, ALU.add),
                     reads=['tmp6', ('ssq6', 1), ('xc', tt % 3)], writes=[('xo6', sl)])
                t.dma('sp', y[tt * 128:(tt + 1) * 128, :], xo[sl][:, :], reads=[('xo6', sl)], writes=[('y', tt)])

            load_t(0)
            load_t(1)
            load_t(2)
            tileC12(0)
            for tt in range(NT):
                tileC3(tt)
                if tt + 1 < NT:
                    tileC12(tt + 1)
                tileC4(tt)
                if tt + 3 < NT:
                    load_t(tt + 3)
            t.barrier()

    if upto >= 1:
        phase1()
    if upto >= 3:
        phase23()
    if upto >= 4:
        phase4a()
    if upto >= 5:
        phase4b()
    if upto >= 6:
        phase4c()
    t.barrier()
    return nc, t


def make_in_maps(inputs):
    c = host_consts()
    maps = []
    sq = {k: np.ascontiguousarray(np.asarray(v, dtype=np.float32)[0]) for k, v in inputs.items() if k not in ('x',)}
    xx = np.asarray(inputs['x'], dtype=np.float32)
    for b in range(8):
        m = dict(c)
        m['x'] = np.ascontiguousarray(xx[b])
        m['p'] = np.ascontiguousarray(sq['p'][b])
        for k, v in sq.items():
            if k != 'p':
                m[k] = v
        maps.append(m)
    return maps


def kernel(**inputs):
    nc, _ = build(debug=False)
    maps = make_in_maps(inputs)
    res = run_bass_kernel_spmd(nc, maps, core_ids=list(range(8)))
    out = np.stack([np.asarray(r['y'], dtype=np.float32) for r in res.results], axis=0)
    return out
```

```python
import os
import math
from contextlib import ExitStack
import numpy as np
import ml_dtypes
import concourse.bass as bass
import concourse.mybir as mybir
from concourse.bass_utils import run_bass_kernel_spmd

F32 = mybir.dt.float32
BF16 = mybir.dt.bfloat16
AF = mybir.ActivationFunctionType
ALU = mybir.AluOpType
AX = mybir.AxisListType

S = 4096
D = 1024
NCOL = 5640
NG = 8
NT = 32
EPS = 1e-6
SB_SCALE = 1.0 / 8.0
LN_C = -0.5 * math.log(128.0)
NEG = -30000.0


class Trk:
    SAME_ENGINE_SYNC = True
    NDMA = 8

    def __init__(self, nc):
        self.nc = nc
        self.eng = {'pe': nc.tensor, 'act': nc.scalar, 'dve': nc.vector, 'pool': nc.gpsimd, 'sp': nc.sync}
        self.sem, self.cnt = {}, {}
        for e in ('pe', 'act', 'dve', 'pool'):
            self.sem[e] = nc.alloc_semaphore('c_' + e)
            self.cnt[e] = 0
        self.dsem, self.dcnt = {}, {}
        for q in ('sp', 'act', 'pool'):
            self.dsem[q] = [nc.alloc_semaphore('d_%s%d' % (q, i)) for i in range(self.NDMA)]
            self.dcnt[q] = 0
        self.waited, self.lastw, self.readers, self.semobj = {}, {}, {}, {}
        for e, s in self.sem.items():
            self.semobj[('c', e)] = s
        for q, l in self.dsem.items():
            for i, s in enumerate(l):
                self.semobj[('d', q, i)] = s
        self.nops = 0

    def _wait(self, e, tok):
        if tok is None:
            return
        sk, val = tok
        if sk[0] == 'c' and sk[1] == e:
            if e == 'pe' or not self.SAME_ENGINE_SYNC:
                return
        if self.waited.get((e, sk), 0) >= val:
            return
        self.eng[e].wait_ge(self.semobj[sk], val)
        self.waited[(e, sk)] = val

    def _deps(self, e, reads, writes):
        need = {}

        def add(tok):
            if tok is not None and need.get(tok[0], 0) < tok[1]:
                need[tok[0]] = tok[1]
        for b in reads:
            add(self.lastw.get(b))
        for b in writes:
            add(self.lastw.get(b))
            for r in self.readers.get(b, ()):
                add(r)
        for sk, val in need.items():
            self._wait(e, (sk, val))

    def _commit(self, tok, reads, writes):
        for b in reads:
            lst = self.readers.setdefault(b, [])
            for k_, r in enumerate(lst):
                if r[0] == tok[0]:
                    lst[k_] = tok
                    break
            else:
                lst.append(tok)
        for b in writes:
            self.lastw[b] = tok
            self.readers[b] = []

    def op(self, e, fn, reads=(), writes=()):
        self._deps(e, reads, writes)
        ins = fn(self.eng[e])
        self.cnt[e] += 1
        ins.then_inc(self.sem[e], 1)
        tok = (('c', e), self.cnt[e])
        self._commit(tok, reads, writes)
        self.nops += 1
        return tok

    def dma(self, q, out, in_, reads=(), writes=(), **kw):
        i = self.dcnt[q]
        slot, rnd = i % self.NDMA, i // self.NDMA
        sk = ('d', q, slot)
        if rnd > 0:
            self._wait(q, (sk, 16 * rnd))
        self._deps(q, reads, writes)
        self.eng[q].dma_start(out=out, in_=in_, **kw).then_inc(self.semobj[sk], 16)
        self.dcnt[q] += 1
        tok = (sk, 16 * (rnd + 1))
        self._commit(tok, reads, writes)
        self.nops += 1
        return tok

    def barrier(self):
        toks = [(('c', e), self.cnt[e]) for e in self.sem if self.cnt[e] > 0]
        for q in self.dsem:
            n = self.dcnt[q]
            for s in range(self.NDMA):
                k = (n - s + self.NDMA - 1) // self.NDMA
                if k > 0:
                    toks.append((('d', q, s), 16 * k))
        for e in ('pe', 'act', 'dve', 'pool', 'sp'):
            for sk, val in toks:
                if sk[0] == 'c' and sk[1] == e:
                    continue
                if self.waited.get((e, sk), 0) >= val:
                    continue
                self.eng[e].wait_ge(self.semobj[sk], val)
                self.waited[(e, sk)] = val
        self.lastw.clear()
        self.readers.clear()


def host_consts():
    bf = ml_dtypes.bfloat16
    c = {}
    c['c_identb'] = np.eye(128, dtype=np.float32).astype(bf)
    c['c_identf'] = np.eye(128, dtype=np.float32)
    j = np.arange(128)[:, None]
    s = np.arange(128)[None, :]
    c['c_uneg'] = np.where(j >= s, -1.0, 0.0).astype(np.float32).astype(bf)
    c['c_oneg'] = np.full((128, 128), -1.0, np.float32).astype(bf)
    negm = np.zeros((128, 4, 512), np.float32)
    for i in range(4):
        key = 128 * i + np.arange(128)[:, None]
        qq = np.arange(512)[None, :]
        negm[:, i, :] = np.where(key >= qq, NEG, 0.0)
    c['c_negm'] = negm.astype(bf)
    ss = np.arange(64)[:, None]
    tt = np.arange(64)[None, :]
    m01 = np.where(ss <= tt, 1.0, 0.0).astype(np.float32)
    c['c_m01'] = np.tile(m01, (1, 4)).astype(bf)
    c['c_ones'] = np.ones((128, 64), np.float32)
    return c


def build(debug=False, upto=99):
    nc = bass.Bass("TRN2", target_bir_lowering=False)
    t = Trk(nc)

    def din(name, shape, dt=F32):
        return nc.dram_tensor(name, list(shape), dt, kind="ExternalInput").ap()

    def dscr(name, shape, dt):
        return nc.dram_tensor(name, list(shape), dt, kind=("ExternalOutput" if debug else "Internal")).ap()

    x = din('x', [S, D]); p_in = din('p', [S, 256])
    g_mix_pre = din('g_mix_pre', [D]); w_in = din('w_in', [D, NCOL])
    b_igate = din('b_igate', [4]); b_fgate = din('b_fgate', [4])
    w_conv = din('w_conv', [4, 1024]); b_conv = din('b_conv', [1024])
    g_mlstm_head = din('g_mlstm_head', [512])
    w_branch_sb = din('w_branch_sb', [512, D]); w_branch_ml = din('w_branch_ml', [512, D])
    w_out = din('w_out', [D, D]); g_mix_post = din('g_mix_post', [D]); g_mlp_pre = din('g_mlp_pre', [D])
    w_mlp_up = din('w_mlp_up', [D, 4096]); w_mlp_down = din('w_mlp_down', [4096, D])
    g_mlp_post = din('g_mlp_post', [D]); g_ple_pre = din('g_ple_pre', [D])
    w_ple_gate = din('w_ple_gate', [D, D]); w_ple_proj = din('w_ple_proj', [256, D]); g_ple_post = din('g_ple_post', [D])
    c_identb = din('c_identb', [128, 128], BF16); c_identf = din('c_identf', [128, 128])
    c_uneg = din('c_uneg', [128, 128], BF16); c_oneg = din('c_oneg', [128, 128], BF16)
    c_negm = din('c_negm', [128, 4, 512], BF16); c_m01 = din('c_m01', [64, 256], BF16)
    c_ones = din('c_ones', [128, 64])
    y = nc.dram_tensor('y', [S, D], F32, kind="ExternalOutput").ap()

    qsbT = dscr('qsbT', [512, S], BF16); ksbT = dscr('ksbT', [512, S], BF16); vsb = dscr('vsb', [S, 512], BF16)
    qmlT = dscr('qmlT', [512, S], BF16); kmlT = dscr('kmlT', [512, S], BF16); kml = dscr('kml', [S, 512], BF16)
    vml = dscr('vml', [S, 512], BF16); osig = dscr('osig', [S, 512], BF16); gsig = dscr('gsig', [S, 2048], BF16)
    gifT = dscr('gifT', [8, S], F32)
    gs1 = dscr('gs1', [2, 256], F32); gs2 = dscr('gs2', [256], F32); gs3 = dscr('gs3', [256], F32)
    ysbT = dscr('ysbT', [512, S], BF16); ymlT = dscr('ymlT', [512, S], BF16)
    x1 = dscr('x1', [S, D], F32); x2 = dscr('x2', [S, D], F32)
    Wsb_b = dscr('Wsb_b', [512, D], BF16); Wml_b = dscr('Wml_b', [512, D], BF16); Wo_b = dscr('Wo_b', [D, D], BF16)
    Wup_b = dscr('Wup_b', [D, 4096], BF16); Wdn_b = dscr('Wdn_b', [4096, D], BF16)
    Wg_b = dscr('Wg_b', [D, D], BF16); Wp_b = dscr('Wp_b', [256, D], BF16)
    h2T_d = dscr('h2T_d', [D, S], BF16)

    PS = [nc.alloc_psum_tensor('ps%d' % i, [128, 512], F32) for i in range(8)]
    PSb = [h.bitcast(BF16) for h in PS]
    identb = nc.alloc_sbuf_tensor('identb', [128, 128], BF16)
    identf = nc.alloc_sbuf_tensor('identf', [128, 128], F32)
    t.dma('sp', identb[:, :], c_identb, writes=['identb'])
    t.dma('sp', identf[:, :], c_identf, writes=['identf'])
    t.barrier()

    NC = dict(allow_slow_non_contiguous=True)

    def rms_rstd(src_ap, rd, junk, ssc, key):
        t.op('act', lambda e: e.activation(junk[:, :], src_ap, AF.Square, accum_out=ssc), reads=rd, writes=['junk', key])
        t.op('act', lambda e: e.activation(ssc, ssc, AF.Sqrt, scale=1.0 / 1024, bias=EPS), reads=[key], writes=[key])
        t.op('dve', lambda e: e.reciprocal(ssc, ssc), reads=[key], writes=[key])

    def transpose8(src, srckey, n, dst_ap, dstkey, bank, eng):
        pb = PSb[bank]
        srckeys = srckey if isinstance(srckey, list) else [srckey]
        for kc in range(n):
            t.op('pe', lambda e, kc=kc: e.transpose(pb[:, kc * 128:(kc + 1) * 128], src[:, kc * 128:(kc + 1) * 128], identb[:, :]),
                 reads=srckeys, writes=['ps%d' % bank])
        src_v = pb[:, 0:n * 128].rearrange('p (k t) -> p k t', k=n)
        if eng == 'act':
            t.op('act', lambda e: e.activation(dst_ap, src_v, AF.Copy), reads=['ps%d' % bank], writes=[dstkey])
        else:
            t.op(eng, lambda e: e.tensor_copy(dst_ap, src_v), reads=['ps%d' % bank], writes=[dstkey])

    def cast_weight(es0, Wdst, wsrc, nk, ncols, gvec, piece, name):
        stg = [es0.enter_context(nc.sbuf_tensor('%s_stg%d' % (name, i), [128, piece], F32)) for i in range(3)]
        n = 0
        for kc in range(nk):
            for c0 in range(0, ncols, piece):
                w = min(piece, ncols - c0)
                sl = n % 3
                t.dma('sp', stg[sl][:, 0:w], wsrc[kc * 128:(kc + 1) * 128, c0:c0 + w], writes=[(name, 'stg', sl)])
                dst = Wdst[:, kc, c0:c0 + w]
                if n % 2 == 0:
                    if gvec is None:
                        t.op('dve', lambda e, dst=dst, sl=sl, w=w: e.tensor_copy(dst, stg[sl][:, 0:w]),
                             reads=[(name, 'stg', sl)], writes=[(name, kc, c0)])
                    else:
                        t.op('dve', lambda e, dst=dst, sl=sl, w=w, kc=kc: e.tensor_scalar(dst, stg[sl][:, 0:w], gvec[:, kc:kc + 1], None, ALU.mult),
                             reads=[(name, 'stg', sl)], writes=[(name, kc, c0)])
                else:
                    if gvec is None:
                        t.op('act', lambda e, dst=dst, sl=sl, w=w: e.activation(dst, stg[sl][:, 0:w], AF.Copy),
                             reads=[(name, 'stg', sl)], writes=[(name, kc, c0)])
                    else:
                        t.op('act', lambda e, dst=dst, sl=sl, w=w, kc=kc: e.activation(dst, stg[sl][:, 0:w], AF.Copy, scale=gvec[:, kc:kc + 1]),
                             reads=[(name, 'stg', sl)], writes=[(name, kc, c0)])
                n += 1

    def load_bf16_weight(Wdst, wsrc_b, nk, name):
        for kc in range(nk):
            t.dma('sp', Wdst[:, kc, :], wsrc_b[kc * 128:(kc + 1) * 128, :], writes=[(name, kc)])

    def phase1():
        with ExitStack() as es:
            def sbt(name, shape, dt):
                return es.enter_context(nc.sbuf_tensor(name, shape, dt))
            Wb = sbt('Wb', [128, 8, NCOL], BF16)
            gpre = sbt('gpre', [128, 8], F32)
            wcv = sbt('wcv', [128, 8, 4], F32)
            bcv = sbt('bcv', [128, 8], F32)
            ghb = sbt('ghb', [128, 512], F32)
            t.dma('sp', gpre[:, :], g_mix_pre.rearrange('(k p) -> p k', p=128), writes=['gpre'], **NC)
            for tap in range(4):
                t.dma('sp', wcv[:, :, tap], w_conv[tap, :].rearrange('(j p) -> p j', p=128), writes=[('wcv', tap)], **NC)
            t.dma('sp', bcv[:, :], b_conv.rearrange('(j p) -> p j', p=128), writes=['bcv'], **NC)
            t.dma('sp', ghb[:, :], g_mlstm_head.partition_broadcast(128), writes=['ghb'])
            t.barrier()
            with ExitStack() as es0:
                cast_weight(es0, Wb, w_in, 8, NCOL, gpre, 1880, 'win')
                t.barrier()
            t.barrier()

            xs = [sbt('xs%d' % i, [128, 1024], F32) for i in range(8)]
            hb = [sbt('hb%d' % i, [128, 1024], BF16) for i in range(4)]
            hT = [sbt('hT%d' % i, [128, 8, 512], BF16) for i in range(2)]
            junk = sbt('junk', [128, 1024], BF16)
            ssq = sbt('ssq', [128, 8], F32)
            cb = sbt('cb', [128, 8, 515], F32)
            acc = [sbt('acc%d' % i, [128, 512], F32) for i in range(2)]
            ost = [sbt('ost%d' % i, [128, 512], BF16) for i in range(6)]
            osf = [sbt('osf%d' % i, [128, 512], F32) for i in range(2)]
            gst = [sbt('gst%d' % i, [8, 512], F32) for i in range(2)]
            kst = [sbt('kst%d' % i, [128, 4, 128], BF16) for i in range(2)]
            kbf = [sbt('kbf%d' % i, [128, 512], BF16) for i in range(4)]
            t.op('pool', lambda e: e.memset(cb[:, :, 0:3], 0.0), writes=[('cb', j) for j in range(8)])

            gv_up = sbt('gv_up', [128, 8], F32); gv_g = sbt('gv_g', [128, 8], F32)
            t.dma('sp', gv_up[:, :], g_mlp_pre.rearrange('(k p) -> p k', p=128), writes=['gv_up'], **NC)
            t.dma('sp', gv_g[:, :], g_ple_pre.rearrange('(k p) -> p k', p=128), writes=['gv_g'], **NC)
            wst = [sbt('wst%d' % i, [128, 1024], F32) for i in range(2)]
            wsb16 = [sbt('wsb16_%d' % i, [128, 1024], BF16) for i in range(2)]
            pieces = []
            for (src, dst, nk, ncols, gv) in ((w_branch_sb, Wsb_b, 4, D, None), (w_branch_ml, Wml_b, 4, D, None), (w_out, Wo_b, 8, D, None),
                                              (w_mlp_up, Wup_b, 8, 4096, gv_up), (w_mlp_down, Wdn_b, 32, D, None),
                                              (w_ple_gate, Wg_b, 8, D, gv_g), (w_ple_proj, Wp_b, 2, D, None)):
                for kc in range(nk):
                    for c0 in range(0, ncols, 1024):
                        pieces.append((src, dst, kc, c0, gv))

            def piece_load(pi):
                src, dst, kc, c0, gv = pieces[pi]
                sl = pi % 2
                t.dma('sp', wst[sl][:, :], src[kc * 128:(kc + 1) * 128, c0:c0 + 1024], writes=[('wst', sl)])

            def piece_cast(pi):
                src, dst, kc, c0, gv = pieces[pi]
                sl = pi % 2
                if gv is None:
                    t.op('act', lambda e: e.activation(wsb16[sl][:, :], wst[sl][:, :], AF.Copy), reads=[('wst', sl)], writes=[('wsb16', sl)])
                else:
                    t.op('act', lambda e: e.activation(wsb16[sl][:, :], wst[sl][:, :], AF.Copy, scale=gv[:, kc:kc + 1]),
                         reads=[('wst', sl), 'gv_up', 'gv_g'], writes=[('wsb16', sl)])
                t.dma('act', dst[kc * 128:(kc + 1) * 128, c0:c0 + 1024], wsb16[sl][:, :], reads=[('wsb16', sl)], writes=[('wcast', pi)])

            st = {'bank': 0, 'ost': 0, 'n': 0, 'slot': 0}

            def precast_tick():
                k_ = st['slot']
                st['slot'] += 1
                pi = k_ // 4
                if k_ % 4 == 0 and pi < len(pieces):
                    piece_load(pi)
                elif k_ % 4 == 2 and 0 <= pi - 1 < len(pieces):
                    piece_cast(pi - 1)

            def nbank():
                b = st['bank']
                st['bank'] = (b + 1) % 6
                precast_tick()
                return b

            def nost():
                o = st['ost']
                st['ost'] = (o + 1) % 6
                return o

            def load_x(tt):
                sl = tt % 8
                t.dma('sp', xs[sl][:, :], x[tt * 128:(tt + 1) * 128, :], writes=[('xs', sl)])

            def norm_C(tt):
                sl, hs, g, col, ti = tt % 8, tt % 4, tt // 4, tt % 8, tt % 4
                rms_rstd(xs[sl][:, :], [('xs', sl)], junk, ssq[:, col:col + 1], ('ssq', col))
                t.op('dve', lambda e: e.tensor_scalar(hb[hs][:, :], xs[sl][:, :], ssq[:, col:col + 1], None, ALU.mult),
                     reads=[('xs', sl), ('ssq', col)], writes=[('hb', hs)])

            def norm_P(tt):
                sl, hs, g, col, ti = tt % 8, tt % 4, tt // 4, tt % 8, tt % 4
                transpose8(hb[hs], ('hb', hs), 8, hT[g % 2][:, :, ti * 128:(ti + 1) * 128], ('hT', g % 2, ti), 6 + (tt % 2),
                           'act' if tt % 2 == 0 else 'dve')

            def norm_T(tt):
                norm_C(tt)
                norm_P(tt)

            def evac_copy(dst, src, rd, wr, scale=None):
                n = st['n']
                st['n'] += 1
                if n % 2 == 0:
                    if scale is None:
                        t.op('act', lambda e: e.activation(dst, src, AF.Copy), reads=rd, writes=wr)
                    else:
                        t.op('act', lambda e: e.activation(dst, src, AF.Copy, scale=scale), reads=rd, writes=wr)
                else:
                    if scale is None:
                        t.op('dve', lambda e: e.tensor_copy(dst, src), reads=rd, writes=wr)
                    else:
                        t.op('dve', lambda e: e.tensor_scalar(dst, src, scale, None, ALU.mult), reads=rd, writes=wr)

            def proj_group(g):
                hTg = hT[g % 2]
                rdh = [('hT', g % 2, i) for i in range(4)]
                tok0 = g * 512
                fm = [('qsb', cc, cc * 128) for cc in range(4)] + [('ksb', cc, 512 + cc * 128) for cc in range(4)] + \
                     [('qml', cc, 1536 + cc * 128) for cc in range(4)] + [('kml', cc, 2048 + cc * 128) for cc in range(4)]
                for kind, cc, col0 in fm:
                    b = nbank()
                    bk = 'ps%d' % b
                    for kc in range(8):
                        t.op('pe', lambda e, kc=kc, b=b, col0=col0: e.matmul(PS[b][:, :], lhsT=Wb[:, kc, col0:col0 + 128], rhs=hTg[:, kc, :],
                                                                       start=(kc == 0), stop=(kc == 7)), reads=rdh, writes=[bk])
                    if kind in ('qsb', 'ksb'):
                        o = nost()
                        evac_copy(ost[o][:, :], PS[b][:, :], [bk], [('ost', o)], scale=(SB_SCALE if kind == 'qsb' else None))
                        dst = (qsbT if kind == 'qsb' else ksbT)[cc * 128:(cc + 1) * 128, tok0:tok0 + 512]
                        t.dma('sp', dst, ost[o][:, :], reads=[('ost', o)], writes=[(kind, cc, g)])
                    else:
                        j = cc if kind == 'qml' else 4 + cc
                        ck = ('cb', j)
                        evac_copy(cb[:, j, 3:515], PS[b][:, :], [bk], [ck])
                        a = acc[j % 2]
                        ak = ('acc', j % 2)
                        t.op('dve', lambda e, j=j, a=a: e.tensor_scalar(a[:, :], cb[:, j, 3:515], wcv[:, j, 3:4], None, ALU.mult), reads=[ck], writes=[ak])
                        for tap in (2, 1, 0):
                            t.op('dve', lambda e, j=j, a=a, tap=tap: e.scalar_tensor_tensor(a[:, :], cb[:, j, tap:tap + 512], wcv[:, j, tap:tap + 1], a[:, :],
                                                                                     ALU.mult, ALU.add), reads=[ck, ak], writes=[ak])
                        t.op('pool', lambda e, j=j: e.tensor_copy(cb[:, j, 0:3], cb[:, j, 512:515]), reads=[ck], writes=[ck])
                        if kind == 'qml':
                            o = nost()
                            t.op('act', lambda e, j=j, a=a, o=o: e.activation(ost[o][:, :], a[:, :], AF.Silu, bias=bcv[:, j:j + 1]), reads=[ak], writes=[('ost', o)])
                            t.dma('sp', qmlT[cc * 128:(cc + 1) * 128, tok0:tok0 + 512], ost[o][:, :], reads=[('ost', o)], writes=[('qmlT', cc, g)])
                        else:
                            kb_ = kbf[cc]
                            t.op('act', lambda e, j=j, a=a, kb_=kb_: e.activation(kb_[:, :], a[:, :], AF.Silu, bias=bcv[:, j:j + 1]), reads=[ak], writes=[('kbf', cc)])
                            t.dma('sp', kmlT[cc * 128:(cc + 1) * 128, tok0:tok0 + 512], kb_[:, :], reads=[('kbf', cc)], writes=[('kmlT', cc, g)])

                def k_transposes(cc):
                    tb = 6 + (cc % 2)
                    pb = PSb[tb]
                    for i in range(4):
                        t.op('pe', lambda e, i=i, pb=pb: e.transpose(pb[:, i * 128:(i + 1) * 128], kbf[cc][:, i * 128:(i + 1) * 128], identb[:, :]),
                             reads=[('kbf', cc)], writes=['ps%d' % tb])
                    ks = kst[cc % 2]
                    t.op('dve', lambda e, ks=ks, pb=pb: e.tensor_copy(ks[:, :, :], pb[:, 0:512].rearrange('p (i d) -> p i d', i=4)),
                         reads=['ps%d' % tb], writes=[('kst', cc % 2)])
                    t.dma('sp', kml[tok0:tok0 + 512, cc * 128:(cc + 1) * 128].rearrange('(i p) d -> p i d', p=128), ks[:, :, :],
                          reads=[('kst', cc % 2)], writes=[('kml', cc, g)])

                b = nbank()
                bk = 'ps%d' % b
                for kc in range(8):
                    t.op('pe', lambda e, kc=kc, b=b: e.matmul(PS[b][0:8, :], lhsT=Wb[:, kc, 3584:3592], rhs=hTg[:, kc, :], start=(kc == 0), stop=(kc == 7)),
                         reads=rdh, writes=[bk])
                gs = gst[g % 2]
                t.op('dve', lambda e, b=b, gs=gs: e.tensor_copy(gs[:, :], PS[b][0:8, :]), reads=[bk], writes=[('gst', g % 2)])
                t.dma('sp', gifT[:, tok0:tok0 + 512], gs[:, :], reads=[('gst', g % 2)], writes=[('gifT', g)])
                tmj = [('vsb', 1024, vsb, 0), ('vml', 2560, vml, 0), ('oml', 3072, osig, 0),
                       ('gate', 3592, gsig, 0), ('gate', 4104, gsig, 512), ('gate', 4616, gsig, 1024), ('gate', 5128, gsig, 1536)]
                for ti in range(4):
                    r0 = tok0 + ti * 128
                    if ti == 1:
                        for cc in range(4):
                            k_transposes(cc)
                    if ti == 2 and g + 1 < NG:
                        for tj in range(4):
                            norm_C((g + 1) * 4 + tj)
                    if ti == 3 and g + 1 < NG:
                        for tj in range(4):
                            norm_P((g + 1) * 4 + tj)
                    for kind, col0, dstT, dcol in tmj:
                        b = nbank()
                        bk = 'ps%d' % b
                        for kc in range(8):
                            t.op('pe', lambda e, kc=kc, b=b, col0=col0, ti=ti: e.matmul(PS[b][:, :], lhsT=hTg[:, kc, ti * 128:(ti + 1) * 128],
                                                                                 rhs=Wb[:, kc, col0:col0 + 512], start=(kc == 0), stop=(kc == 7)),
                                 reads=[('hT', g % 2, ti)], writes=[bk])
                        o = nost()
                        if kind in ('vsb', 'vml'):
                            evac_copy(ost[o][:, :], PS[b][:, :], [bk], [('ost', o)])
                        elif kind == 'gate':
                            t.op('act', lambda e, b=b, o=o: e.activation(ost[o][:, :], PS[b][:, :], AF.Sigmoid), reads=[bk], writes=[('ost', o)])
                        else:
                            f = osf[ti % 2]
                            t.op('act', lambda e, b=b, f=f: e.activation(f[:, :], PS[b][:, :], AF.Sigmoid), reads=[bk], writes=[('osf', ti % 2)])
                            t.op('dve', lambda e, f=f, o=o: e.tensor_tensor(ost[o][:, :], f[:, :], ghb[:, :], ALU.mult), reads=[('osf', ti % 2)], writes=[('ost', o)])
                        t.dma('sp', dstT[r0:r0 + 128, dcol:dcol + 512], ost[o][:, :], reads=[('ost', o)], writes=[(kind, col0, g, ti)])

            for tt in range(8):
                load_x(tt)
            for ti in range(4):
                norm_T(ti)
            for g in range(NG):
                proj_group(g)
                if g + 2 < NG:
                    for ti in range(4):
                        load_x((g + 2) * 4 + ti)
            assert st['slot'] >= 4 * len(pieces)
            piece_cast(len(pieces) - 1)
            t.barrier()

    def phase23():
        with ExitStack() as es:
            def sbt(name, shape, dt):
                return es.enter_context(nc.sbuf_tensor(name, shape, dt))
            KT = sbt('KT', [128, 4, S], BF16)
            V = sbt('V', [128, 32, 512], BF16)
            Q = [sbt('Q%d' % i, [128, 4, 512], BF16) for i in range(2)]
            E = [sbt('E%d' % i, [128, 512], F32) for i in range(3)]
            SPt = [sbt('SP%d' % i, [128, 512], BF16) for i in range(3)]
            A = [sbt('A%d' % i, [128, 512], BF16) for i in range(3)]
            R = [sbt('R%d' % i, [128, 512], BF16) for i in range(2)]
            uneg = sbt('uneg', [128, 128], BF16)
            oneg = sbt('oneg', [128, 128], BF16)
            negm = sbt('negm', [128, 4, 512], BF16)
            ostg = [sbt('ostg%d' % i, [64, 512], BF16) for i in range(2)]
            t.dma('sp', uneg[:, :], c_uneg, writes=['uneg'])
            t.dma('sp', oneg[:, :], c_oneg, writes=['oneg'])
            t.dma('sp', negm[:, :, :], c_negm, writes=['negm'])
            for j in range(4):
                t.dma('sp', KT[:, j, :], ksbT[j * 128:(j + 1) * 128, :], writes=[('KT', j)])
            for j in range(4):
                t.dma('sp', V[:, j * 8:(j + 1) * 8, :], vsb[j * 1024:(j + 1) * 1024, :].rearrange('(kb p) c -> p kb c', p=128), writes=[('V', j)])
            GI = sbt('GI', [128, 2, 64], F32); GF = sbt('GF', [128, 2, 64], F32)
            bi_t = sbt('bi_t', [128, 2], F32); bf_t = sbt('bf_t', [128, 2], F32); nbf = sbt('nbf', [128, 2], F32)
            ones = sbt('ones', [128, 64], F32)
            ef = sbt('ef', [128, 2, 64], F32); cs = sbt('cs', [128, 2, 64], F32); aa = sbt('aa', [128, 2, 64], F32)
            amax = sbt('amax', [128, 2], F32); bend = sbt('bend', [128, 2], F32); abe = sbt('abe', [128, 2], F32)
            b4 = sbt('b4', [4, 64], F32); a4 = sbt('a4', [4, 64], F32); mn4 = sbt('mn4', [4, 64], F32); mp4 = sbt('mp4', [4, 64], F32)
            mp = sbt('mp', [128, 2], F32); nM = sbt('nM', [128, 2], F32); thb = sbt('thb', [128, 2], F32); dec = sbt('dec', [128, 2], F32)
            wq = sbt('wq', [128, 2, 64], F32); th = sbt('th', [128, 2, 64], F32)
            wT = sbt('wT', [64, 256], F32); thT = sbt('thT', [64, 256], F32); decb = sbt('decb', [128, 256], F32)
            m01 = sbt('m01', [64, 256], BF16)
            t.dma('sp', ones[:, :], c_ones, writes=['ones'])
            t.dma('sp', m01[:, :], c_m01, writes=['m01'])
            for h in range(4):
                r0, q = (h % 2) * 64, h // 2
                t.dma('sp', GI[r0:r0 + 64, q, :], gifT[h, :].rearrange('(c l) -> c l', l=64), writes=[('GI', h)])
                t.dma('sp', GF[r0:r0 + 64, q, :], gifT[4 + h, :].rearrange('(c l) -> c l', l=64), writes=[('GF', h)])
                t.dma('sp', bi_t[r0:r0 + 64, q:q + 1], b_igate[h:h + 1].partition_broadcast(64), writes=[('bi', h)])
                t.dma('sp', bf_t[r0:r0 + 64, q:q + 1], b_fgate[h:h + 1].partition_broadcast(64), writes=[('bf', h)])
            t.barrier()
            G = ['gp']
            t.op('dve', lambda e: e.tensor_scalar(nbf[:, :], bf_t[:, :], -1.0, None, ALU.mult), reads=G, writes=G)
            for q in range(2):
                t.op('act', lambda e, q=q: e.activation(ef[:, q, :], GF[:, q, :], AF.Exp, bias=nbf[:, q:q + 1], scale=-1.0), reads=G, writes=G)
            t.op('act', lambda e: e.activation(ef[:, :, :], ef[:, :, :], AF.Ln, bias=1.0), reads=G, writes=G)
            for q in range(2):
                t.op('dve', lambda e, q=q: e.tensor_tensor_scan(cs[:, q, :], ones[:, :], ef[:, q, :], 0.0, ALU.mult, ALU.add), reads=G, writes=G)
                t.op('dve', lambda e, q=q: e.scalar_tensor_tensor(aa[:, q, :], GI[:, q, :], bi_t[:, q:q + 1], cs[:, q, :], ALU.add, ALU.add), reads=G, writes=G)
            t.op('dve', lambda e: e.tensor_reduce(amax[:, :], aa[:, :, :], AX.X, ALU.max), reads=G, writes=G)
            t.op('dve', lambda e: e.tensor_scalar(bend[:, :], cs[:, :, 63], -1.0, None, ALU.mult), reads=G, writes=G)
            t.op('dve', lambda e: e.tensor_tensor(abe[:, :], amax[:, :], bend[:, :], ALU.add), reads=G, writes=G)
            t.dma('sp', gs1[0, :].rearrange('(q p) -> p q', p=128), bend[:, :], reads=G, writes=['gs1a'], **NC)
            t.dma('sp', gs1[1, :].rearrange('(q p) -> p q', p=128), abe[:, :], reads=G, writes=['gs1b'], **NC)
            t.dma('sp', b4[:, :], gs1[0, :].rearrange('(h c) -> h c', c=64), reads=['gs1a'], writes=['b4'])
            t.dma('sp', a4[:, :], gs1[1, :].rearrange('(h c) -> h c', c=64), reads=['gs1b'], writes=['a4'])
            t.op('dve', lambda e: e.tensor_tensor_scan(mn4[:, :], b4[:, :], a4[:, :], 0.0, ALU.add, ALU.max), reads=['b4', 'a4'], writes=['mn4'])
            t.op('dve', lambda e: e.memset(mp4[:, 0:1], 0.0), reads=[], writes=['mp4'])
            t.op('dve', lambda e: e.tensor_copy(mp4[:, 1:64], mn4[:, 0:63]), reads=['mn4', 'mp4'], writes=['mp4'])
            t.dma('sp', gs2.rearrange('(h c) -> h c', c=64), mp4[:, :], reads=['mp4'], writes=['gs2'])
            t.dma('sp', mp[:, :], gs2.rearrange('(q p) -> p q', p=128), reads=['gs2'], writes=G, **NC)
            t.op('dve', lambda e: e.tensor_tensor(nM[:, :], mp[:, :], amax[:, :], ALU.max), reads=G, writes=G)
            t.op('dve', lambda e: e.tensor_scalar(nM[:, :], nM[:, :], -1.0, None, ALU.mult), reads=G, writes=G)
            t.op('dve', lambda e: e.tensor_scalar(thb[:, :], nM[:, :], -LN_C, None, ALU.add), reads=G, writes=G)
            t.op('dve', lambda e: e.tensor_tensor(dec[:, :], mp[:, :], nM[:, :], ALU.add), reads=G, writes=G)
            t.op('act', lambda e: e.activation(dec[:, :], dec[:, :], AF.Exp), reads=G, writes=G)
            for q in range(2):
                t.op('act', lambda e, q=q: e.activation(wq[:, q, :], aa[:, q, :], AF.Exp, bias=nM[:, q:q + 1]), reads=G, writes=G)
                t.op('act', lambda e, q=q: e.activation(th[:, q, :], cs[:, q, :], AF.Exp, bias=thb[:, q:q + 1]), reads=G, writes=G)
            t.dma('sp', gs3.rearrange('(q p) -> p q', p=128), dec[:, :], reads=G, writes=['gs3'], **NC)
            t.dma('sp', decb[:, :], gs3.partition_broadcast(128), reads=['gs3'], writes=['decb'])
            for q in range(2):
                t.op('pe', lambda e, q=q: e.transpose(PS[0][0:64, q * 128:(q + 1) * 128], wq[:, q, :], identf[:, :]), reads=G, writes=['ps0'])
                t.op('pe', lambda e, q=q: e.transpose(PS[1][0:64, q * 128:(q + 1) * 128], th[:, q, :], identf[:, :]), reads=G, writes=['ps1'])
            t.op('dve', lambda e: e.tensor_copy(wT[:, :], PS[0][0:64, 0:256]), reads=['ps0'], writes=['wT'])
            t.op('dve', lambda e: e.tensor_copy(thT[:, :], PS[1][0:64, 0:256]), reads=['ps1'], writes=['thT'])
            t.barrier()

            qTs = [sbt('qTs%d' % i, [128, 4, 512], BF16) for i in range(2)]
            kTs = [sbt('kTs%d' % i, [128, 4, 512], BF16) for i in range(2)]
            ktm = [sbt('ktm%d' % i, [64, 8, 512], BF16) for i in range(2)]
            vx = [sbt('vx%d' % i, [64, 8, 4, 129], BF16) for i in range(2)]
            og = [sbt('og%d' % i, [64, 8, 512], BF16) for i in range(2)]
            yst = [sbt('yst%d' % i, [128, 4, 512], BF16) for i in range(2)]
            CT = sbt('CT', [128, 4, 129], F32); CTd = sbt('CTd', [128, 4, 129], F32); CTb = sbt('CTb', [128, 4, 129], BF16)
            SmT = [sbt('SmT%d' % i, [64, 256], BF16) for i in range(2)]
            vw = [sbt('vw%d' % i, [64, 4, 129], BF16) for i in range(2)]
            dd = sbt('dd', [64, 4], F32); rr = sbt('rr', [64, 4], F32); s2 = sbt('s2', [64, 4], F32)
            hh = sbt('hh', [64, 4, 128], F32); sq = sbt('sq', [64, 4, 128], F32); hn = sbt('hn', [64, 4, 128], F32)
            yt = [sbt('yt%d' % i, [64, 512], BF16) for i in range(2)]
            for i in range(2):
                t.op('pool', lambda e, i=i: e.memset(vx[i][:, :, :, :], 1.0), writes=[('vx', i)])
            t.op('dve', lambda e: e.memset(CT[:, :, :], 0.0), writes=['CT'])

            def load_sc(sc):
                sl = sc % 2
                c0 = sc * 512
                t.dma('sp', qTs[sl][:, :, :], qmlT[:, c0:c0 + 512].rearrange('(h p) tk -> p h tk', p=128), writes=[('qTs', sl)])
                t.dma('sp', kTs[sl][:, :, :], kmlT[:, c0:c0 + 512].rearrange('(h p) tk -> p h tk', p=128), writes=[('kTs', sl)])
                t.dma('sp', ktm[sl][:, :, :], kml[c0:c0 + 512, :].rearrange('(c l) d -> l c d', l=64), writes=[('ktm', sl)])
                for ci in range(8):
                    t.dma('sp', vx[sl][:, ci, :, 0:128], vml[c0 + ci * 64:c0 + (ci + 1) * 64, :].rearrange('l (h d) -> l h d', h=4),
                          writes=[('vx', sl)])
                t.dma('sp', og[sl][:, :, :], osig[c0:c0 + 512, :].rearrange('(c l) d -> l c d', l=64), writes=[('og', sl)])

            def XB(j):
                return 4 + j

            def YB(j):
                return 6 + j

            def mstage(c, k):
                sc, ci = c // 8, c % 8
                sl = sc % 2
                tk = slice(ci * 64, (ci + 1) * 64)
                par = c % 2
                if k == 0:
                    if ci == 0 and sc + 1 < 8:
                        load_sc(sc + 1)
                    for h in range(4):
                        j, hl = h // 2, h % 2
                        t.op('pe', lambda e, h=h, j=j, hl=hl: e.matmul(PS[XB(j)][0:64, 258 + hl * 64:258 + (hl + 1) * 64], lhsT=kTs[sl][:, h, tk], rhs=qTs[sl][:, h, tk],
                                                                  start=True, stop=True), reads=[('qTs', sl), ('kTs', sl)], writes=['ps%d' % XB(j)])
                    wbc = wT[:, c:256:64].unsqueeze(2).broadcast_to([64, 4, 129])
                    t.op('pool', lambda e: e.tensor_tensor(vw[par][:, :, :], vx[sl][:, ci, :, :], wbc, ALU.mult), reads=[('vx', sl)], writes=[('vw', par)])
                    dbc = decb[:, c:256:64].unsqueeze(2).broadcast_to([128, 4, 129])
                    t.op('pool', lambda e: e.tensor_tensor(CTd[:, :, :], CT[:, :, :], dbc, ALU.mult), reads=['CT'], writes=['CTd'])
                    t.op('pool', lambda e: e.tensor_copy(CTb[:, :, :], CTd[:, :, :]), reads=['CTd'], writes=['CTb'])
                elif k == 1:
                    for j in range(2):
                        t.op('dve', lambda e, j=j: e.tensor_tensor(SmT[par][:, j * 128:(j + 1) * 128], PS[XB(j)][0:64, 258:386], m01[:, 0:128], ALU.mult),
                             reads=['ps%d' % XB(j)], writes=[('SmT', par, j)])
                elif k == 2:
                    for h in range(4):
                        j, off = h // 2, (h % 2) * 129
                        t.op('pe', lambda e, h=h, j=j, off=off: e.matmul(PS[XB(j)][0:64, off:off + 129], lhsT=SmT[par][:, h * 64:(h + 1) * 64], rhs=vw[par][:, h, :],
                                                                    start=True, stop=False), reads=[('SmT', par, j), ('vw', par)], writes=['ps%d' % XB(j)])
                        t.op('pe', lambda e, h=h, j=j, off=off: e.matmul(PS[XB(j)][0:64, off:off + 129], lhsT=qTs[sl][:, h, tk], rhs=CTb[:, h, :],
                                                                    start=False, stop=True), reads=[('qTs', sl), 'CTb'], writes=['ps%d' % XB(j)])
                    for h in range(4):
                        j, off = h // 2, (h % 2) * 129
                        t.op('pe', lambda e, h=h, j=j, off=off: e.matmul(PS[YB(j)][:, off:off + 129], lhsT=ktm[sl][:, ci, h * 128:(h + 1) * 128], rhs=vw[par][:, h, :],
                                                                    start=True, stop=True), reads=[('ktm', sl), ('vw', par)], writes=['ps%d' % YB(j)])
                elif k == 3:
                    for j in range(2):
                        t.op('dve', lambda e, j=j: e.tensor_tensor(CT[:, 2 * j:2 * j + 2, :], CTd[:, 2 * j:2 * j + 2, :],
                                                                   PS[YB(j)][:, 0:258].rearrange('p (a b) -> p a b', a=2), ALU.add),
                             reads=['CTd', 'ps%d' % YB(j)], writes=['CT'])
                    for j in range(2):
                        den = PS[XB(j)][0:64, 128:258:129]
                        t.op('dve', lambda e, j=j, den=den: e.tensor_tensor(dd[:, 2 * j:2 * j + 2], den, thT[:, 2 * j * 64 + c:2 * j * 64 + c + 65:64], ALU.max),
                             reads=['ps%d' % XB(j)], writes=['dd'])
                        t.op('dve', lambda e, j=j, den=den: e.scalar_tensor_tensor(dd[:, 2 * j:2 * j + 2], den, -1.0, dd[:, 2 * j:2 * j + 2], ALU.mult, ALU.max),
                             reads=['ps%d' % XB(j), 'dd'], writes=['dd'])
                    t.op('dve', lambda e: e.reciprocal(rr[:, :], dd[:, :]), reads=['dd'], writes=['rr'])
                    for j in range(2):
                        t.op('dve', lambda e, j=j: e.tensor_tensor(hh[:, 2 * j:2 * j + 2, :], PS[XB(j)][0:64, 0:258].rearrange('p (a b) -> p a b', a=2)[:, :, 0:128],
                                                                   rr[:, 2 * j:2 * j + 2].unsqueeze(2).broadcast_to([64, 2, 128]), ALU.mult),
                             reads=['rr', 'ps%d' % XB(j)], writes=['hh'])
                    t.op('pool', lambda e: e.tensor_tensor(sq[:, :, :], hh[:, :, :], hh[:, :, :], ALU.mult), reads=['hh'], writes=['sq'])
                elif k == 4:
                    t.op('dve', lambda e: e.tensor_reduce(s2[:, :], sq[:, :, :], AX.X, ALU.add), reads=['sq'], writes=['s2'])
                    t.op('act', lambda e: e.activation(s2[:, :], s2[:, :], AF.Ln, scale=1.0 / 128, bias=EPS), reads=['s2'], writes=['s2'])
                    t.op('act', lambda e: e.activation(s2[:, :], s2[:, :], AF.Exp, scale=-0.5), reads=['s2'], writes=['s2'])
                    t.op('dve', lambda e: e.tensor_tensor(hn[:, :, :], hh[:, :, :], s2[:, :].unsqueeze(2).broadcast_to([64, 4, 128]), ALU.mult),
                         reads=['hh', 's2'], writes=['hn'])
                    t.op('pool', lambda e: e.tensor_tensor(yt[par][:, :], hn[:, :, :].rearrange('p a b -> p (a b)'), og[sl][:, ci, :], ALU.mult),
                         reads=['hn', ('og', sl)], writes=[('yt', par)])
                elif k == 5:
                    for h in range(4):
                        j, hl = h // 2, h % 2
                        t.op('pe', lambda e, h=h, j=j, hl=hl: e.transpose(PSb[YB(j)][:, 516 + hl * 64:516 + (hl + 1) * 64], yt[par][:, h * 128:(h + 1) * 128], identb[0:64, 0:64]),
                             reads=[('yt', par)], writes=['ps%d' % YB(j)])
                    for j in range(2):
                        t.op('dve', lambda e, j=j: e.tensor_copy(yst[sl][:, 2 * j:2 * j + 2, tk], PSb[YB(j)][:, 516:644].rearrange('p (h tk) -> p h tk', h=2)),
                             reads=['ps%d' % YB(j)], writes=[('yst', sl)])
                    if ci == 7:
                        t.dma('sp', ymlT[:, sc * 512:(sc + 1) * 512].rearrange('(h p) tk -> p h tk', p=128), yst[sl][:, :, :],
                              reads=[('yst', sl)], writes=[('ymlT', sc)])

            def load_q(g):
                t.dma('sp', Q[g % 2][:, :, :], qsbT[:, g * 512:(g + 1) * 512].rearrange('(j p) tk -> p j tk', p=128), writes=[('Q', g % 2)])

            units = []
            m = 0
            for g in range(NG):
                for h in range(8):
                    kbs = list(range(4 * g + 3, -1, -1))
                    for n_, kb in enumerate(kbs):
                        units.append(dict(g=g, h=h, kb=kb, first=(n_ == 0), last=(n_ == len(kbs) - 1), m=m, newg=(h == 0 and n_ == 0)))
                    m += 1
            for i_, u in enumerate(units):
                u['i'] = i_

            def c0_of(u):
                return max(0, (u['kb'] - 4 * u['g']) * 128)

            def S1(u):
                g, h, kb, i = u['g'], u['h'], u['kb'], u['i']
                if u['newg'] and g + 1 < NG:
                    load_q(g + 1)
                pb = i % 3
                j, r0 = h // 2, (h % 2) * 64
                diag = kb >= 4 * g
                c0 = c0_of(u)
                t.op('pe', lambda e: e.matmul(PS[pb][:, c0:512], lhsT=KT[r0:r0 + 64, j, kb * 128:(kb + 1) * 128], rhs=Q[g % 2][r0:r0 + 64, j, c0:512],
                                              start=True, stop=(not diag)), reads=[('Q', g % 2)], writes=['ps%d' % pb])
                if diag:
                    di = kb - 4 * g
                    t.op('pe', lambda e: e.matmul(PS[pb][:, c0:c0 + 128], lhsT=identb[:, :], rhs=negm[:, di, c0:c0 + 128], start=False, stop=True,
                                                  skip_group_check=True), reads=[], writes=['ps%d' % pb])

            def S2a(u):
                i = u['i']
                pb, sl = i % 3, i % 3
                c0 = c0_of(u)
                t.op('act', lambda e: e.activation(E[sl][:, c0:512], PS[pb][:, c0:512], AF.Exp), reads=['ps%d' % pb], writes=[('E', sl)])

            def S2b(u):
                i = u['i']
                pb, sl = i % 3, i % 3
                c0 = c0_of(u)
                t.op('act', lambda e: e.activation(SPt[sl][:, c0:512], E[sl][:, c0:512], AF.Ln, bias=1.0), reads=[('E', sl)], writes=[('SP', sl)])

            def S3(u):
                i, m_ = u['i'], u['m']
                pb, sl, rs = i % 3, i % 3, m_ % 2
                c0 = c0_of(u)
                t.op('pe', lambda e: e.matmul(PS[pb][:, c0:512], lhsT=uneg[:, :], rhs=SPt[sl][:, c0:512], start=False, stop=u['first'], skip_group_check=True),
                     reads=[('SP', sl)], writes=['ps%d' % pb])
                if not u['first']:
                    t.op('pe', lambda e: e.matmul(PS[pb][:, c0:512], lhsT=oneg[:, :], rhs=R[rs][:, c0:512], start=False, stop=True, skip_group_check=True),
                         reads=[('R', rs)], writes=['ps%d' % pb])
                if not u['last']:
                    if u['first']:
                        if c0 > 0:
                            t.op('dve', lambda e: e.memset(R[rs][:, 0:c0], 0.0), reads=[], writes=[('R', rs)])
                        t.op('dve', lambda e: e.tensor_copy(R[rs][:, c0:512], SPt[sl][:, c0:512]), reads=[('SP', sl), ('R', rs)], writes=[('R', rs)])
                    else:
                        t.op('dve', lambda e: e.tensor_tensor(R[rs][:, c0:512], R[rs][:, c0:512], SPt[sl][:, c0:512], ALU.add),
                             reads=[('SP', sl), ('R', rs)], writes=[('R', rs)])

            def S4(u):
                i = u['i']
                pb, sl = i % 3, i % 3
                c0 = c0_of(u)
                t.op('act', lambda e: e.activation(A[sl][:, c0:512], PS[pb][:, c0:512], AF.Exp), reads=['ps%d' % pb], writes=[('A', sl)])

            def S5(u):
                g, h, kb, i, m_ = u['g'], u['h'], u['kb'], u['i'], u['m']
                sl = i % 3
                o0 = (m_ % 2) * 64
                ok = ('ps3', m_ % 2)
                c0 = c0_of(u)
                t.op('pe', lambda e: e.matmul(PS[3][o0:o0 + 64, c0:512], lhsT=V[:, kb, h * 64:(h + 1) * 64], rhs=A[sl][:, c0:512], start=u['first'], stop=u['last'],
                                              skip_group_check=True), reads=[('A', sl)], writes=[ok])
                if u['last']:
                    os_ = ostg[m_ % 2]
                    t.op('dve', lambda e: e.tensor_copy(os_[:, :], PS[3][o0:o0 + 64, :]), reads=[ok], writes=[('ostg', m_ % 2)])
                    t.dma('sp', ysbT[h * 64:(h + 1) * 64, g * 512:(g + 1) * 512], os_[:, :], reads=[('ostg', m_ % 2)], writes=[('ysbT', m_)])

            load_q(0)
            load_sc(0)
            n = len(units)
            GAP = 3
            for i in range(n + 2):
                if i < n:
                    S1(units[i])
                    S2a(units[i])
                    S2b(units[i])
                if 0 <= i - 1 < n:
                    S3(units[i - 1])
                    S4(units[i - 1])
                if 0 <= i - 2 < n:
                    S5(units[i - 2])
                if i % GAP == 0:
                    sidx = i // GAP
                    c, k = sidx // 6, sidx % 6
                    if c < 64:
                        mstage(c, k)
            t.barrier()

    def phase4a():
        with ExitStack() as es:
            def sbt(name, shape, dt):
                return es.enter_context(nc.sbuf_tensor(name, shape, dt))
            Wsb = sbt('Wsb', [128, 4, D], BF16); Wml = sbt('Wml', [128, 4, D], BF16); Wo = sbt('Wo', [128, 8, D], BF16)
            gbc = sbt('gbc', [128, D], F32)
            t.dma('sp', gbc[:, :], g_mix_post.partition_broadcast(128), writes=['gbc'])
            load_bf16_weight(Wsb, Wsb_b, 4, 'wsb')
            load_bf16_weight(Wml, Wml_b, 4, 'wml')
            load_bf16_weight(Wo, Wo_b, 8, 'wo')
            t.barrier()
            ysT = [sbt('ysT%d' % i, [128, 4, 512], BF16) for i in range(2)]
            ymT = [sbt('ymT%d' % i, [128, 4, 512], BF16) for i in range(2)]
            gsx = [sbt('gsx%d' % i, [128, 2048], BF16) for i in range(3)]
            xs = [sbt('xa%d' % i, [128, D], F32) for i in range(3)]
            t1 = sbt('t1', [128, D], F32); t2 = sbt('t2', [128, D], F32)
            mg = [sbt('mg%d' % i, [128, D], BF16) for i in range(2)]
            mT = [sbt('mT%d' % i, [128, 8, 128], BF16) for i in range(2)]
            junk = sbt('junk4', [128, 512], BF16)
            ssq = sbt('ssq4', [128, 4], F32)
            tmp = sbt('tmp4', [128, D], F32)
            xo = [sbt('xo%d' % i, [128, D], F32) for i in range(2)]
            junk2 = sbt('junk4b', [128, D], BF16)
            h2 = [sbt('h2_%d' % i, [128, D], BF16) for i in range(2)]
            h2T = [sbt('h2T_%d' % i, [128, 8, 128], BF16) for i in range(2)]

            def tileB2a(tt):
                sl = tt % 2
                rms_rstd(xo[sl][:, :], [('xo', sl, 0), ('xo', sl, 1)], junk2, ssq[:, 3:4], ('ssq4', 3))
                t.op('dve', lambda e: e.tensor_scalar(h2[sl][:, :], xo[sl][:, :], ssq[:, 3:4], None, ALU.mult),
                     reads=[('xo', sl, 0), ('xo', sl, 1), ('ssq4', 3)], writes=[('h2', sl)])

            def tileB2b(tt):
                sl = tt % 2
                transpose8(h2[sl], ('h2', sl), 8, h2T[sl][:, :, :], ('h2T', sl), 6 + sl, 'dve')
                t.dma('sp', h2T_d[:, tt * 128:(tt + 1) * 128].rearrange('(k p) tk -> p k tk', p=128), h2T[sl][:, :, :],
                      reads=[('h2T', sl)], writes=[('h2T_d', tt)])

            def load_g(g):
                sl = g % 2
                t.dma('sp', ysT[sl][:, :, :], ysbT[:, g * 512:(g + 1) * 512].rearrange('(j p) tk -> p j tk', p=128), writes=[('ysT', sl)])
                t.dma('sp', ymT[sl][:, :, :], ymlT[:, g * 512:(g + 1) * 512].rearrange('(j p) tk -> p j tk', p=128), writes=[('ymT', sl)])

            def load_t(tt):
                sl = tt % 3
                t.dma('sp', gsx[sl][:, :], gsig[tt * 128:(tt + 1) * 128, :], writes=[('gsx', sl)])
                t.dma('sp', xs[sl][:, :], x[tt * 128:(tt + 1) * 128, :], writes=[('xa', sl)])

            def tileA1(tt):
                g, ti, sl = tt // 4, tt % 4, tt % 2
                gl = g % 2
                tks = slice(ti * 128, (ti + 1) * 128)
                for half in range(2):
                    cs_ = slice(half * 512, (half + 1) * 512)
                    for (Ysrc, Wsrc, bk, yk) in ((ysT, Wsb, half, 'ysT'), (ymT, Wml, 2 + half, 'ymT')):
                        for j in range(4):
                            t.op('pe', lambda e, Ysrc=Ysrc, Wsrc=Wsrc, bk=bk, j=j, cs_=cs_: e.matmul(PS[bk][:, :], lhsT=Ysrc[gl][:, j, tks], rhs=Wsrc[:, j, cs_],
                                                                                                start=(j == 0), stop=(j == 3)),
                                 reads=[(yk, gl)], writes=['ps%d' % bk])
                for half in range(2):
                    cs_ = slice(half * 512, (half + 1) * 512)
                    t.op('dve', lambda e, half=half, cs_=cs_: e.tensor_tensor(t1[:, cs_], PS[half][:, :], gsx[tt % 3][:, cs_], ALU.mult),
                         reads=['ps%d' % half, ('gsx', tt % 3)], writes=[('t1', half)])
                    t.op('dve', lambda e, half=half, cs_=cs_: e.tensor_tensor(t2[:, cs_], PS[2 + half][:, :], gsx[tt % 3][:, 1024 + half * 512:1024 + (half + 1) * 512], ALU.mult),
                         reads=['ps%d' % (2 + half), ('gsx', tt % 3)], writes=[('t2', half)])
                    t.op('dve', lambda e, cs_=cs_: e.tensor_tensor(mg[sl][:, cs_], t1[:, cs_], t2[:, cs_], ALU.add),
                         reads=[('t1', half), ('t2', half)], writes=[('mg', sl, half)])

            def tileA2(tt):
                sl = tt % 2
                transpose8(mg[sl], [('mg', sl, 0), ('mg', sl, 1)], 8, mT[sl][:, :, :], ('mT', sl), 6 + sl, 'act')

            def tileB(tt):
                sl = tt % 2
                for half in range(2):
                    cs_ = slice(half * 512, (half + 1) * 512)
                    bk = 4 + half
                    for kc in range(8):
                        t.op('pe', lambda e, kc=kc, bk=bk, cs_=cs_: e.matmul(PS[bk][:, :], lhsT=mT[sl][:, kc, :], rhs=Wo[:, kc, cs_], start=(kc == 0), stop=(kc == 7)),
                             reads=[('mT', sl)], writes=['ps%d' % bk])
                for half in range(2):
                    t.op('act', lambda e, half=half: e.activation(junk[:, :], PS[4 + half][:, :], AF.Square, accum_out=ssq[:, half:half + 1]),
                         reads=['ps%d' % (4 + half)], writes=['junk4', ('ssq4', half)])
                t.op('dve', lambda e: e.tensor_tensor(ssq[:, 2:3], ssq[:, 0:1], ssq[:, 1:2], ALU.add), reads=[('ssq4', 0), ('ssq4', 1)], writes=[('ssq4', 2)])
                t.op('act', lambda e: e.activation(ssq[:, 2:3], ssq[:, 2:3], AF.Sqrt, scale=1.0 / 1024, bias=EPS), reads=[('ssq4', 2)], writes=[('ssq4', 2)])
                t.op('dve', lambda e: e.reciprocal(ssq[:, 2:3], ssq[:, 2:3]), reads=[('ssq4', 2)], writes=[('ssq4', 2)])
                for half in range(2):
                    cs_ = slice(half * 512, (half + 1) * 512)
                    t.op('dve', lambda e, half=half, cs_=cs_: e.tensor_tensor(tmp[:, cs_], PS[4 + half][:, :], gbc[:, cs_], ALU.mult),
                         reads=['ps%d' % (4 + half)], writes=[('tmp4', half)])
                    t.op('dve', lambda e, cs_=cs_: e.scalar_tensor_tensor(xo[sl][:, cs_], tmp[:, cs_], ssq[:, 2:3], xs[tt % 3][:, cs_], ALU.mult, ALU.add),
                         reads=[('tmp4', half), ('ssq4', 2), ('xa', tt % 3)], writes=[('xo', sl, half)])
                t.dma('sp', x1[tt * 128:(tt + 1) * 128, :], xo[sl][:, :], reads=[('xo', sl, 0), ('xo', sl, 1)], writes=[('x1', tt)])

            load_g(0)
            load_t(0)
            load_t(1)
            load_t(2)
            tileA1(0)
            tileA2(0)
            for tt in range(NT):
                if (tt + 1) % 4 == 0 and (tt + 1) // 4 + 1 < NG:
                    load_g((tt + 1) // 4 + 1)
                if tt == 0:
                    load_g(1)
                if tt + 1 < NT:
                    tileA1(tt + 1)
                if tt >= 1:
                    tileB2b(tt - 1)
                tileB(tt)
                tileB2a(tt)
                if tt + 3 < NT:
                    load_t(tt + 3)
                if tt + 1 < NT:
                    tileA2(tt + 1)
            tileB2b(NT - 1)
            t.barrier()

    def phase4b():
        with ExitStack() as es:
            def sbt(name, shape, dt):
                return es.enter_context(nc.sbuf_tensor(name, shape, dt))
            Wup = sbt('Wup', [128, 8, 4096], BF16); Wdn = sbt('Wdn', [128, 32, D], BF16)
            gbc = sbt('gbc5', [128, D], F32)
            t.dma('sp', gbc[:, :], g_mlp_post.partition_broadcast(128), writes=['gbc'])
            hT = [sbt('hT5_%d' % i, [128, 8, 512], BF16) for i in range(2)]
            xr = [sbt('xr%d' % i, [128, D], F32) for i in range(3)]

            def load_h(g):
                t.dma('sp', hT[g % 2][:, :, :], h2T_d[:, g * 512:(g + 1) * 512].rearrange('(k p) tk -> p k tk', p=128), writes=[('hT5', g % 2)])

            def load_xr(tt):
                t.dma('sp', xr[tt % 3][:, :], x1[tt * 128:(tt + 1) * 128, :], writes=[('xr', tt % 3)])

            load_h(0)
            load_bf16_weight(Wup, Wup_b, 8, 'wup')
            load_h(1)
            load_bf16_weight(Wdn, Wdn_b, 32, 'wdn')
            for tt in range(3):
                load_xr(tt)
            UT = sbt('UT', [128, 32, 512], BF16)
            rl = [sbt('rl%d' % i, [128, 512], F32) for i in range(2)]
            junk = sbt('junk5', [128, 512], BF16)
            ssq = sbt('ssq5', [128, 8], F32)
            tmp = sbt('tmp5', [128, 512], F32)
            xo = [sbt('xo5_%d' % i, [128, 512], F32) for i in range(2)]
            rdw_up = [('wup', kc) for kc in range(8)]
            rdw_dn = [('wdn', f) for f in range(32)]

            def group(g):
                hTg = hT[g % 2]
                for f in range(32):
                    bk = f % 4
                    for kc in range(8):
                        t.op('pe', lambda e, kc=kc, bk=bk, f=f: e.matmul(PS[bk][:, :], lhsT=Wup[:, kc, f * 128:(f + 1) * 128], rhs=hTg[:, kc, :], start=(kc == 0), stop=(kc == 7)),
                             reads=[('hT5', g % 2)] + (rdw_up if g == 0 else []), writes=['ps%d' % bk])
                    r = rl[f % 2]
                    t.op('act', lambda e, bk=bk, r=r: e.activation(r[:, :], PS[bk][:, :], AF.Relu), reads=['ps%d' % bk], writes=[('rl', f % 2)])
                    t.op('pool', lambda e, f=f, r=r: e.tensor_tensor(UT[:, f, :], r[:, :], r[:, :], ALU.mult), reads=[('rl', f % 2)], writes=[('UT', f)])
                rdu = [('UT', f) for f in range(32)]
                for ti in range(4):
                    tt = g * 4 + ti
                    xs_ = xr[tt % 3]
                    b0 = 4 + 2 * (ti % 2)
                    for half in range(2):
                        bk = b0 + half
                        cs_ = slice(half * 512, (half + 1) * 512)
                        for f in range(32):
                            t.op('pe', lambda e, f=f, bk=bk, cs_=cs_, ti=ti: e.matmul(PS[bk][:, :], lhsT=UT[:, f, ti * 128:(ti + 1) * 128], rhs=Wdn[:, f, cs_],
                                                                                  start=(f == 0), stop=(f == 31)),
                                 reads=rdu + (rdw_dn if (g == 0 and ti == 0) else []), writes=['ps%d' % bk])
                    c0 = 2 * (ti % 2)
                    for half in range(2):
                        t.op('act', lambda e, half=half: e.activation(junk[:, :], PS[b0 + half][:, :], AF.Square, accum_out=ssq[:, c0 + half:c0 + half + 1]),
                             reads=['ps%d' % (b0 + half)], writes=['junk', ('ssq5', c0 + half)])
                    sc_ = ssq[:, 4 + ti % 2:5 + ti % 2]
                    sk_ = ('ssq5', 4 + ti % 2)
                    t.op('dve', lambda e: e.tensor_tensor(sc_, ssq[:, c0:c0 + 1], ssq[:, c0 + 1:c0 + 2], ALU.add), reads=[('ssq5', c0), ('ssq5', c0 + 1)], writes=[sk_])
                    t.op('act', lambda e: e.activation(sc_, sc_, AF.Sqrt, scale=1.0 / 1024, bias=EPS), reads=[sk_], writes=[sk_])
                    t.op('dve', lambda e: e.reciprocal(sc_, sc_), reads=[sk_], writes=[sk_])
                    for half in range(2):
                        cs_ = slice(half * 512, (half + 1) * 512)
                        t.op('dve', lambda e, half=half, cs_=cs_: e.tensor_tensor(tmp[:, :], PS[b0 + half][:, :], gbc[:, cs_], ALU.mult),
                             reads=['ps%d' % (b0 + half), 'gbc'], writes=['tmp5'])
                        t.op('dve', lambda e, half=half, cs_=cs_: e.scalar_tensor_tensor(xo[half][:, :], tmp[:, :], sc_, xs_[:, cs_], ALU.mult, ALU.add),
                             reads=['tmp5', sk_, ('xr', tt % 3)], writes=[('xo5', half)])
                        t.dma('sp', x2[tt * 128:(tt + 1) * 128, cs_], xo[half][:, :], reads=[('xo5', half)], writes=[('x2', tt, half)])
                    if tt + 3 < NT:
                        load_xr(tt + 3)

            for g in range(NG):
                group(g)
                if g + 2 < NG:
                    load_h(g + 2)
            t.barrier()

    def phase4c():
        with ExitStack() as es:
            def sbt(name, shape, dt):
                return es.enter_context(nc.sbuf_tensor(name, shape, dt))
            Wg = sbt('Wg', [128, 8, D], BF16); Wp = sbt('Wp', [128, 2, D], BF16)
            gbc = sbt('gbc6', [128, D], F32)
            t.dma('sp', gbc[:, :], g_ple_post.partition_broadcast(128), writes=['gbc'])
            load_bf16_weight(Wg, Wg_b, 8, 'wg')
            load_bf16_weight(Wp, Wp_b, 2, 'wp')
            t.barrier()
            xs = [sbt('xc%d' % i, [128, D], F32) for i in range(3)]
            pt = [sbt('pt%d' % i, [128, 256], F32) for i in range(3)]
            pbf = [sbt('pbf%d' % i, [128, 256], BF16) for i in range(2)]
            hb = [sbt('hb6_%d' % i, [128, D], BF16) for i in range(2)]
            hT = [sbt('hT6_%d' % i, [128, 8, 128], BF16) for i in range(2)]
            pT = [sbt('pT6_%d' % i, [128, 2, 128], BF16) for i in range(2)]
            sg = sbt('sg6', [128, D], F32); ee = sbt('ee6', [128, D], F32)
            junk = sbt('junk6', [128, D], BF16)
            ssq = sbt('ssq6', [128, 4], F32)
            tmp = sbt('tmp6', [128, D], F32)
            xo = [sbt('xo6_%d' % i, [128, D], F32) for i in range(2)]

            def load_t(tt):
                sl = tt % 3
                t.dma('sp', xs[tt % 3][:, :], x2[tt * 128:(tt + 1) * 128, :], writes=[('xc', tt % 3)])
                t.dma('sp', pt[tt % 3][:, :], p_in[tt * 128:(tt + 1) * 128, :], writes=[('pt', tt % 3)])

            def tileC12(tt):
                sl = tt % 2
                rms_rstd(xs[tt % 3][:, :], [('xc', tt % 3)], junk, ssq[:, 0:1], ('ssq6', 0))
                t.op('dve', lambda e: e.tensor_scalar(hb[sl][:, :], xs[tt % 3][:, :], ssq[:, 0:1], None, ALU.mult), reads=[('xc', tt % 3), ('ssq6', 0)], writes=[('hb6', sl)])
                t.op('pool', lambda e: e.tensor_copy(pbf[sl][:, :], pt[tt % 3][:, :]), reads=[('pt', tt % 3)], writes=[('pbf', sl)])
                transpose8(hb[sl], ('hb6', sl), 8, hT[sl][:, :, :], ('hT6', sl), 6, 'act')
                transpose8(pbf[sl], ('pbf', sl), 2, pT[sl][:, :, :], ('pT6', sl), 7, 'dve')

            def tileC3(tt):
                sl = tt % 2
                for half in range(2):
                    cs_ = slice(half * 512, (half + 1) * 512)
                    for kc in range(8):
                        t.op('pe', lambda e, kc=kc, half=half, cs_=cs_: e.matmul(PS[half][:, :], lhsT=hT[sl][:, kc, :], rhs=Wg[:, kc, cs_], start=(kc == 0), stop=(kc == 7)),
                             reads=[('hT6', sl)], writes=['ps%d' % half])
                    for j in range(2):
                        t.op('pe', lambda e, j=j, half=half, cs_=cs_: e.matmul(PS[2 + half][:, :], lhsT=pT[sl][:, j, :], rhs=Wp[:, j, cs_], start=(j == 0), stop=(j == 1)),
                             reads=[('pT6', sl)], writes=['ps%d' % (2 + half)])

            def tileC4(tt):
                sl = tt % 2
                for half in range(2):
                    cs_ = slice(half * 512, (half + 1) * 512)
                    t.op('act', lambda e, half=half, cs_=cs_: e.activation(sg[:, cs_], PS[half][:, :], AF.Sigmoid), reads=['ps%d' % half], writes=[('sg6', half)])
                    t.op('dve', lambda e, half=half, cs_=cs_: e.tensor_tensor(ee[:, cs_], PS[2 + half][:, :], sg[:, cs_], ALU.mult),
                         reads=['ps%d' % (2 + half), ('sg6', half)], writes=[('ee6', half)])
                rms_rstd(ee[:, :], [('ee6', 0), ('ee6', 1)], junk, ssq[:, 1:2], ('ssq6', 1))
                t.op('dve', lambda e: e.tensor_tensor(tmp[:, :], ee[:, :], gbc[:, :], ALU.mult), reads=[('ee6', 0), ('ee6', 1)], writes=['tmp6'])
                t.op('dve', lambda e: e.scalar_tensor_tensor(xo[sl][:, :], tmp[:, :], ssq[:, 1:2], xs[tt % 3][:, :], ALU.mult, ALU.add),
                     reads=['tmp6', ('ssq6', 1), ('xc', tt % 3)], writes=[('xo6', sl)])
                t.dma('sp', y[tt * 128:(tt + 1) * 128, :], xo[sl][:, :], reads=[('xo6', sl)], writes=[('y', tt)])

            load_t(0)
            load_t(1)
            load_t(2)
            tileC12(0)
            for tt in range(NT):
                tileC3(tt)
                if tt + 1 < NT:
                    tileC12(tt + 1)
                tileC4(tt)
                if tt + 3 < NT:
                    load_t(tt + 3)
            t.barrier()

    if upto >= 1:
        phase1()
    if upto >= 3:
        phase23()
    if upto >= 4:
        phase4a()
    if upto >= 5:
        phase4b()
    if upto >= 6:
        phase4c()
    t.barrier()
    return nc, t


def make_in_maps(inputs):
    c = host_consts()
    maps = []
    sq = {k: np.ascontiguousarray(np.asarray(v, dtype=np.float32)[0]) for k, v in inputs.items() if k not in ('x',)}
    xx = np.asarray(inputs['x'], dtype=np.float32)
    for b in range(8):
        m = dict(c)
        m['x'] = np.ascontiguousarray(xx[b])
        m['p'] = np.ascontiguousarray(sq['p'][b])
        for k, v in sq.items():
            if k != 'p':
                m[k] = v
        maps.append(m)
    return maps


def kernel(**inputs):
    nc, _ = build(debug=False)
    maps = make_in_maps(inputs)
    res = run_bass_kernel_spmd(nc, maps, core_ids=list(range(8)))
    out = np.stack([np.asarray(r['y'], dtype=np.float32) for r in res.results], axis=0)
    return out
```

```python
import os
import math
from contextlib import ExitStack
import numpy as np
import ml_dtypes
import concourse.bass as bass
import concourse.mybir as mybir
from concourse.bass_utils import run_bass_kernel_spmd

F32 = mybir.dt.float32
BF16 = mybir.dt.bfloat16
AF = mybir.ActivationFunctionType
ALU = mybir.AluOpType
AX = mybir.AxisListType

S = 4096
D = 1024
NCOL = 5640
NG = 8
NT = 32
EPS = 1e-6
SB_SCALE = 1.0 / 8.0
LN_C = -0.5 * math.log(128.0)
NEG = -30000.0


class Trk:
    SAME_ENGINE_SYNC = True
    NDMA = 8

    def __init__(self, nc):
        self.nc = nc
        self.eng = {'pe': nc.tensor, 'act': nc.scalar, 'dve': nc.vector, 'pool': nc.gpsimd, 'sp': nc.sync}
        self.sem, self.cnt = {}, {}
        for e in ('pe', 'act', 'dve', 'pool'):
            self.sem[e] = nc.alloc_semaphore('c_' + e)
            self.cnt[e] = 0
        self.dsem, self.dcnt = {}, {}
        for q in ('sp', 'act', 'pool'):
            self.dsem[q] = [nc.alloc_semaphore('d_%s%d' % (q, i)) for i in range(self.NDMA)]
            self.dcnt[q] = 0
        self.waited, self.lastw, self.readers, self.semobj = {}, {}, {}, {}
        for e, s in self.sem.items():
            self.semobj[('c', e)] = s
        for q, l in self.dsem.items():
            for i, s in enumerate(l):
                self.semobj[('d', q, i)] = s
        self.nops = 0

    def _wait(self, e, tok):
        if tok is None:
            return
        sk, val = tok
        if sk[0] == 'c' and sk[1] == e:
            if e == 'pe' or not self.SAME_ENGINE_SYNC:
                return
        if self.waited.get((e, sk), 0) >= val:
            return
        self.eng[e].wait_ge(self.semobj[sk], val)
        self.waited[(e, sk)] = val

    def _deps(self, e, reads, writes):
        need = {}

        def add(tok):
            if tok is not None and need.get(tok[0], 0) < tok[1]:
                need[tok[0]] = tok[1]
        for b in reads:
            add(self.lastw.get(b))
        for b in writes:
            add(self.lastw.get(b))
            for r in self.readers.get(b, ()):
                add(r)
        for sk, val in need.items():
            self._wait(e, (sk, val))

    def _commit(self, tok, reads, writes):
        for b in reads:
            lst = self.readers.setdefault(b, [])
            for k_, r in enumerate(lst):
                if r[0] == tok[0]:
                    lst[k_] = tok
                    break
            else:
                lst.append(tok)
        for b in writes:
            self.lastw[b] = tok
            self.readers[b] = []

    def op(self, e, fn, reads=(), writes=()):
        self._deps(e, reads, writes)
        ins = fn(self.eng[e])
        self.cnt[e] += 1
        ins.then_inc(self.sem[e], 1)
        tok = (('c', e), self.cnt[e])
        self._commit(tok, reads, writes)
        self.nops += 1
        return tok

    def dma(self, q, out, in_, reads=(), writes=(), **kw):
        i = self.dcnt[q]
        slot, rnd = i % self.NDMA, i // self.NDMA
        sk = ('d', q, slot)
        if rnd > 0:
            self._wait(q, (sk, 16 * rnd))
        self._deps(q, reads, writes)
        self.eng[q].dma_start(out=out, in_=in_, **kw).then_inc(self.semobj[sk], 16)
        self.dcnt[q] += 1
        tok = (sk, 16 * (rnd + 1))
        self._commit(tok, reads, writes)
        self.nops += 1
        return tok

    def barrier(self):
        toks = [(('c', e), self.cnt[e]) for e in self.sem if self.cnt[e] > 0]
        for q in self.dsem:
            n = self.dcnt[q]
            for s in range(self.NDMA):
                k = (n - s + self.NDMA - 1) // self.NDMA
                if k > 0:
                    toks.append((('d', q, s), 16 * k))
        for e in ('pe', 'act', 'dve', 'pool', 'sp'):
            for sk, val in toks:
                if sk[0] == 'c' and sk[1] == e:
                    continue
                if self.waited.get((e, sk), 0) >= val:
                    continue
                self.eng[e].wait_ge(self.semobj[sk], val)
                self.waited[(e, sk)] = val
        self.lastw.clear()
        self.readers.clear()


def host_consts():
    bf = ml_dtypes.bfloat16
    c = {}
    c['c_identb'] = np.eye(128, dtype=np.float32).astype(bf)
    c['c_identf'] = np.eye(128, dtype=np.float32)
    j = np.arange(128)[:, None]
    s = np.arange(128)[None, :]
    c['c_uneg'] = np.where(j >= s, -1.0, 0.0).astype(np.float32).astype(bf)
    c['c_oneg'] = np.full((128, 128), -1.0, np.float32).astype(bf)
    negm = np.zeros((128, 4, 512), np.float32)
    for i in range(4):
        key = 128 * i + np.arange(128)[:, None]
        qq = np.arange(512)[None, :]
        negm[:, i, :] = np.where(key >= qq, NEG, 0.0)
    c['c_negm'] = negm.astype(bf)
    ss = np.arange(64)[:, None]
    tt = np.arange(64)[None, :]
    m01 = np.where(ss <= tt, 1.0, 0.0).astype(np.float32)
    c['c_m01'] = np.tile(m01, (1, 4)).astype(bf)
    c['c_ones'] = np.ones((128, 64), np.float32)
    return c


def build(debug=False, upto=99):
    nc = bass.Bass("TRN2", target_bir_lowering=False)
    t = Trk(nc)

    def din(name, shape, dt=F32):
        return nc.dram_tensor(name, list(shape), dt, kind="ExternalInput").ap()

    def dscr(name, shape, dt):
        return nc.dram_tensor(name, list(shape), dt, kind=("ExternalOutput" if debug else "Internal")).ap()

    x = din('x', [S, D]); p_in = din('p', [S, 256])
    g_mix_pre = din('g_mix_pre', [D]); w_in = din('w_in', [D, NCOL])
    b_igate = din('b_igate', [4]); b_fgate = din('b_fgate', [4])
    w_conv = din('w_conv', [4, 1024]); b_conv = din('b_conv', [1024])
    g_mlstm_head = din('g_mlstm_head', [512])
    w_branch_sb = din('w_branch_sb', [512, D]); w_branch_ml = din('w_branch_ml', [512, D])
    w_out = din('w_out', [D, D]); g_mix_post = din('g_mix_post', [D]); g_mlp_pre = din('g_mlp_pre', [D])
    w_mlp_up = din('w_mlp_up', [D, 4096]); w_mlp_down = din('w_mlp_down', [4096, D])
    g_mlp_post = din('g_mlp_post', [D]); g_ple_pre = din('g_ple_pre', [D])
    w_ple_gate = din('w_ple_gate', [D, D]); w_ple_proj = din('w_ple_proj', [256, D]); g_ple_post = din('g_ple_post', [D])
    c_identb = din('c_identb', [128, 128], BF16); c_identf = din('c_identf', [128, 128])
    c_uneg = din('c_uneg', [128, 128], BF16); c_oneg = din('c_oneg', [128, 128], BF16)
    c_negm = din('c_negm', [128, 4, 512], BF16); c_m01 = din('c_m01', [64, 256], BF16)
    c_ones = din('c_ones', [128, 64])
    y = nc.dram_tensor('y', [S, D], F32, kind="ExternalOutput").ap()

    qsbT = dscr('qsbT', [512, S], BF16); ksbT = dscr('ksbT', [512, S], BF16); vsb = dscr('vsb', [S, 512], BF16)
    qmlT = dscr('qmlT', [512, S], BF16); kmlT = dscr('kmlT', [512, S], BF16); kml = dscr('kml', [S, 512], BF16)
    vml = dscr('vml', [S, 512], BF16); osig = dscr('osig', [S, 512], BF16); gsig = dscr('gsig', [S, 2048], BF16)
    gifT = dscr('gifT', [8, S], F32)
    gs1 = dscr('gs1', [2, 256], F32); gs2 = dscr('gs2', [256], F32); gs3 = dscr('gs3', [256], F32)
    ysbT = dscr('ysbT', [512, S], BF16); ymlT = dscr('ymlT', [512, S], BF16)
    x1 = dscr('x1', [S, D], F32); x2 = dscr('x2', [S, D], F32)
    Wsb_b = dscr('Wsb_b', [512, D], BF16); Wml_b = dscr('Wml_b', [512, D], BF16); Wo_b = dscr('Wo_b', [D, D], BF16)
    Wup_b = dscr('Wup_b', [D, 4096], BF16); Wdn_b = dscr('Wdn_b', [4096, D], BF16)
    Wg_b = dscr('Wg_b', [D, D], BF16); Wp_b = dscr('Wp_b', [256, D], BF16)
    h2T_d = dscr('h2T_d', [D, S], BF16)

    PS = [nc.alloc_psum_tensor('ps%d' % i, [128, 512], F32) for i in range(8)]
    PSb = [h.bitcast(BF16) for h in PS]
    identb = nc.alloc_sbuf_tensor('identb', [128, 128], BF16)
    identf = nc.alloc_sbuf_tensor('identf', [128, 128], F32)
    t.dma('sp', identb[:, :], c_identb, writes=['identb'])
    t.dma('sp', identf[:, :], c_identf, writes=['identf'])
    t.barrier()

    NC = dict(allow_slow_non_contiguous=True)

    def rms_rstd(src_ap, rd, junk, ssc, key):
        t.op('act', lambda e: e.activation(junk[:, :], src_ap, AF.Square, accum_out=ssc), reads=rd, writes=['junk', key])
        t.op('act', lambda e: e.activation(ssc, ssc, AF.Sqrt, scale=1.0 / 1024, bias=EPS), reads=[key], writes=[key])
        t.op('dve', lambda e: e.reciprocal(ssc, ssc), reads=[key], writes=[key])

    def transpose8(src, srckey, n, dst_ap, dstkey, bank, eng):
        pb = PSb[bank]
        srckeys = srckey if isinstance(srckey, list) else [srckey]
        for kc in range(n):
            t.op('pe', lambda e, kc=kc: e.transpose(pb[:, kc * 128:(kc + 1) * 128], src[:, kc * 128:(kc + 1) * 128], identb[:, :]),
                 reads=srckeys, writes=['ps%d' % bank])
        src_v = pb[:, 0:n * 128].rearrange('p (k t) -> p k t', k=n)
        if eng == 'act':
            t.op('act', lambda e: e.activation(dst_ap, src_v, AF.Copy), reads=['ps%d' % bank], writes=[dstkey])
        else:
            t.op(eng, lambda e: e.tensor_copy(dst_ap, src_v), reads=['ps%d' % bank], writes=[dstkey])

    def cast_weight(es0, Wdst, wsrc, nk, ncols, gvec, piece, name):
        stg = [es0.enter_context(nc.sbuf_tensor('%s_stg%d' % (name, i), [128, piece], F32)) for i in range(3)]
        n = 0
        for kc in range(nk):
            for c0 in range(0, ncols, piece):
                w = min(piece, ncols - c0)
                sl = n % 3
                t.dma('sp', stg[sl][:, 0:w], wsrc[kc * 128:(kc + 1) * 128, c0:c0 + w], writes=[(name, 'stg', sl)])
                dst = Wdst[:, kc, c0:c0 + w]
                if n % 2 == 0:
                    if gvec is None:
                        t.op('dve', lambda e, dst=dst, sl=sl, w=w: e.tensor_copy(dst, stg[sl][:, 0:w]),
                             reads=[(name, 'stg', sl)], writes=[(name, kc, c0)])
                    else:
                        t.op('dve', lambda e, dst=dst, sl=sl, w=w, kc=kc: e.tensor_scalar(dst, stg[sl][:, 0:w], gvec[:, kc:kc + 1], None, ALU.mult),
                             reads=[(name, 'stg', sl)], writes=[(name, kc, c0)])
                else:
                    if gvec is None:
                        t.op('act', lambda e, dst=dst, sl=sl, w=w: e.activation(dst, stg[sl][:, 0:w], AF.Copy),
                             reads=[(name, 'stg', sl)], writes=[(name, kc, c0)])
                    else:
                        t.op('act', lambda e, dst=dst, sl=sl, w=w, kc=kc: e.activation(dst, stg[sl][:, 0:w], AF.Copy, scale=gvec[:, kc:kc + 1]),
                             reads=[(name, 'stg', sl)], writes=[(name, kc, c0)])
                n += 1

    def load_bf16_weight(Wdst, wsrc_b, nk, name):
        for kc in range(nk):
            t.dma('sp', Wdst[:, kc, :], wsrc_b[kc * 128:(kc + 1) * 128, :], writes=[(name, kc)])

    def phase1():
        with ExitStack() as es:
            def sbt(name, shape, dt):
                return es.enter_context(nc.sbuf_tensor(name, shape, dt))
            Wb = sbt('Wb', [128, 8, NCOL], BF16)
            gpre = sbt('gpre', [128, 8], F32)
            wcv = sbt('wcv', [128, 8, 4], F32)
            bcv = sbt('bcv', [128, 8], F32)
            ghb = sbt('ghb', [128, 512], F32)
            t.dma('sp', gpre[:, :], g_mix_pre.rearrange('(k p) -> p k', p=128), writes=['gpre'], **NC)
            for tap in range(4):
                t.dma('sp', wcv[:, :, tap], w_conv[tap, :].rearrange('(j p) -> p j', p=128), writes=[('wcv', tap)], **NC)
            t.dma('sp', bcv[:, :], b_conv.rearrange('(j p) -> p j', p=128), writes=['bcv'], **NC)
            t.dma('sp', ghb[:, :], g_mlstm_head.partition_broadcast(128), writes=['ghb'])
            t.barrier()
            with ExitStack() as es0:
                cast_weight(es0, Wb, w_in, 8, NCOL, gpre, 1880, 'win')
                t.barrier()
            t.barrier()

            xs = [sbt('xs%d' % i, [128, 1024], F32) for i in range(8)]
            hb = [sbt('hb%d' % i, [128, 1024], BF16) for i in range(4)]
            hT = [sbt('hT%d' % i, [128, 8, 512], BF16) for i in range(2)]
            junk = sbt('junk', [128, 1024], BF16)
            ssq = sbt('ssq', [128, 8], F32)
            cb = sbt('cb', [128, 8, 515], F32)
            acc = [sbt('acc%d' % i, [128, 512], F32) for i in range(2)]
            ost = [sbt('ost%d' % i, [128, 512], BF16) for i in range(6)]
            osf = [sbt('osf%d' % i, [128, 512], F32) for i in range(2)]
            gst = [sbt('gst%d' % i, [8, 512], F32) for i in range(2)]
            kst = [sbt('kst%d' % i, [128, 4, 128], BF16) for i in range(2)]
            kbf = [sbt('kbf%d' % i, [128, 512], BF16) for i in range(4)]
            t.op('pool', lambda e: e.memset(cb[:, :, 0:3], 0.0), writes=[('cb', j) for j in range(8)])

            gv_up = sbt('gv_up', [128, 8], F32); gv_g = sbt('gv_g', [128, 8], F32)
            t.dma('sp', gv_up[:, :], g_mlp_pre.rearrange('(k p) -> p k', p=128), writes=['gv_up'], **NC)
            t.dma('sp', gv_g[:, :], g_ple_pre.rearrange('(k p) -> p k', p=128), writes=['gv_g'], **NC)
            wst = [sbt('wst%d' % i, [128, 1024], F32) for i in range(2)]
            wsb16 = [sbt('wsb16_%d' % i, [128, 1024], BF16) for i in range(2)]
            pieces = []
            for (src, dst, nk, ncols, gv) in ((w_branch_sb, Wsb_b, 4, D, None), (w_branch_ml, Wml_b, 4, D, None), (w_out, Wo_b, 8, D, None),
                                              (w_mlp_up, Wup_b, 8, 4096, gv_up), (w_mlp_down, Wdn_b, 32, D, None),
                                              (w_ple_gate, Wg_b, 8, D, gv_g), (w_ple_proj, Wp_b, 2, D, None)):
                for kc in range(nk):
                    for c0 in range(0, ncols, 1024):
                        pieces.append((src, dst, kc, c0, gv))

            def piece_load(pi):
                src, dst, kc, c0, gv = pieces[pi]
                sl = pi % 2
                t.dma('sp', wst[sl][:, :], src[kc * 128:(kc + 1) * 128, c0:c0 + 1024], writes=[('wst', sl)])

            def piece_cast(pi):
                src, dst, kc, c0, gv = pieces[pi]
                sl = pi % 2
                if gv is None:
                    t.op('act', lambda e: e.activation(wsb16[sl][:, :], wst[sl][:, :], AF.Copy), reads=[('wst', sl)], writes=[('wsb16', sl)])
                else:
                    t.op('act', lambda e: e.activation(wsb16[sl][:, :], wst[sl][:, :], AF.Copy, scale=gv[:, kc:kc + 1]),
                         reads=[('wst', sl), 'gv_up', 'gv_g'], writes=[('wsb16', sl)])
                t.dma('act', dst[kc * 128:(kc + 1) * 128, c0:c0 + 1024], wsb16[sl][:, :], reads=[('wsb16', sl)], writes=[('wcast', pi)])

            st = {'bank': 0, 'ost': 0, 'n': 0, 'slot': 0}

            def precast_tick():
                k_ = st['slot']
                st['slot'] += 1
                pi = k_ // 4
                if k_ % 4 == 0 and pi < len(pieces):
                    piece_load(pi)
                elif k_ % 4 == 2 and 0 <= pi - 1 < len(pieces):
                    piece_cast(pi - 1)

            def nbank():
                b = st['bank']
                st['bank'] = (b + 1) % 6
                precast_tick()
                return b

            def nost():
                o = st['ost']
                st['ost'] = (o + 1) % 6
                return o

            def load_x(tt):
                sl = tt % 8
                t.dma('sp', xs[sl][:, :], x[tt * 128:(tt + 1) * 128, :], writes=[('xs', sl)])

            def norm_C(tt):
                sl, hs, g, col, ti = tt % 8, tt % 4, tt // 4, tt % 8, tt % 4
                rms_rstd(xs[sl][:, :], [('xs', sl)], junk, ssq[:, col:col + 1], ('ssq', col))
                t.op('dve', lambda e: e.tensor_scalar(hb[hs][:, :], xs[sl][:, :], ssq[:, col:col + 1], None, ALU.mult),
                     reads=[('xs', sl), ('ssq', col)], writes=[('hb', hs)])

            def norm_P(tt):
                sl, hs, g, col, ti = tt % 8, tt % 4, tt // 4, tt % 8, tt % 4
                transpose8(hb[hs], ('hb', hs), 8, hT[g % 2][:, :, ti * 128:(ti + 1) * 128], ('hT', g % 2, ti), 6 + (tt % 2),
                           'act' if tt % 2 == 0 else 'dve')

            def norm_T(tt):
                norm_C(tt)
                norm_P(tt)

            def evac_copy(dst, src, rd, wr, scale=None):
                n = st['n']
                st['n'] += 1
                if n % 2 == 0:
                    if scale is None:
                        t.op('act', lambda e: e.activation(dst, src, AF.Copy), reads=rd, writes=wr)
                    else:
                        t.op('act', lambda e: e.activation(dst, src, AF.Copy, scale=scale), reads=rd, writes=wr)
                else:
                    if scale is None:
                        t.op('dve', lambda e: e.tensor_copy(dst, src), reads=rd, writes=wr)
                    else:
                        t.op('dve', lambda e: e.tensor_scalar(dst, src, scale, None, ALU.mult), reads=rd, writes=wr)

            def proj_group(g):
                hTg = hT[g % 2]
                rdh = [('hT', g % 2, i) for i in range(4)]
                tok0 = g * 512
                fm = [('qsb', cc, cc * 128) for cc in range(4)] + [('ksb', cc, 512 + cc * 128) for cc in range(4)] + \
                     [('qml', cc, 1536 + cc * 128) for cc in range(4)] + [('kml', cc, 2048 + cc * 128) for cc in range(4)]
                for kind, cc, col0 in fm:
                    b = nbank()
                    bk = 'ps%d' % b
                    for kc in range(8):
                        t.op('pe', lambda e, kc=kc, b=b, col0=col0: e.matmul(PS[b][:, :], lhsT=Wb[:, kc, col0:col0 + 128], rhs=hTg[:, kc, :],
                                                                       start=(kc == 0), stop=(kc == 7)), reads=rdh, writes=[bk])
                    if kind in ('qsb', 'ksb'):
                        o = nost()
                        evac_copy(ost[o][:, :], PS[b][:, :], [bk], [('ost', o)], scale=(SB_SCALE if kind == 'qsb' else None))
                        dst = (qsbT if kind == 'qsb' else ksbT)[cc * 128:(cc + 1) * 128, tok0:tok0 + 512]
                        t.dma('sp', dst, ost[o][:, :], reads=[('ost', o)], writes=[(kind, cc, g)])
                    else:
                        j = cc if kind == 'qml' else 4 + cc
                        ck = ('cb', j)
                        evac_copy(cb[:, j, 3:515], PS[b][:, :], [bk], [ck])
                        a = acc[j % 2]
                        ak = ('acc', j % 2)
                        t.op('dve', lambda e, j=j, a=a: e.tensor_scalar(a[:, :], cb[:, j, 3:515], wcv[:, j, 3:4], None, ALU.mult), reads=[ck], writes=[ak])
                        for tap in (2, 1, 0):
                            t.op('dve', lambda e, j=j, a=a, tap=tap: e.scalar_tensor_tensor(a[:, :], cb[:, j, tap:tap + 512], wcv[:, j, tap:tap + 1], a[:, :],
                                                                                     ALU.mult, ALU.add), reads=[ck, ak], writes=[ak])
                        t.op('pool', lambda e, j=j: e.tensor_copy(cb[:, j, 0:3], cb[:, j, 512:515]), reads=[ck], writes=[ck])
                        if kind == 'qml':
                            o = nost()
                            t.op('act', lambda e, j=j, a=a, o=o: e.activation(ost[o][:, :], a[:, :], AF.Silu, bias=bcv[:, j:j + 1]), reads=[ak], writes=[('ost', o)])
                            t.dma('sp', qmlT[cc * 128:(cc + 1) * 128, tok0:tok0 + 512], ost[o][:, :], reads=[('ost', o)], writes=[('qmlT', cc, g)])
                        else:
                            kb_ = kbf[cc]
                            t.op('act', lambda e, j=j, a=a, kb_=kb_: e.activation(kb_[:, :], a[:, :], AF.Silu, bias=bcv[:, j:j + 1]), reads=[ak], writes=[('kbf', cc)])
                            t.dma('sp', kmlT[cc * 128:(cc + 1) * 128, tok0:tok0 + 512], kb_[:, :], reads=[('kbf', cc)], writes=[('kmlT', cc, g)])

                def k_transposes(cc):
                    tb = 6 + (cc % 2)
                    pb = PSb[tb]
                    for i in range(4):
                        t.op('pe', lambda e, i=i, pb=pb: e.transpose(pb[:, i * 128:(i + 1) * 128], kbf[cc][:, i * 128:(i + 1) * 128], identb[:, :]),
                             reads=[('kbf', cc)], writes=['ps%d' % tb])
                    ks = kst[cc % 2]
                    t.op('dve', lambda e, ks=ks, pb=pb: e.tensor_copy(ks[:, :, :], pb[:, 0:512].rearrange('p (i d) -> p i d', i=4)),
                         reads=['ps%d' % tb], writes=[('kst', cc % 2)])
                    t.dma('sp', kml[tok0:tok0 + 512, cc * 128:(cc + 1) * 128].rearrange('(i p) d -> p i d', p=128), ks[:, :, :],
                          reads=[('kst', cc % 2)], writes=[('kml', cc, g)])

                b = nbank()
                bk = 'ps%d' % b
                for kc in range(8):
                    t.op('pe', lambda e, kc=kc, b=b: e.matmul(PS[b][0:8, :], lhsT=Wb[:, kc, 3584:3592], rhs=hTg[:, kc, :], start=(kc == 0), stop=(kc == 7)),
                         reads=rdh, writes=[bk])
                gs = gst[g % 2]
                t.op('dve', lambda e, b=b, gs=gs: e.tensor_copy(gs[:, :], PS[b][0:8, :]), reads=[bk], writes=[('gst', g % 2)])
                t.dma('sp', gifT[:, tok0:tok0 + 512], gs[:, :], reads=[('gst', g % 2)], writes=[('gifT', g)])
                tmj = [('vsb', 1024, vsb, 0), ('vml', 2560, vml, 0), ('oml', 3072, osig, 0),
                       ('gate', 3592, gsig, 0), ('gate', 4104, gsig, 512), ('gate', 4616, gsig, 1024), ('gate', 5128, gsig, 1536)]
                for ti in range(4):
                    r0 = tok0 + ti * 128
                    if ti == 1:
                        for cc in range(4):
                            k_transposes(cc)
                    if ti == 2 and g + 1 < NG:
                        for tj in range(4):
                            norm_C((g + 1) * 4 + tj)
                    if ti == 3 and g + 1 < NG:
                        for tj in range(4):
                            norm_P((g + 1) * 4 + tj)
                    for kind, col0, dstT, dcol in tmj:
                        b = nbank()
                        bk = 'ps%d' % b
                        for kc in range(8):
                            t.op('pe', lambda e, kc=kc, b=b, col0=col0, ti=ti: e.matmul(PS[b][:, :], lhsT=hTg[:, kc, ti * 128:(ti + 1) * 128],
                                                                                 rhs=Wb[:, kc, col0:col0 + 512], start=(kc == 0), stop=(kc == 7)),
                                 reads=[('hT', g % 2, ti)], writes=[bk])
                        o = nost()
                        if kind in ('vsb', 'vml'):
                            evac_copy(ost[o][:, :], PS[b][:, :], [bk], [('ost', o)])
                        elif kind == 'gate':
                            t.op('act', lambda e, b=b, o=o: e.activation(ost[o][:, :], PS[b][:, :], AF.Sigmoid), reads=[bk], writes=[('ost', o)])
                        else:
                            f = osf[ti % 2]
                            t.op('act', lambda e, b=b, f=f: e.activation(f[:, :], PS[b][:, :], AF.Sigmoid), reads=[bk], writes=[('osf', ti % 2)])
                            t.op('dve', lambda e, f=f, o=o: e.tensor_tensor(ost[o][:, :], f[:, :], ghb[:, :], ALU.mult), reads=[('osf', ti % 2)], writes=[('ost', o)])
                        t.dma('sp', dstT[r0:r0 + 128, dcol:dcol + 512], ost[o][:, :], reads=[('ost', o)], writes=[(kind, col0, g, ti)])

            for tt in range(8):
                load_x(tt)
            for ti in range(4):
                norm_T(ti)
            for g in range(NG):
                proj_group(g)
                if g + 2 < NG:
                    for ti in range(4):
                        load_x((g + 2) * 4 + ti)
            assert st['slot'] >= 4 * len(pieces)
            piece_cast(len(pieces) - 1)
            t.barrier()

    def phase23():
        with ExitStack() as es:
            def sbt(name, shape, dt):
                return es.enter_context(nc.sbuf_tensor(name, shape, dt))
            KT = sbt('KT', [128, 4, S], BF16)
            V = sbt('V', [128, 32, 512], BF16)
            Q = [sbt('Q%d' % i, [128, 4, 512], BF16) for i in range(2)]
            E = [sbt('E%d' % i, [128, 512], F32) for i in range(3)]
            SPt = [sbt('SP%d' % i, [128, 512], BF16) for i in range(3)]
            A = [sbt('A%d' % i, [128, 512], BF16) for i in range(3)]
            R = [sbt('R%d' % i, [128, 512], BF16) for i in range(2)]
            uneg = sbt('uneg', [128, 128], BF16)
            oneg = sbt('oneg', [128, 128], BF16)
            negm = sbt('negm', [128, 4, 512], BF16)
            ostg = [sbt('ostg%d' % i, [64, 512], BF16) for i in range(2)]
            t.dma('sp', uneg[:, :], c_uneg, writes=['uneg'])
            t.dma('sp', oneg[:, :], c_oneg, writes=['oneg'])
            t.dma('sp', negm[:, :, :], c_negm, writes=['negm'])
            for j in range(4):
                t.dma('sp', KT[:, j, :], ksbT[j * 128:(j + 1) * 128, :], writes=[('KT', j)])
            for j in range(4):
                t.dma('sp', V[:, j * 8:(j + 1) * 8, :], vsb[j * 1024:(j + 1) * 1024, :].rearrange('(kb p) c -> p kb c', p=128), writes=[('V', j)])
            GI = sbt('GI', [128, 2, 64], F32); GF = sbt('GF', [128, 2, 64], F32)
            bi_t = sbt('bi_t', [128, 2], F32); bf_t = sbt('bf_t', [128, 2], F32); nbf = sbt('nbf', [128, 2], F32)
            ones = sbt('ones', [128, 64], F32)
            ef = sbt('ef', [128, 2, 64], F32); cs = sbt('cs', [128, 2, 64], F32); aa = sbt('aa', [128, 2, 64], F32)
            amax = sbt('amax', [128, 2], F32); bend = sbt('bend', [128, 2], F32); abe = sbt('abe', [128, 2], F32)
            b4 = sbt('b4', [4, 64], F32); a4 = sbt('a4', [4, 64], F32); mn4 = sbt('mn4', [4, 64], F32); mp4 = sbt('mp4', [4, 64], F32)
            mp = sbt('mp', [128, 2], F32); nM = sbt('nM', [128, 2], F32); thb = sbt('thb', [128, 2], F32); dec = sbt('dec', [128, 2], F32)
            wq = sbt('wq', [128, 2, 64], F32); th = sbt('th', [128, 2, 64], F32)
            wT = sbt('wT', [64, 256], F32); thT = sbt('thT', [64, 256], F32); decb = sbt('decb', [128, 256], F32)
            m01 = sbt('m01', [64, 256], BF16)
            t.dma('sp', ones[:, :], c_ones, writes=['ones'])
            t.dma('sp', m01[:, :], c_m01, writes=['m01'])
            for h in range(4):
                r0, q = (h % 2) * 64, h // 2
                t.dma('sp', GI[r0:r0 + 64, q, :], gifT[h, :].rearrange('(c l) -> c l', l=64), writes=[('GI', h)])
                t.dma('sp', GF[r0:r0 + 64, q, :], gifT[4 + h, :].rearrange('(c l) -> c l', l=64), writes=[('GF', h)])
                t.dma('sp', bi_t[r0:r0 + 64, q:q + 1], b_igate[h:h + 1].partition_broadcast(64), writes=[('bi', h)])
                t.dma('sp', bf_t[r0:r0 + 64, q:q + 1], b_fgate[h:h + 1].partition_broadcast(64), writes=[('bf', h)])
            t.barrier()
            G = ['gp']
            t.op('dve', lambda e: e.tensor_scalar(nbf[:, :], bf_t[:, :], -1.0, None, ALU.mult), reads=G, writes=G)
            for q in range(2):
                t.op('act', lambda e, q=q: e.activation(ef[:, q, :], GF[:, q, :], AF.Exp, bias=nbf[:, q:q + 1], scale=-1.0), reads=G, writes=G)
            t.op('act', lambda e: e.activation(ef[:, :, :], ef[:, :, :], AF.Ln, bias=1.0), reads=G, writes=G)
            for q in range(2):
                t.op('dve', lambda e, q=q: e.tensor_tensor_scan(cs[:, q, :], ones[:, :], ef[:, q, :], 0.0, ALU.mult, ALU.add), reads=G, writes=G)
                t.op('dve', lambda e, q=q: e.scalar_tensor_tensor(aa[:, q, :], GI[:, q, :], bi_t[:, q:q + 1], cs[:, q, :], ALU.add, ALU.add), reads=G, writes=G)
            t.op('dve', lambda e: e.tensor_reduce(amax[:, :], aa[:, :, :], AX.X, ALU.max), reads=G, writes=G)
            t.op('dve', lambda e: e.tensor_scalar(bend[:, :], cs[:, :, 63], -1.0, None, ALU.mult), reads=G, writes=G)
            t.op('dve', lambda e: e.tensor_tensor(abe[:, :], amax[:, :], bend[:, :], ALU.add), reads=G, writes=G)
            t.dma('sp', gs1[0, :].rearrange('(q p) -> p q', p=128), bend[:, :], reads=G, writes=['gs1a'], **NC)
            t.dma('sp', gs1[1, :].rearrange('(q p) -> p q', p=128), abe[:, :], reads=G, writes=['gs1b'], **NC)
            t.dma('sp', b4[:, :], gs1[0, :].rearrange('(h c) -> h c', c=64), reads=['gs1a'], writes=['b4'])
            t.dma('sp', a4[:, :], gs1[1, :].rearrange('(h c) -> h c', c=64), reads=['gs1b'], writes=['a4'])
            t.op('dve', lambda e: e.tensor_tensor_scan(mn4[:, :], b4[:, :], a4[:, :], 0.0, ALU.add, ALU.max), reads=['b4', 'a4'], writes=['mn4'])
            t.op('dve', lambda e: e.memset(mp4[:, 0:1], 0.0), reads=[], writes=['mp4'])
            t.op('dve', lambda e: e.tensor_copy(mp4[:, 1:64], mn4[:, 0:63]), reads=['mn4', 'mp4'], writes=['mp4'])
            t.dma('sp', gs2.rearrange('(h c) -> h c', c=64), mp4[:, :], reads=['mp4'], writes=['gs2'])
            t.dma('sp', mp[:, :], gs2.rearrange('(q p) -> p q', p=128), reads=['gs2'], writes=G, **NC)
            t.op('dve', lambda e: e.tensor_tensor(nM[:, :], mp[:, :], amax[:, :], ALU.max), reads=G, writes=G)
            t.op('dve', lambda e: e.tensor_scalar(nM[:, :], nM[:, :], -1.0, None, ALU.mult), reads=G, writes=G)
            t.op('dve', lambda e: e.tensor_scalar(thb[:, :], nM[:, :], -LN_C, None, ALU.add), reads=G, writes=G)
            t.op('dve', lambda e: e.tensor_tensor(dec[:, :], mp[:, :], nM[:, :], ALU.add), reads=G, writes=G)
            t.op('act', lambda e: e.activation(dec[:, :], dec[:, :], AF.Exp), reads=G, writes=G)
            for q in range(2):
                t.op('act', lambda e, q=q: e.activation(wq[:, q, :], aa[:, q, :], AF.Exp, bias=nM[:, q:q + 1]), reads=G, writes=G)
                t.op('act', lambda e, q=q: e.activation(th[:, q, :], cs[:, q, :], AF.Exp, bias=thb[:, q:q + 1]), reads=G, writes=G)
            t.dma('sp', gs3.rearrange('(q p) -> p q', p=128), dec[:, :], reads=G, writes=['gs3'], **NC)
            t.dma('sp', decb[:, :], gs3.partition_broadcast(128), reads=['gs3'], writes=['decb'])
            for q in range(2):
                t.op('pe', lambda e, q=q: e.transpose(PS[0][0:64, q * 128:(q + 1) * 128], wq[:, q, :], identf[:, :]), reads=G, writes=['ps0'])
                t.op('pe', lambda e, q=q: e.transpose(PS[1][0:64, q * 128:(q + 1) * 128], th[:, q, :], identf[:, :]), reads=G, writes=['ps1'])
            t.op('dve', lambda e: e.tensor_copy(wT[:, :], PS[0][0:64, 0:256]), reads=['ps0'], writes=['wT'])
            t.op('dve', lambda e: e.tensor_copy(thT[:, :], PS[1][0:64, 0:256]), reads=['ps1'], writes=['thT'])
            t.barrier()

            qTs = [sbt('qTs%d' % i, [128, 4, 512], BF16) for i in range(2)]
            kTs = [sbt('kTs%d' % i, [128, 4, 512], BF16) for i in range(2)]
            ktm = [sbt('ktm%d' % i, [64, 8, 512], BF16) for i in range(2)]
            vx = [sbt('vx%d' % i, [64, 8, 4, 129], BF16) for i in range(2)]
            og = [sbt('og%d' % i, [64, 8, 512], BF16) for i in range(2)]
            yst = [sbt('yst%d' % i, [128, 4, 512], BF16) for i in range(2)]
            CT = sbt('CT', [128, 4, 129], F32); CTd = sbt('CTd', [128, 4, 129], F32); CTb = sbt('CTb', [128, 4, 129], BF16)
            SmT = [sbt('SmT%d' % i, [64, 256], BF16) for i in range(2)]
            vw = [sbt('vw%d' % i, [64, 4, 129], BF16) for i in range(2)]
            dd = sbt('dd', [64, 4], F32); rr = sbt('rr', [64, 4], F32); s2 = sbt('s2', [64, 4], F32)
            hh = sbt('hh', [64, 4, 128], F32); sq = sbt('sq', [64, 4, 128], F32); hn = sbt('hn', [64, 4, 128], F32)
            yt = [sbt('yt%d' % i, [64, 512], BF16) for i in range(2)]
            for i in range(2):
                t.op('pool', lambda e, i=i: e.memset(vx[i][:, :, :, :], 1.0), writes=[('vx', i)])
            t.op('dve', lambda e: e.memset(CT[:, :, :], 0.0), writes=['CT'])

            def load_sc(sc):
                sl = sc % 2
                c0 = sc * 512
                t.dma('sp', qTs[sl][:, :, :], qmlT[:, c0:c0 + 512].rearrange('(h p) tk -> p h tk', p=128), writes=[('qTs', sl)])
                t.dma('sp', kTs[sl][:, :, :], kmlT[:, c0:c0 + 512].rearrange('(h p) tk -> p h tk', p=128), writes=[('kTs', sl)])
                t.dma('sp', ktm[sl][:, :, :], kml[c0:c0 + 512, :].rearrange('(c l) d -> l c d', l=64), writes=[('ktm', sl)])
                for ci in range(8):
                    t.dma('sp', vx[sl][:, ci, :, 0:128], vml[c0 + ci * 64:c0 + (ci + 1) * 64, :].rearrange('l (h d) -> l h d', h=4),
                          writes=[('vx', sl)])
                t.dma('sp', og[sl][:, :, :], osig[c0:c0 + 512, :].rearrange('(c l) d -> l c d', l=64), writes=[('og', sl)])

            def XB(j):
                return 4 + j

            def YB(j):
                return 6 + j

            def mstage(c, k):
                sc, ci = c // 8, c % 8
                sl = sc % 2
                tk = slice(ci * 64, (ci + 1) * 64)
                par = c % 2
                if k == 0:
                    if ci == 0 and sc + 1 < 8:
                        load_sc(sc + 1)
                    for h in range(4):
                        j, hl = h // 2, h % 2
                        t.op('pe', lambda e, h=h, j=j, hl=hl: e.matmul(PS[XB(j)][0:64, 258 + hl * 64:258 + (hl + 1) * 64], lhsT=kTs[sl][:, h, tk], rhs=qTs[sl][:, h, tk],
                                                                  start=True, stop=True), reads=[('qTs', sl), ('kTs', sl)], writes=['ps%d' % XB(j)])
                    wbc = wT[:, c:256:64].unsqueeze(2).broadcast_to([64, 4, 129])
                    t.op('pool', lambda e: e.tensor_tensor(vw[par][:, :, :], vx[sl][:, ci, :, :], wbc, ALU.mult), reads=[('vx', sl)], writes=[('vw', par)])
                    dbc = decb[:, c:256:64].unsqueeze(2).broadcast_to([128, 4, 129])
                    t.op('pool', lambda e: e.tensor_tensor(CTd[:, :, :], CT[:, :, :], dbc, ALU.mult), reads=['CT'], writes=['CTd'])
                    t.op('pool', lambda e: e.tensor_copy(CTb[:, :, :], CTd[:, :, :]), reads=['CTd'], writes=['CTb'])
                elif k == 1:
                    for j in range(2):
                        t.op('dve', lambda e, j=j: e.tensor_tensor(SmT[par][:, j * 128:(j + 1) * 128], PS[XB(j)][0:64, 258:386], m01[:, 0:128], ALU.mult),
                             reads=['ps%d' % XB(j)], writes=[('SmT', par, j)])
                elif k == 2:
                    for h in range(4):
                        j, off = h // 2, (h % 2) * 129
                        t.op('pe', lambda e, h=h, j=j, off=off: e.matmul(PS[XB(j)][0:64, off:off + 129], lhsT=SmT[par][:, h * 64:(h + 1) * 64], rhs=vw[par][:, h, :],
                                                                    start=True, stop=False), reads=[('SmT', par, j), ('vw', par)], writes=['ps%d' % XB(j)])
                        t.op('pe', lambda e, h=h, j=j, off=off: e.matmul(PS[XB(j)][0:64, off:off + 129], lhsT=qTs[sl][:, h, tk], rhs=CTb[:, h, :],
                                                                    start=False, stop=True), reads=[('qTs', sl), 'CTb'], writes=['ps%d' % XB(j)])
                    for h in range(4):
                        j, off = h // 2, (h % 2) * 129
                        t.op('pe', lambda e, h=h, j=j, off=off: e.matmul(PS[YB(j)][:, off:off + 129], lhsT=ktm[sl][:, ci, h * 128:(h + 1) * 128], rhs=vw[par][:, h, :],
                                                                    start=True, stop=True), reads=[('ktm', sl), ('vw', par)], writes=['ps%d' % YB(j)])
                elif k == 3:
                    for j in range(2):
                        t.op('dve', lambda e, j=j: e.tensor_tensor(CT[:, 2 * j:2 * j + 2, :], CTd[:, 2 * j:2 * j + 2, :],
                                                                   PS[YB(j)][:, 0:258].rearrange('p (a b) -> p a b', a=2), ALU.add),
                             reads=['CTd', 'ps%d' % YB(j)], writes=['CT'])
                    for j in range(2):
                        den = PS[XB(j)][0:64, 128:258:129]
                        t.op('dve', lambda e, j=j, den=den: e.tensor_tensor(dd[:, 2 * j:2 * j + 2], den, thT[:, 2 * j * 64 + c:2 * j * 64 + c + 65:64], ALU.max),
                             reads=['ps%d' % XB(j)], writes=['dd'])
                        t.op('dve', lambda e, j=j, den=den: e.scalar_tensor_tensor(dd[:, 2 * j:2 * j + 2], den, -1.0, dd[:, 2 * j:2 * j + 2], ALU.mult, ALU.max),
                             reads=['ps%d' % XB(j), 'dd'], writes=['dd'])
                    t.op('dve', lambda e: e.reciprocal(rr[:, :], dd[:, :]), reads=['dd'], writes=['rr'])
                    for j in range(2):
                        t.op('dve', lambda e, j=j: e.tensor_tensor(hh[:, 2 * j:2 * j + 2, :], PS[XB(j)][0:64, 0:258].rearrange('p (a b) -> p a b', a=2)[:, :, 0:128],
                                                                   rr[:, 2 * j:2 * j + 2].unsqueeze(2).broadcast_to([64, 2, 128]), ALU.mult),
                             reads=['rr', 'ps%d' % XB(j)], writes=['hh'])
                    t.op('pool', lambda e: e.tensor_tensor(sq[:, :, :], hh[:, :, :], hh[:, :, :], ALU.mult), reads=['hh'], writes=['sq'])
                elif k == 4:
                    t.op('dve', lambda e: e.tensor_reduce(s2[:, :], sq[:, :, :], AX.X, ALU.add), reads=['sq'], writes=['s2'])
                    t.op('act', lambda e: e.activation(s2[:, :], s2[:, :], AF.Ln, scale=1.0 / 128, bias=EPS), reads=['s2'], writes=['s2'])
                    t.op('act', lambda e: e.activation(s2[:, :], s2[:, :], AF.Exp, scale=-0.5), reads=['s2'], writes=['s2'])
                    t.op('dve', lambda e: e.tensor_tensor(hn[:, :, :], hh[:, :, :], s2[:, :].unsqueeze(2).broadcast_to([64, 4, 128]), ALU.mult),
                         reads=['hh', 's2'], writes=['hn'])
                    t.op('pool', lambda e: e.tensor_tensor(yt[par][:, :], hn[:, :, :].rearrange('p a b -> p (a b)'), og[sl][:, ci, :], ALU.mult),
                         reads=['hn', ('og', sl)], writes=[('yt', par)])
                elif k == 5:
                    for h in range(4):
                        j, hl = h // 2, h % 2
                        t.op('pe', lambda e, h=h, j=j, hl=hl: e.transpose(PSb[YB(j)][:, 516 + hl * 64:516 + (hl + 1) * 64], yt[par][:, h * 128:(h + 1) * 128], identb[0:64, 0:64]),
                             reads=[('yt', par)], writes=['ps%d' % YB(j)])
                    for j in range(2):
                        t.op('dve', lambda e, j=j: e.tensor_copy(yst[sl][:, 2 * j:2 * j + 2, tk], PSb[YB(j)][:, 516:644].rearrange('p (h tk) -> p h tk', h=2)),
                             reads=['ps%d' % YB(j)], writes=[('yst', sl)])
                    if ci == 7:
                        t.dma('sp', ymlT[:, sc * 512:(sc + 1) * 512].rearrange('(h p) tk -> p h tk', p=128), yst[sl][:, :, :],
                              reads=[('yst', sl)], writes=[('ymlT', sc)])

            def load_q(g):
                t.dma('sp', Q[g % 2][:, :, :], qsbT[:, g * 512:(g + 1) * 512].rearrange('(j p) tk -> p j tk', p=128), writes=[('Q', g % 2)])

            units = []
            m = 0
            for g in range(NG):
                for h in range(8):
                    kbs = list(range(4 * g + 3, -1, -1))
                    for n_, kb in enumerate(kbs):
                        units.append(dict(g=g, h=h, kb=kb, first=(n_ == 0), last=(n_ == len(kbs) - 1), m=m, newg=(h == 0 and n_ == 0)))
                    m += 1
            for i_, u in enumerate(units):
                u['i'] = i_

            def c0_of(u):
                return max(0, (u['kb'] - 4 * u['g']) * 128)

            def S1(u):
                g, h, kb, i = u['g'], u['h'], u['kb'], u['i']
                if u['newg'] and g + 1 < NG:
                    load_q(g + 1)
                pb = i % 3
                j, r0 = h // 2, (h % 2) * 64
                diag = kb >= 4 * g
                c0 = c0_of(u)
                t.op('pe', lambda e: e.matmul(PS[pb][:, c0:512], lhsT=KT[r0:r0 + 64, j, kb * 128:(kb + 1) * 128], rhs=Q[g % 2][r0:r0 + 64, j, c0:512],
                                              start=True, stop=True), reads=[('Q', g % 2)], writes=['ps%d' % pb])
                if diag:
                    di = kb - 4 * g
                    t.op('pe', lambda e: e.matmul(PS[pb][:, c0:c0 + 128], lhsT=identb[:, :], rhs=negm[:, di, c0:c0 + 128], start=False, stop=True,
                                                  skip_group_check=True), reads=[], writes=['ps%d' % pb])

            def S2a(u):
                i = u['i']
                pb, sl = i % 3, i % 3
                c0 = c0_of(u)
                t.op('act', lambda e: e.activation(E[sl][:, c0:512], PS[pb][:, c0:512], AF.Exp), reads=['ps%d' % pb], writes=[('E', sl)])

            def S2b(u):
                i = u['i']
                pb, sl = i % 3, i % 3
                c0 = c0_of(u)
                t.op('act', lambda e: e.activation(SPt[sl][:, c0:512], E[sl][:, c0:512], AF.Ln, bias=1.0), reads=[('E', sl)], writes=[('SP', sl)])

            def S3(u):
                i, m_ = u['i'], u['m']
                pb, sl, rs = i % 3, i % 3, m_ % 2
                c0 = c0_of(u)
                t.op('pe', lambda e: e.matmul(PS[pb][:, c0:512], lhsT=uneg[:, :], rhs=SPt[sl][:, c0:512], start=False, stop=u['first'], skip_group_check=True),
                     reads=[('SP', sl)], writes=['ps%d' % pb])
                if not u['first']:
                    t.op('pe', lambda e: e.matmul(PS[pb][:, c0:512], lhsT=oneg[:, :], rhs=R[rs][:, c0:512], start=False, stop=True, skip_group_check=True),
                         reads=[('R', rs)], writes=['ps%d' % pb])
                if not u['last']:
                    if u['first']:
                        if c0 > 0:
                            t.op('dve', lambda e: e.memset(R[rs][:, 0:c0], 0.0), reads=[], writes=[('R', rs)])
                        t.op('dve', lambda e: e.tensor_copy(R[rs][:, c0:512], SPt[sl][:, c0:512]), reads=[('SP', sl), ('R', rs)], writes=[('R', rs)])
                    else:
                        t.op('dve', lambda e: e.tensor_tensor(R[rs][:, c0:512], R[rs][:, c0:512], SPt[sl][:, c0:512], ALU.add),
                             reads=[('SP', sl), ('R', rs)], writes=[('R', rs)])

            def S4(u):
                i = u['i']
                pb, sl = i % 3, i % 3
                c0 = c0_of(u)
                t.op('act', lambda e: e.activation(A[sl][:, c0:512], PS[pb][:, c0:512], AF.Exp), reads=['ps%d' % pb], writes=[('A', sl)])

            def S5(u):
                g, h, kb, i, m_ = u['g'], u['h'], u['kb'], u['i'], u['m']
                sl = i % 3
                o0 = (m_ % 2) * 64
                ok = ('ps3', m_ % 2)
                c0 = c0_of(u)
                t.op('pe', lambda e: e.matmul(PS[3][o0:o0 + 64, c0:512], lhsT=V[:, kb, h * 64:(h + 1) * 64], rhs=A[sl][:, c0:512], start=u['first'], stop=u['last'],
                                              skip_group_check=True), reads=[('A', sl)], writes=[ok])
                if u['last']:
                    os_ = ostg[m_ % 2]
                    t.op('dve', lambda e: e.tensor_copy(os_[:, :], PS[3][o0:o0 + 64, :]), reads=[ok], writes=[('ostg', m_ % 2)])
                    t.dma('sp', ysbT[h * 64:(h + 1) * 64, g * 512:(g + 1) * 512], os_[:, :], reads=[('ostg', m_ % 2)], writes=[('ysbT', m_)])

            load_q(0)
            load_sc(0)
            n = len(units)
            GAP = 3
            for i in range(n + 2):
                if i < n:
                    S1(units[i])
                    S2a(units[i])
                    S2b(units[i])
                if 0 <= i - 1 < n:
                    S3(units[i - 1])
                    S4(units[i - 1])
                if 0 <= i - 2 < n:
                    S5(units[i - 2])
                if i % GAP == 0:
                    sidx = i // GAP
                    c, k = sidx // 6, sidx % 6
                    if c < 64:
                        mstage(c, k)
            t.barrier()

    def phase4a():
        with ExitStack() as es:
            def sbt(name, shape, dt):
                return es.enter_context(nc.sbuf_tensor(name, shape, dt))
            Wsb = sbt('Wsb', [128, 4, D], BF16); Wml = sbt('Wml', [128, 4, D], BF16); Wo = sbt('Wo', [128, 8, D], BF16)
            gbc = sbt('gbc', [128, D], F32)
            t.dma('sp', gbc[:, :], g_mix_post.partition_broadcast(128), writes=['gbc'])
            load_bf16_weight(Wsb, Wsb_b, 4, 'wsb')
            load_bf16_weight(Wml, Wml_b, 4, 'wml')
            load_bf16_weight(Wo, Wo_b, 8, 'wo')
            t.barrier()
            ysT = [sbt('ysT%d' % i, [128, 4, 512], BF16) for i in range(2)]
            ymT = [sbt('ymT%d' % i, [128, 4, 512], BF16) for i in range(2)]
            gsx = [sbt('gsx%d' % i, [128, 2048], BF16) for i in range(3)]
            xs = [sbt('xa%d' % i, [128, D], F32) for i in range(3)]
            t1 = sbt('t1', [128, D], F32); t2 = sbt('t2', [128, D], F32)
            mg = [sbt('mg%d' % i, [128, D], BF16) for i in range(2)]
            mT = [sbt('mT%d' % i, [128, 8, 128], BF16) for i in range(2)]
            junk = sbt('junk4', [128, 512], BF16)
            ssq = sbt('ssq4', [128, 4], F32)
            tmp = sbt('tmp4', [128, D], F32)
            xo = [sbt('xo%d' % i, [128, D], F32) for i in range(2)]
            junk2 = sbt('junk4b', [128, D], BF16)
            h2 = [sbt('h2_%d' % i, [128, D], BF16) for i in range(2)]
            h2T = [sbt('h2T_%d' % i, [128, 8, 128], BF16) for i in range(2)]

            def tileB2a(tt):
                sl = tt % 2
                rms_rstd(xo[sl][:, :], [('xo', sl, 0), ('xo', sl, 1)], junk2, ssq[:, 3:4], ('ssq4', 3))
                t.op('dve', lambda e: e.tensor_scalar(h2[sl][:, :], xo[sl][:, :], ssq[:, 3:4], None, ALU.mult),
                     reads=[('xo', sl, 0), ('xo', sl, 1), ('ssq4', 3)], writes=[('h2', sl)])

            def tileB2b(tt):
                sl = tt % 2
                transpose8(h2[sl], ('h2', sl), 8, h2T[sl][:, :, :], ('h2T', sl), 6 + sl, 'dve')
                t.dma('sp', h2T_d[:, tt * 128:(tt + 1) * 128].rearrange('(k p) tk -> p k tk', p=128), h2T[sl][:, :, :],
                      reads=[('h2T', sl)], writes=[('h2T_d', tt)])

            def load_g(g):
                sl = g % 2
                t.dma('sp', ysT[sl][:, :, :], ysbT[:, g * 512:(g + 1) * 512].rearrange('(j p) tk -> p j tk', p=128), writes=[('ysT', sl)])
                t.dma('sp', ymT[sl][:, :, :], ymlT[:, g * 512:(g + 1) * 512].rearrange('(j p) tk -> p j tk', p=128), writes=[('ymT', sl)])

            def load_t(tt):
                sl = tt % 3
                t.dma('sp', gsx[sl][:, :], gsig[tt * 128:(tt + 1) * 128, :], writes=[('gsx', sl)])
                t.dma('sp', xs[sl][:, :], x[tt * 128:(tt + 1) * 128, :], writes=[('xa', sl)])

            def tileA1(tt):
                g, ti, sl = tt // 4, tt % 4, tt % 2
                gl = g % 2
                tks = slice(ti * 128, (ti + 1) * 128)
                for half in range(2):
                    cs_ = slice(half * 512, (half + 1) * 512)
                    for (Ysrc, Wsrc, bk, yk) in ((ysT, Wsb, half, 'ysT'), (ymT, Wml, 2 + half, 'ymT')):
                        for j in range(4):
                            t.op('pe', lambda e, Ysrc=Ysrc, Wsrc=Wsrc, bk=bk, j=j, cs_=cs_: e.matmul(PS[bk][:, :], lhsT=Ysrc[gl][:, j, tks], rhs=Wsrc[:, j, cs_],
                                                                                                start=(j == 0), stop=(j == 3)),
                                 reads=[(yk, gl)], writes=['ps%d' % bk])
                for half in range(2):
                    cs_ = slice(half * 512, (half + 1) * 512)
                    t.op('dve', lambda e, half=half, cs_=cs_: e.tensor_tensor(t1[:, cs_], PS[half][:, :], gsx[tt % 3][:, cs_], ALU.mult),
                         reads=['ps%d' % half, ('gsx', tt % 3)], writes=[('t1', half)])
                    t.op('dve', lambda e, half=half, cs_=cs_: e.tensor_tensor(t2[:, cs_], PS[2 + half][:, :], gsx[tt % 3][:, 1024 + half * 512:1024 + (half + 1) * 512], ALU.mult),
                         reads=['ps%d' % (2 + half), ('gsx', tt % 3)], writes=[('t2', half)])
                    t.op('pool', lambda e, cs_=cs_: e.tensor_tensor(mg[sl][:, cs_], t1[:, cs_], t2[:, cs_], ALU.add),
                         reads=[('t1', half), ('t2', half)], writes=[('mg', sl, half)])

            def tileA2(tt):
                sl = tt % 2
                transpose8(mg[sl], [('mg', sl, 0), ('mg', sl, 1)], 8, mT[sl][:, :, :], ('mT', sl), 6 + sl, 'act')

            def tileB(tt):
                sl = tt % 2
                for half in range(2):
                    cs_ = slice(half * 512, (half + 1) * 512)
                    bk = 4 + half
                    for kc in range(8):
                        t.op('pe', lambda e, kc=kc, bk=bk, cs_=cs_: e.matmul(PS[bk][:, :], lhsT=mT[sl][:, kc, :], rhs=Wo[:, kc, cs_], start=(kc == 0), stop=(kc == 7)),
                             reads=[('mT', sl)], writes=['ps%d' % bk])
                for half in range(2):
                    t.op('act', lambda e, half=half: e.activation(junk[:, :], PS[4 + half][:, :], AF.Square, accum_out=ssq[:, half:half + 1]),
                         reads=['ps%d' % (4 + half)], writes=['junk4', ('ssq4', half)])
                t.op('dve', lambda e: e.tensor_tensor(ssq[:, 2:3], ssq[:, 0:1], ssq[:, 1:2], ALU.add), reads=[('ssq4', 0), ('ssq4', 1)], writes=[('ssq4', 2)])
                t.op('act', lambda e: e.activation(ssq[:, 2:3], ssq[:, 2:3], AF.Sqrt, scale=1.0 / 1024, bias=EPS), reads=[('ssq4', 2)], writes=[('ssq4', 2)])
                t.op('dve', lambda e: e.reciprocal(ssq[:, 2:3], ssq[:, 2:3]), reads=[('ssq4', 2)], writes=[('ssq4', 2)])
                for half in range(2):
                    cs_ = slice(half * 512, (half + 1) * 512)
                    t.op('dve', lambda e, half=half, cs_=cs_: e.tensor_tensor(tmp[:, cs_], PS[4 + half][:, :], gbc[:, cs_], ALU.mult),
                         reads=['ps%d' % (4 + half)], writes=[('tmp4', half)])
                    t.op('dve', lambda e, cs_=cs_: e.scalar_tensor_tensor(xo[sl][:, cs_], tmp[:, cs_], ssq[:, 2:3], xs[tt % 3][:, cs_], ALU.mult, ALU.add),
                         reads=[('tmp4', half), ('ssq4', 2), ('xa', tt % 3)], writes=[('xo', sl, half)])
                t.dma('sp', x1[tt * 128:(tt + 1) * 128, :], xo[sl][:, :], reads=[('xo', sl, 0), ('xo', sl, 1)], writes=[('x1', tt)])

            load_g(0)
            load_t(0)
            load_t(1)
            load_t(2)
            tileA1(0)
            tileA2(0)
            for tt in range(NT):
                if (tt + 1) % 4 == 0 and (tt + 1) // 4 + 1 < NG:
                    load_g((tt + 1) // 4 + 1)
                if tt == 0:
                    load_g(1)
                if tt + 1 < NT:
                    tileA1(tt + 1)
                if tt >= 1:
                    tileB2b(tt - 1)
                tileB(tt)
                tileB2a(tt)
                if tt + 3 < NT:
                    load_t(tt + 3)
                if tt + 1 < NT:
                    tileA2(tt + 1)
            tileB2b(NT - 1)
            t.barrier()

    def phase4b():
        with ExitStack() as es:
            def sbt(name, shape, dt):
                return es.enter_context(nc.sbuf_tensor(name, shape, dt))
            Wup = sbt('Wup', [128, 8, 4096], BF16); Wdn = sbt('Wdn', [128, 32, D], BF16)
            gbc = sbt('gbc5', [128, D], F32)
            t.dma('sp', gbc[:, :], g_mlp_post.partition_broadcast(128), writes=['gbc'])
            hT = [sbt('hT5_%d' % i, [128, 8, 512], BF16) for i in range(2)]
            xr = [sbt('xr%d' % i, [128, D], F32) for i in range(3)]

            def load_h(g):
                t.dma('sp', hT[g % 2][:, :, :], h2T_d[:, g * 512:(g + 1) * 512].rearrange('(k p) tk -> p k tk', p=128), writes=[('hT5', g % 2)])

            def load_xr(tt):
                t.dma('sp', xr[tt % 3][:, :], x1[tt * 128:(tt + 1) * 128, :], writes=[('xr', tt % 3)])

            load_h(0)
            load_bf16_weight(Wup, Wup_b, 8, 'wup')
            load_h(1)
            load_bf16_weight(Wdn, Wdn_b, 32, 'wdn')
            for tt in range(3):
                load_xr(tt)
            UT = sbt('UT', [128, 32, 512], BF16)
            rl = [sbt('rl%d' % i, [128, 512], F32) for i in range(2)]
            junk = sbt('junk5', [128, 512], BF16)
            ssq = sbt('ssq5', [128, 8], F32)
            tmp = sbt('tmp5', [128, 512], F32)
            xo = [sbt('xo5_%d' % i, [128, 512], F32) for i in range(2)]
            rdw_up = [('wup', kc) for kc in range(8)]
            rdw_dn = [('wdn', f) for f in range(32)]

            def group(g):
                hTg = hT[g % 2]
                for f in range(32):
                    bk = f % 4
                    for kc in range(8):
                        t.op('pe', lambda e, kc=kc, bk=bk, f=f: e.matmul(PS[bk][:, :], lhsT=Wup[:, kc, f * 128:(f + 1) * 128], rhs=hTg[:, kc, :], start=(kc == 0), stop=(kc == 7)),
                             reads=[('hT5', g % 2)] + (rdw_up if g == 0 else []), writes=['ps%d' % bk])
                    r = rl[f % 2]
                    t.op('act', lambda e, bk=bk, r=r: e.activation(r[:, :], PS[bk][:, :], AF.Relu), reads=['ps%d' % bk], writes=[('rl', f % 2)])
                    t.op('pool', lambda e, f=f, r=r: e.tensor_tensor(UT[:, f, :], r[:, :], r[:, :], ALU.mult), reads=[('rl', f % 2)], writes=[('UT', f)])
                rdu = [('UT', f) for f in range(32)]
                for ti in range(4):
                    tt = g * 4 + ti
                    xs_ = xr[tt % 3]
                    b0 = 4 + 2 * (ti % 2)
                    for half in range(2):
                        bk = b0 + half
                        cs_ = slice(half * 512, (half + 1) * 512)
                        for f in range(32):
                            t.op('pe', lambda e, f=f, bk=bk, cs_=cs_, ti=ti: e.matmul(PS[bk][:, :], lhsT=UT[:, f, ti * 128:(ti + 1) * 128], rhs=Wdn[:, f, cs_],
                                                                                  start=(f == 0), stop=(f == 31)),
                                 reads=rdu + (rdw_dn if (g == 0 and ti == 0) else []), writes=['ps%d' % bk])
                    c0 = 2 * (ti % 2)
                    for half in range(2):
                        t.op('act', lambda e, half=half: e.activation(junk[:, :], PS[b0 + half][:, :], AF.Square, accum_out=ssq[:, c0 + half:c0 + half + 1]),
                             reads=['ps%d' % (b0 + half)], writes=['junk', ('ssq5', c0 + half)])
                    sc_ = ssq[:, 4 + ti % 2:5 + ti % 2]
                    sk_ = ('ssq5', 4 + ti % 2)
                    t.op('dve', lambda e: e.tensor_tensor(sc_, ssq[:, c0:c0 + 1], ssq[:, c0 + 1:c0 + 2], ALU.add), reads=[('ssq5', c0), ('ssq5', c0 + 1)], writes=[sk_])
                    t.op('act', lambda e: e.activation(sc_, sc_, AF.Sqrt, scale=1.0 / 1024, bias=EPS), reads=[sk_], writes=[sk_])
                    t.op('dve', lambda e: e.reciprocal(sc_, sc_), reads=[sk_], writes=[sk_])
                    for half in range(2):
                        cs_ = slice(half * 512, (half + 1) * 512)
                        t.op('dve', lambda e, half=half, cs_=cs_: e.tensor_tensor(tmp[:, :], PS[b0 + half][:, :], gbc[:, cs_], ALU.mult),
                             reads=['ps%d' % (b0 + half), 'gbc'], writes=['tmp5'])
                        t.op('dve', lambda e, half=half, cs_=cs_: e.scalar_tensor_tensor(xo[half][:, :], tmp[:, :], sc_, xs_[:, cs_], ALU.mult, ALU.add),
                             reads=['tmp5', sk_, ('xr', tt % 3)], writes=[('xo5', half)])
                        t.dma('sp', x2[tt * 128:(tt + 1) * 128, cs_], xo[half][:, :], reads=[('xo5', half)], writes=[('x2', tt, half)])
                    if tt + 3 < NT:
                        load_xr(tt + 3)

            for g in range(NG):
                group(g)
                if g + 2 < NG:
                    load_h(g + 2)
            t.barrier()

    def phase4c():
        with ExitStack() as es:
            def sbt(name, shape, dt):
                return es.enter_context(nc.sbuf_tensor(name, shape, dt))
            Wg = sbt('Wg', [128, 8, D], BF16); Wp = sbt('Wp', [128, 2, D], BF16)
            gbc = sbt('gbc6', [128, D], F32)
            t.dma('sp', gbc[:, :], g_ple_post.partition_broadcast(128), writes=['gbc'])
            load_bf16_weight(Wg, Wg_b, 8, 'wg')
            load_bf16_weight(Wp, Wp_b, 2, 'wp')
            t.barrier()
            xs = [sbt('xc%d' % i, [128, D], F32) for i in range(3)]
            pt = [sbt('pt%d' % i, [128, 256], F32) for i in range(3)]
            pbf = [sbt('pbf%d' % i, [128, 256], BF16) for i in range(2)]
            hb = [sbt('hb6_%d' % i, [128, D], BF16) for i in range(2)]
            hT = [sbt('hT6_%d' % i, [128, 8, 128], BF16) for i in range(2)]
            pT = [sbt('pT6_%d' % i, [128, 2, 128], BF16) for i in range(2)]
            sg = sbt('sg6', [128, D], F32); ee = sbt('ee6', [128, D], F32)
            junk = sbt('junk6', [128, D], BF16)
            ssq = sbt('ssq6', [128, 4], F32)
            tmp = sbt('tmp6', [128, D], F32)
            xo = [sbt('xo6_%d' % i, [128, D], F32) for i in range(2)]

            def load_t(tt):
                sl = tt % 3
                t.dma('sp', xs[tt % 3][:, :], x2[tt * 128:(tt + 1) * 128, :], writes=[('xc', tt % 3)])
                t.dma('sp', pt[tt % 3][:, :], p_in[tt * 128:(tt + 1) * 128, :], writes=[('pt', tt % 3)])

            def tileC12(tt):
                sl = tt % 2
                rms_rstd(xs[tt % 3][:, :], [('xc', tt % 3)], junk, ssq[:, 0:1], ('ssq6', 0))
                t.op('dve', lambda e: e.tensor_scalar(hb[sl][:, :], xs[tt % 3][:, :], ssq[:, 0:1], None, ALU.mult), reads=[('xc', tt % 3), ('ssq6', 0)], writes=[('hb6', sl)])
                t.op('pool', lambda e: e.tensor_copy(pbf[sl][:, :], pt[tt % 3][:, :]), reads=[('pt', tt % 3)], writes=[('pbf', sl)])
                transpose8(hb[sl], ('hb6', sl), 8, hT[sl][:, :, :], ('hT6', sl), 6, 'act')
                transpose8(pbf[sl], ('pbf', sl), 2, pT[sl][:, :, :], ('pT6', sl), 7, 'dve')

            def tileC3(tt):
                sl = tt % 2
                for half in range(2):
                    cs_ = slice(half * 512, (half + 1) * 512)
                    for kc in range(8):
                        t.op('pe', lambda e, kc=kc, half=half, cs_=cs_: e.matmul(PS[half][:, :], lhsT=hT[sl][:, kc, :], rhs=Wg[:, kc, cs_], start=(kc == 0), stop=(kc == 7)),
                             reads=[('hT6', sl)], writes=['ps%d' % half])
                    for j in range(2):
                        t.op('pe', lambda e, j=j, half=half, cs_=cs_: e.matmul(PS[2 + half][:, :], lhsT=pT[sl][:, j, :], rhs=Wp[:, j, cs_], start=(j == 0), stop=(j == 1)),
                             reads=[('pT6', sl)], writes=['ps%d' % (2 + half)])

            def tileC4(tt):
                sl = tt % 2
                for half in range(2):
                    cs_ = slice(half * 512, (half + 1) * 512)
                    t.op('act', lambda e, half=half, cs_=cs_: e.activation(sg[:, cs_], PS[half][:, :], AF.Sigmoid), reads=['ps%d' % half], writes=[('sg6', half)])
                    t.op('dve', lambda e, half=half, cs_=cs_: e.tensor_tensor(ee[:, cs_], PS[2 + half][:, :], sg[:, cs_], ALU.mult),
                         reads=['ps%d' % (2 + half), ('sg6', half)], writes=[('ee6', half)])
                rms_rstd(ee[:, :], [('ee6', 0), ('ee6', 1)], junk, ssq[:, 1:2], ('ssq6', 1))
                t.op('pool', lambda e: e.tensor_tensor(tmp[:, :], ee[:, :], gbc[:, :], ALU.mult), reads=[('ee6', 0), ('ee6', 1)], writes=['tmp6'])
                t.op('dve', lambda e: e.scalar_tensor_tensor(xo[sl][:, :], tmp[:, :], ssq[:, 1:2], xs[tt % 3][:, :], ALU.mult, ALU.add),
                     reads=['tmp6', ('ssq6', 1), ('xc', tt % 3)], writes=[('xo6', sl)])
                t.dma('sp', y[tt * 128:(tt + 1) * 128, :], xo[sl][:, :], reads=[('xo6', sl)], writes=[('y', tt)])

            load_t(0)
            load_t(1)
            load_t(2)
            tileC12(0)
            for tt in range(NT):
                tileC3(tt)
                if tt + 1 < NT:
                    tileC12(tt + 1)
                tileC4(tt)
                if tt + 3 < NT:
                    load_t(tt + 3)
            t.barrier()

    if upto >= 1:
        phase1()
    if upto >= 3:
        phase23()
    if upto >= 4:
        phase4a()
    if upto >= 5:
        phase4b()
    if upto >= 6:
        phase4c()
    t.barrier()
    return nc, t


def make_in_maps(inputs):
    c = host_consts()
    maps = []
    sq = {k: np.ascontiguousarray(np.asarray(v, dtype=np.float32)[0]) for k, v in inputs.items() if k not in ('x',)}
    xx = np.asarray(inputs['x'], dtype=np.float32)
    for b in range(8):
        m = dict(c)
        m['x'] = np.ascontiguousarray(xx[b])
        m['p'] = np.ascontiguousarray(sq['p'][b])
        for k, v in sq.items():
            if k != 'p':
                m[k] = v
        maps.append(m)
    return maps


def kernel(**inputs):
    nc, _ = build(debug=False)
    maps = make_in_maps(inputs)
    res = run_bass_kernel_spmd(nc, maps, core_ids=list(range(8)))
    out = np.stack([np.asarray(r['y'], dtype=np.float32) for r in res.results], axis=0)
    return out
```

```python
import os
import math
from contextlib import ExitStack
import numpy as np
import ml_dtypes
import concourse.bass as bass
import concourse.mybir as mybir
from concourse.bass_utils import run_bass_kernel_spmd

F32 = mybir.dt.float32
BF16 = mybir.dt.bfloat16
AF = mybir.ActivationFunctionType
ALU = mybir.AluOpType
AX = mybir.AxisListType

S = 4096
D = 1024
NCOL = 5640
NG = 8
NT = 32
EPS = 1e-6
SB_SCALE = 1.0 / 8.0
LN_C = -0.5 * math.log(128.0)
NEG = -30000.0


class Trk:
    SAME_ENGINE_SYNC = True
    NDMA = 8

    def __init__(self, nc):
        self.nc = nc
        self.eng = {'pe': nc.tensor, 'act': nc.scalar, 'dve': nc.vector, 'pool': nc.gpsimd, 'sp': nc.sync}
        self.sem, self.cnt = {}, {}
        for e in ('pe', 'act', 'dve', 'pool'):
            self.sem[e] = nc.alloc_semaphore('c_' + e)
            self.cnt[e] = 0
        self.dsem, self.dcnt = {}, {}
        for q in ('sp', 'act', 'pool'):
            self.dsem[q] = [nc.alloc_semaphore('d_%s%d' % (q, i)) for i in range(self.NDMA)]
            self.dcnt[q] = 0
        self.waited, self.lastw, self.readers, self.semobj = {}, {}, {}, {}
        for e, s in self.sem.items():
            self.semobj[('c', e)] = s
        for q, l in self.dsem.items():
            for i, s in enumerate(l):
                self.semobj[('d', q, i)] = s
        self.nops = 0

    def _wait(self, e, tok):
        if tok is None:
            return
        sk, val = tok
        if sk[0] == 'c' and sk[1] == e:
            if e == 'pe' or not self.SAME_ENGINE_SYNC:
                return
        if self.waited.get((e, sk), 0) >= val:
            return
        self.eng[e].wait_ge(self.semobj[sk], val)
        self.waited[(e, sk)] = val

    def _deps(self, e, reads, writes):
        need = {}

        def add(tok):
            if tok is not None and need.get(tok[0], 0) < tok[1]:
                need[tok[0]] = tok[1]
        for b in reads:
            add(self.lastw.get(b))
        for b in writes:
            add(self.lastw.get(b))
            for r in self.readers.get(b, ()):
                add(r)
        for sk, val in need.items():
            self._wait(e, (sk, val))

    def _commit(self, tok, reads, writes):
        for b in reads:
            lst = self.readers.setdefault(b, [])
            for k_, r in enumerate(lst):
                if r[0] == tok[0]:
                    lst[k_] = tok
                    break
            else:
                lst.append(tok)
        for b in writes:
            self.lastw[b] = tok
            self.readers[b] = []

    def op(self, e, fn, reads=(), writes=()):
        self._deps(e, reads, writes)
        ins = fn(self.eng[e])
        self.cnt[e] += 1
        ins.then_inc(self.sem[e], 1)
        tok = (('c', e), self.cnt[e])
        self._commit(tok, reads, writes)
        self.nops += 1
        return tok

    def dma(self, q, out, in_, reads=(), writes=(), **kw):
        i = self.dcnt[q]
        slot, rnd = i % self.NDMA, i // self.NDMA
        sk = ('d', q, slot)
        if rnd > 0:
            self._wait(q, (sk, 16 * rnd))
        self._deps(q, reads, writes)
        self.eng[q].dma_start(out=out, in_=in_, **kw).then_inc(self.semobj[sk], 16)
        self.dcnt[q] += 1
        tok = (sk, 16 * (rnd + 1))
        self._commit(tok, reads, writes)
        self.nops += 1
        return tok

    def barrier(self):
        toks = [(('c', e), self.cnt[e]) for e in self.sem if self.cnt[e] > 0]
        for q in self.dsem:
            n = self.dcnt[q]
            for s in range(self.NDMA):
                k = (n - s + self.NDMA - 1) // self.NDMA
                if k > 0:
                    toks.append((('d', q, s), 16 * k))
        for e in ('pe', 'act', 'dve', 'pool', 'sp'):
            for sk, val in toks:
                if sk[0] == 'c' and sk[1] == e:
                    continue
                if self.waited.get((e, sk), 0) >= val:
                    continue
                self.eng[e].wait_ge(self.semobj[sk], val)
                self.waited[(e, sk)] = val
        self.lastw.clear()
        self.readers.clear()


def host_consts():
    bf = ml_dtypes.bfloat16
    c = {}
    c['c_identb'] = np.eye(128, dtype=np.float32).astype(bf)
    c['c_identf'] = np.eye(128, dtype=np.float32)
    j = np.arange(128)[:, None]
    s = np.arange(128)[None, :]
    c['c_uneg'] = np.where(j >= s, -1.0, 0.0).astype(np.float32).astype(bf)
    c['c_oneg'] = np.full((128, 128), -1.0, np.float32).astype(bf)
    negm = np.zeros((128, 4, 512), np.float32)
    for i in range(4):
        key = 128 * i + np.arange(128)[:, None]
        qq = np.arange(512)[None, :]
        negm[:, i, :] = np.where(key >= qq, NEG, 0.0)
    c['c_negm'] = negm.astype(bf)
    ss = np.arange(64)[:, None]
    tt = np.arange(64)[None, :]
    m01 = np.where(ss <= tt, 1.0, 0.0).astype(np.float32)
    c['c_m01'] = np.tile(m01, (1, 4)).astype(bf)
    c['c_ones'] = np.ones((128, 64), np.float32)
    return c


def build(debug=False, upto=99):
    nc = bass.Bass("TRN2", target_bir_lowering=False)
    t = Trk(nc)

    def din(name, shape, dt=F32):
        return nc.dram_tensor(name, list(shape), dt, kind="ExternalInput").ap()

    def dscr(name, shape, dt):
        return nc.dram_tensor(name, list(shape), dt, kind=("ExternalOutput" if debug else "Internal")).ap()

    x = din('x', [S, D]); p_in = din('p', [S, 256])
    g_mix_pre = din('g_mix_pre', [D]); w_in = din('w_in', [D, NCOL])
    b_igate = din('b_igate', [4]); b_fgate = din('b_fgate', [4])
    w_conv = din('w_conv', [4, 1024]); b_conv = din('b_conv', [1024])
    g_mlstm_head = din('g_mlstm_head', [512])
    w_branch_sb = din('w_branch_sb', [512, D]); w_branch_ml = din('w_branch_ml', [512, D])
    w_out = din('w_out', [D, D]); g_mix_post = din('g_mix_post', [D]); g_mlp_pre = din('g_mlp_pre', [D])
    w_mlp_up = din('w_mlp_up', [D, 4096]); w_mlp_down = din('w_mlp_down', [4096, D])
    g_mlp_post = din('g_mlp_post', [D]); g_ple_pre = din('g_ple_pre', [D])
    w_ple_gate = din('w_ple_gate', [D, D]); w_ple_proj = din('w_ple_proj', [256, D]); g_ple_post = din('g_ple_post', [D])
    c_identb = din('c_identb', [128, 128], BF16); c_identf = din('c_identf', [128, 128])
    c_uneg = din('c_uneg', [128, 128], BF16); c_oneg = din('c_oneg', [128, 128], BF16)
    c_negm = din('c_negm', [128, 4, 512], BF16); c_m01 = din('c_m01', [64, 256], BF16)
    c_ones = din('c_ones', [128, 64])
    y = nc.dram_tensor('y', [S, D], F32, kind="ExternalOutput").ap()

    qsbT = dscr('qsbT', [512, S], BF16); ksbT = dscr('ksbT', [512, S], BF16); vsb = dscr('vsb', [S, 512], BF16)
    qmlT = dscr('qmlT', [512, S], BF16); kmlT = dscr('kmlT', [512, S], BF16); kml = dscr('kml', [S, 512], BF16)
    vml = dscr('vml', [S, 512], BF16); osig = dscr('osig', [S, 512], BF16); gsig = dscr('gsig', [S, 2048], BF16)
    gifT = dscr('gifT', [8, S], F32)
    gs1 = dscr('gs1', [2, 256], F32); gs2 = dscr('gs2', [256], F32); gs3 = dscr('gs3', [256], F32)
    ysbT = dscr('ysbT', [512, S], BF16); ymlT = dscr('ymlT', [512, S], BF16)
    x1 = dscr('x1', [S, D], F32); x2 = dscr('x2', [S, D], F32)
    Wsb_b = dscr('Wsb_b', [512, D], BF16); Wml_b = dscr('Wml_b', [512, D], BF16); Wo_b = dscr('Wo_b', [D, D], BF16)
    Wup_b = dscr('Wup_b', [D, 4096], BF16); Wdn_b = dscr('Wdn_b', [4096, D], BF16)
    Wg_b = dscr('Wg_b', [D, D], BF16); Wp_b = dscr('Wp_b', [256, D], BF16)
    h2T_d = dscr('h2T_d', [D, S], BF16)

    PS = [nc.alloc_psum_tensor('ps%d' % i, [128, 512], F32) for i in range(8)]
    PSb = [h.bitcast(BF16) for h in PS]
    identb = nc.alloc_sbuf_tensor('identb', [128, 128], BF16)
    identf = nc.alloc_sbuf_tensor('identf', [128, 128], F32)
    t.dma('sp', identb[:, :], c_identb, writes=['identb'])
    t.dma('sp', identf[:, :], c_identf, writes=['identf'])
    t.barrier()

    NC = dict(allow_slow_non_contiguous=True)

    def rms_rstd(src_ap, rd, junk, ssc, key):
        t.op('act', lambda e: e.activation(junk[:, :], src_ap, AF.Square, accum_out=ssc), reads=rd, writes=['junk', key])
        t.op('act', lambda e: e.activation(ssc, ssc, AF.Sqrt, scale=1.0 / 1024, bias=EPS), reads=[key], writes=[key])
        t.op('dve', lambda e: e.reciprocal(ssc, ssc), reads=[key], writes=[key])

    def transpose8(src, srckey, n, dst_ap, dstkey, bank, eng):
        pb = PSb[bank]
        srckeys = srckey if isinstance(srckey, list) else [srckey]
        for kc in range(n):
            t.op('pe', lambda e, kc=kc: e.transpose(pb[:, kc * 128:(kc + 1) * 128], src[:, kc * 128:(kc + 1) * 128], identb[:, :]),
                 reads=srckeys, writes=['ps%d' % bank])
        src_v = pb[:, 0:n * 128].rearrange('p (k t) -> p k t', k=n)
        if eng == 'act':
            t.op('act', lambda e: e.activation(dst_ap, src_v, AF.Copy), reads=['ps%d' % bank], writes=[dstkey])
        else:
            t.op(eng, lambda e: e.tensor_copy(dst_ap, src_v), reads=['ps%d' % bank], writes=[dstkey])

    def cast_weight(es0, Wdst, wsrc, nk, ncols, gvec, piece, name):
        stg = [es0.enter_context(nc.sbuf_tensor('%s_stg%d' % (name, i), [128, piece], F32)) for i in range(3)]
        n = 0
        for kc in range(nk):
            for c0 in range(0, ncols, piece):
                w = min(piece, ncols - c0)
                sl = n % 3
                t.dma('sp', stg[sl][:, 0:w], wsrc[kc * 128:(kc + 1) * 128, c0:c0 + w], writes=[(name, 'stg', sl)])
                dst = Wdst[:, kc, c0:c0 + w]
                if n % 2 == 0:
                    if gvec is None:
                        t.op('dve', lambda e, dst=dst, sl=sl, w=w: e.tensor_copy(dst, stg[sl][:, 0:w]),
                             reads=[(name, 'stg', sl)], writes=[(name, kc, c0)])
                    else:
                        t.op('dve', lambda e, dst=dst, sl=sl, w=w, kc=kc: e.tensor_scalar(dst, stg[sl][:, 0:w], gvec[:, kc:kc + 1], None, ALU.mult),
                             reads=[(name, 'stg', sl)], writes=[(name, kc, c0)])
                else:
                    if gvec is None:
                        t.op('act', lambda e, dst=dst, sl=sl, w=w: e.activation(dst, stg[sl][:, 0:w], AF.Copy),
                             reads=[(name, 'stg', sl)], writes=[(name, kc, c0)])
                    else:
                        t.op('act', lambda e, dst=dst, sl=sl, w=w, kc=kc: e.activation(dst, stg[sl][:, 0:w], AF.Copy, scale=gvec[:, kc:kc + 1]),
                             reads=[(name, 'stg', sl)], writes=[(name, kc, c0)])
                n += 1

    def load_bf16_weight(Wdst, wsrc_b, nk, name):
        for kc in range(nk):
            t.dma('sp', Wdst[:, kc, :], wsrc_b[kc * 128:(kc + 1) * 128, :], writes=[(name, kc)])

    def phase1():
        with ExitStack() as es:
            def sbt(name, shape, dt):
                return es.enter_context(nc.sbuf_tensor(name, shape, dt))
            Wb = sbt('Wb', [128, 8, NCOL], BF16)
            gpre = sbt('gpre', [128, 8], F32)
            wcv = sbt('wcv', [128, 8, 4], F32)
            bcv = sbt('bcv', [128, 8], F32)
            ghb = sbt('ghb', [128, 512], F32)
            t.dma('sp', gpre[:, :], g_mix_pre.rearrange('(k p) -> p k', p=128), writes=['gpre'], **NC)
            for tap in range(4):
                t.dma('sp', wcv[:, :, tap], w_conv[tap, :].rearrange('(j p) -> p j', p=128), writes=[('wcv', tap)], **NC)
            t.dma('sp', bcv[:, :], b_conv.rearrange('(j p) -> p j', p=128), writes=['bcv'], **NC)
            t.dma('sp', ghb[:, :], g_mlstm_head.partition_broadcast(128), writes=['ghb'])
            t.barrier()
            with ExitStack() as es0:
                cast_weight(es0, Wb, w_in, 8, NCOL, gpre, 1880, 'win')
                t.barrier()
            t.barrier()

            xs = [sbt('xs%d' % i, [128, 1024], F32) for i in range(8)]
            hb = [sbt('hb%d' % i, [128, 1024], BF16) for i in range(4)]
            hT = [sbt('hT%d' % i, [128, 8, 512], BF16) for i in range(2)]
            junk = sbt('junk', [128, 1024], BF16)
            ssq = sbt('ssq', [128, 8], F32)
            cb = sbt('cb', [128, 8, 515], F32)
            acc = [sbt('acc%d' % i, [128, 512], F32) for i in range(2)]
            ost = [sbt('ost%d' % i, [128, 512], BF16) for i in range(6)]
            osf = [sbt('osf%d' % i, [128, 512], F32) for i in range(2)]
            gst = [sbt('gst%d' % i, [8, 512], F32) for i in range(2)]
            kst = [sbt('kst%d' % i, [128, 4, 128], BF16) for i in range(2)]
            kbf = [sbt('kbf%d' % i, [128, 512], BF16) for i in range(4)]
            t.op('pool', lambda e: e.memset(cb[:, :, 0:3], 0.0), writes=[('cb', j) for j in range(8)])

            gv_up = sbt('gv_up', [128, 8], F32); gv_g = sbt('gv_g', [128, 8], F32)
            t.dma('sp', gv_up[:, :], g_mlp_pre.rearrange('(k p) -> p k', p=128), writes=['gv_up'], **NC)
            t.dma('sp', gv_g[:, :], g_ple_pre.rearrange('(k p) -> p k', p=128), writes=['gv_g'], **NC)
            wst = [sbt('wst%d' % i, [128, 1024], F32) for i in range(2)]
            wsb16 = [sbt('wsb16_%d' % i, [128, 1024], BF16) for i in range(2)]
            pieces = []
            for (src, dst, nk, ncols, gv) in ((w_branch_sb, Wsb_b, 4, D, None), (w_branch_ml, Wml_b, 4, D, None), (w_out, Wo_b, 8, D, None),
                                              (w_mlp_up, Wup_b, 8, 4096, gv_up), (w_mlp_down, Wdn_b, 32, D, None),
                                              (w_ple_gate, Wg_b, 8, D, gv_g), (w_ple_proj, Wp_b, 2, D, None)):
                for kc in range(nk):
                    for c0 in range(0, ncols, 1024):
                        pieces.append((src, dst, kc, c0, gv))

            def piece_load(pi):
                src, dst, kc, c0, gv = pieces[pi]
                sl = pi % 2
                t.dma('sp', wst[sl][:, :], src[kc * 128:(kc + 1) * 128, c0:c0 + 1024], writes=[('wst', sl)])

            def piece_cast(pi):
                src, dst, kc, c0, gv = pieces[pi]
                sl = pi % 2
                if gv is None:
                    t.op('act', lambda e: e.activation(wsb16[sl][:, :], wst[sl][:, :], AF.Copy), reads=[('wst', sl)], writes=[('wsb16', sl)])
                else:
                    t.op('act', lambda e: e.activation(wsb16[sl][:, :], wst[sl][:, :], AF.Copy, scale=gv[:, kc:kc + 1]),
                         reads=[('wst', sl), 'gv_up', 'gv_g'], writes=[('wsb16', sl)])
                t.dma('act', dst[kc * 128:(kc + 1) * 128, c0:c0 + 1024], wsb16[sl][:, :], reads=[('wsb16', sl)], writes=[('wcast', pi)])

            st = {'bank': 0, 'ost': 0, 'n': 0, 'slot': 0}

            def precast_tick():
                k_ = st['slot']
                st['slot'] += 1
                pi = k_ // 4
                if k_ % 4 == 0 and pi < len(pieces):
                    piece_load(pi)
                elif k_ % 4 == 2 and 0 <= pi - 1 < len(pieces):
                    piece_cast(pi - 1)

            def nbank():
                b = st['bank']
                st['bank'] = (b + 1) % 6
                precast_tick()
                return b

            def nost():
                o = st['ost']
                st['ost'] = (o + 1) % 6
                return o

            def load_x(tt):
                sl = tt % 8
                t.dma('sp', xs[sl][:, :], x[tt * 128:(tt + 1) * 128, :], writes=[('xs', sl)])

            def norm_C(tt):
                sl, hs, g, col, ti = tt % 8, tt % 4, tt // 4, tt % 8, tt % 4
                rms_rstd(xs[sl][:, :], [('xs', sl)], junk, ssq[:, col:col + 1], ('ssq', col))
                t.op('dve', lambda e: e.tensor_scalar(hb[hs][:, :], xs[sl][:, :], ssq[:, col:col + 1], None, ALU.mult),
                     reads=[('xs', sl), ('ssq', col)], writes=[('hb', hs)])

            def norm_P(tt):
                sl, hs, g, col, ti = tt % 8, tt % 4, tt // 4, tt % 8, tt % 4
                transpose8(hb[hs], ('hb', hs), 8, hT[g % 2][:, :, ti * 128:(ti + 1) * 128], ('hT', g % 2, ti), 6 + (tt % 2),
                           'act' if tt % 2 == 0 else 'dve')

            def norm_T(tt):
                norm_C(tt)
                norm_P(tt)

            def evac_copy(dst, src, rd, wr, scale=None):
                n = st['n']
                st['n'] += 1
                if n % 2 == 0:
                    if scale is None:
                        t.op('act', lambda e: e.activation(dst, src, AF.Copy), reads=rd, writes=wr)
                    else:
                        t.op('act', lambda e: e.activation(dst, src, AF.Copy, scale=scale), reads=rd, writes=wr)
                else:
                    if scale is None:
                        t.op('dve', lambda e: e.tensor_copy(dst, src), reads=rd, writes=wr)
                    else:
                        t.op('dve', lambda e: e.tensor_scalar(dst, src, scale, None, ALU.mult), reads=rd, writes=wr)

            def proj_group(g):
                hTg = hT[g % 2]
                rdh = [('hT', g % 2, i) for i in range(4)]
                tok0 = g * 512
                fm = [('qsb', cc, cc * 128) for cc in range(4)] + [('ksb', cc, 512 + cc * 128) for cc in range(4)] + \
                     [('qml', cc, 1536 + cc * 128) for cc in range(4)] + [('kml', cc, 2048 + cc * 128) for cc in range(4)]
                for kind, cc, col0 in fm:
                    b = nbank()
                    bk = 'ps%d' % b
                    for kc in range(8):
                        t.op('pe', lambda e, kc=kc, b=b, col0=col0: e.matmul(PS[b][:, :], lhsT=Wb[:, kc, col0:col0 + 128], rhs=hTg[:, kc, :],
                                                                       start=(kc == 0), stop=(kc == 7)), reads=rdh, writes=[bk])
                    if kind in ('qsb', 'ksb'):
                        o = nost()
                        evac_copy(ost[o][:, :], PS[b][:, :], [bk], [('ost', o)], scale=(SB_SCALE if kind == 'qsb' else None))
                        dst = (qsbT if kind == 'qsb' else ksbT)[cc * 128:(cc + 1) * 128, tok0:tok0 + 512]
                        t.dma('sp', dst, ost[o][:, :], reads=[('ost', o)], writes=[(kind, cc, g)])
                    else:
                        j = cc if kind == 'qml' else 4 + cc
                        ck = ('cb', j)
                        evac_copy(cb[:, j, 3:515], PS[b][:, :], [bk], [ck])
                        a = acc[j % 2]
                        ak = ('acc', j % 2)
                        t.op('dve', lambda e, j=j, a=a: e.tensor_scalar(a[:, :], cb[:, j, 3:515], wcv[:, j, 3:4], None, ALU.mult), reads=[ck], writes=[ak])
                        for tap in (2, 1, 0):
                            t.op('dve', lambda e, j=j, a=a, tap=tap: e.scalar_tensor_tensor(a[:, :], cb[:, j, tap:tap + 512], wcv[:, j, tap:tap + 1], a[:, :],
                                                                                     ALU.mult, ALU.add), reads=[ck, ak], writes=[ak])
                        t.op('pool', lambda e, j=j: e.tensor_copy(cb[:, j, 0:3], cb[:, j, 512:515]), reads=[ck], writes=[ck])
                        if kind == 'qml':
                            o = nost()
                            t.op('act', lambda e, j=j, a=a, o=o: e.activation(ost[o][:, :], a[:, :], AF.Silu, bias=bcv[:, j:j + 1]), reads=[ak], writes=[('ost', o)])
                            t.dma('sp', qmlT[cc * 128:(cc + 1) * 128, tok0:tok0 + 512], ost[o][:, :], reads=[('ost', o)], writes=[('qmlT', cc, g)])
                        else:
                            kb_ = kbf[cc]
                            t.op('act', lambda e, j=j, a=a, kb_=kb_: e.activation(kb_[:, :], a[:, :], AF.Silu, bias=bcv[:, j:j + 1]), reads=[ak], writes=[('kbf', cc)])
                            t.dma('sp', kmlT[cc * 128:(cc + 1) * 128, tok0:tok0 + 512], kb_[:, :], reads=[('kbf', cc)], writes=[('kmlT', cc, g)])

                def k_transposes(cc):
                    tb = 6 + (cc % 2)
                    pb = PSb[tb]
                    for i in range(4):
                        t.op('pe', lambda e, i=i, pb=pb: e.transpose(pb[:, i * 128:(i + 1) * 128], kbf[cc][:, i * 128:(i + 1) * 128], identb[:, :]),
                             reads=[('kbf', cc)], writes=['ps%d' % tb])
                    ks = kst[cc % 2]
                    t.op('dve', lambda e, ks=ks, pb=pb: e.tensor_copy(ks[:, :, :], pb[:, 0:512].rearrange('p (i d) -> p i d', i=4)),
                         reads=['ps%d' % tb], writes=[('kst', cc % 2)])
                    t.dma('sp', kml[tok0:tok0 + 512, cc * 128:(cc + 1) * 128].rearrange('(i p) d -> p i d', p=128), ks[:, :, :],
                          reads=[('kst', cc % 2)], writes=[('kml', cc, g)])

                b = nbank()
                bk = 'ps%d' % b
                for kc in range(8):
                    t.op('pe', lambda e, kc=kc, b=b: e.matmul(PS[b][0:8, :], lhsT=Wb[:, kc, 3584:3592], rhs=hTg[:, kc, :], start=(kc == 0), stop=(kc == 7)),
                         reads=rdh, writes=[bk])
                gs = gst[g % 2]
                t.op('dve', lambda e, b=b, gs=gs: e.tensor_copy(gs[:, :], PS[b][0:8, :]), reads=[bk], writes=[('gst', g % 2)])
                t.dma('sp', gifT[:, tok0:tok0 + 512], gs[:, :], reads=[('gst', g % 2)], writes=[('gifT', g)])
                tmj = [('vsb', 1024, vsb, 0), ('vml', 2560, vml, 0), ('oml', 3072, osig, 0),
                       ('gate', 3592, gsig, 0), ('gate', 4104, gsig, 512), ('gate', 4616, gsig, 1024), ('gate', 5128, gsig, 1536)]
                for ti in range(4):
                    r0 = tok0 + ti * 128
                    if ti == 1:
                        for cc in range(4):
                            k_transposes(cc)
                    if ti == 2 and g + 1 < NG:
                        for tj in range(4):
                            norm_C((g + 1) * 4 + tj)
                    if ti == 3 and g + 1 < NG:
                        for tj in range(4):
                            norm_P((g + 1) * 4 + tj)
                    for kind, col0, dstT, dcol in tmj:
                        b = nbank()
                        bk = 'ps%d' % b
                        for kc in range(8):
                            t.op('pe', lambda e, kc=kc, b=b, col0=col0, ti=ti: e.matmul(PS[b][:, :], lhsT=hTg[:, kc, ti * 128:(ti + 1) * 128],
                                                                                 rhs=Wb[:, kc, col0:col0 + 512], start=(kc == 0), stop=(kc == 7)),
                                 reads=[('hT', g % 2, ti)], writes=[bk])
                        o = nost()
                        if kind in ('vsb', 'vml'):
                            evac_copy(ost[o][:, :], PS[b][:, :], [bk], [('ost', o)])
                        elif kind == 'gate':
                            t.op('act', lambda e, b=b, o=o: e.activation(ost[o][:, :], PS[b][:, :], AF.Sigmoid), reads=[bk], writes=[('ost', o)])
                        else:
                            f = osf[ti % 2]
                            t.op('act', lambda e, b=b, f=f: e.activation(f[:, :], PS[b][:, :], AF.Sigmoid), reads=[bk], writes=[('osf', ti % 2)])
                            t.op('dve', lambda e, f=f, o=o: e.tensor_tensor(ost[o][:, :], f[:, :], ghb[:, :], ALU.mult), reads=[('osf', ti % 2)], writes=[('ost', o)])
                        t.dma('sp', dstT[r0:r0 + 128, dcol:dcol + 512], ost[o][:, :], reads=[('ost', o)], writes=[(kind, col0, g, ti)])

            for tt in range(8):
                load_x(tt)
            for ti in range(4):
                norm_T(ti)
            for g in range(NG):
                proj_group(g)
                if g + 2 < NG:
                    for ti in range(4):
                        load_x((g + 2) * 4 + ti)
            assert st['slot'] >= 4 * len(pieces)
            piece_cast(len(pieces) - 1)
            t.barrier()

    def phase23():
        with ExitStack() as es:
            def sbt(name, shape, dt):
                return es.enter_context(nc.sbuf_tensor(name, shape, dt))
            KT = sbt('KT', [128, 4, S], BF16)
            V = sbt('V', [128, 32, 512], BF16)
            Q = [sbt('Q%d' % i, [128, 4, 512], BF16) for i in range(2)]
            E = [sbt('E%d' % i, [128, 512], F32) for i in range(3)]
            SPt = [sbt('SP%d' % i, [128, 512], BF16) for i in range(3)]
            A = [sbt('A%d' % i, [128, 512], BF16) for i in range(3)]
            R = [sbt('R%d' % i, [128, 512], BF16) for i in range(2)]
            uneg = sbt('uneg', [128, 128], BF16)
            oneg = sbt('oneg', [128, 128], BF16)
            negm = sbt('negm', [128, 4, 512], BF16)
            ostg = [sbt('ostg%d' % i, [64, 512], BF16) for i in range(2)]
            t.dma('sp', uneg[:, :], c_uneg, writes=['uneg'])
            t.dma('sp', oneg[:, :], c_oneg, writes=['oneg'])
            t.dma('sp', negm[:, :, :], c_negm, writes=['negm'])
            for j in range(4):
                t.dma('sp', KT[:, j, :], ksbT[j * 128:(j + 1) * 128, :], writes=[('KT', j)])
            for j in range(4):
                t.dma('sp', V[:, j * 8:(j + 1) * 8, :], vsb[j * 1024:(j + 1) * 1024, :].rearrange('(kb p) c -> p kb c', p=128), writes=[('V', j)])
            GI = sbt('GI', [128, 2, 64], F32); GF = sbt('GF', [128, 2, 64], F32)
            bi_t = sbt('bi_t', [128, 2], F32); bf_t = sbt('bf_t', [128, 2], F32); nbf = sbt('nbf', [128, 2], F32)
            ones = sbt('ones', [128, 64], F32)
            ef = sbt('ef', [128, 2, 64], F32); cs = sbt('cs', [128, 2, 64], F32); aa = sbt('aa', [128, 2, 64], F32)
            amax = sbt('amax', [128, 2], F32); bend = sbt('bend', [128, 2], F32); abe = sbt('abe', [128, 2], F32)
            b4 = sbt('b4', [4, 64], F32); a4 = sbt('a4', [4, 64], F32); mn4 = sbt('mn4', [4, 64], F32); mp4 = sbt('mp4', [4, 64], F32)
            mp = sbt('mp', [128, 2], F32); nM = sbt('nM', [128, 2], F32); thb = sbt('thb', [128, 2], F32); dec = sbt('dec', [128, 2], F32)
            wq = sbt('wq', [128, 2, 64], F32); th = sbt('th', [128, 2, 64], F32)
            wT = sbt('wT', [64, 256], F32); thT = sbt('thT', [64, 256], F32); decb = sbt('decb', [128, 256], F32)
            m01 = sbt('m01', [64, 256], BF16)
            t.dma('sp', ones[:, :], c_ones, writes=['ones'])
            t.dma('sp', m01[:, :], c_m01, writes=['m01'])
            for h in range(4):
                r0, q = (h % 2) * 64, h // 2
                t.dma('sp', GI[r0:r0 + 64, q, :], gifT[h, :].rearrange('(c l) -> c l', l=64), writes=[('GI', h)])
                t.dma('sp', GF[r0:r0 + 64, q, :], gifT[4 + h, :].rearrange('(c l) -> c l', l=64), writes=[('GF', h)])
                t.dma('sp', bi_t[r0:r0 + 64, q:q + 1], b_igate[h:h + 1].partition_broadcast(64), writes=[('bi', h)])
                t.dma('sp', bf_t[r0:r0 + 64, q:q + 1], b_fgate[h:h + 1].partition_broadcast(64), writes=[('bf', h)])
            G = ['gp', 'ones'] + [(k_, h_) for k_ in ('GI', 'GF', 'bi', 'bf') for h_ in range(4)]
            t.op('dve', lambda e: e.tensor_scalar(nbf[:, :], bf_t[:, :], -1.0, None, ALU.mult), reads=G, writes=G)
            for q in range(2):
                t.op('act', lambda e, q=q: e.activation(ef[:, q, :], GF[:, q, :], AF.Exp, bias=nbf[:, q:q + 1], scale=-1.0), reads=G, writes=G)
            t.op('act', lambda e: e.activation(ef[:, :, :], ef[:, :, :], AF.Ln, bias=1.0), reads=G, writes=G)
            for q in range(2):
                t.op('dve', lambda e, q=q: e.tensor_tensor_scan(cs[:, q, :], ones[:, :], ef[:, q, :], 0.0, ALU.mult, ALU.add), reads=G, writes=G)
                t.op('dve', lambda e, q=q: e.scalar_tensor_tensor(aa[:, q, :], GI[:, q, :], bi_t[:, q:q + 1], cs[:, q, :], ALU.add, ALU.add), reads=G, writes=G)
            t.op('dve', lambda e: e.tensor_reduce(amax[:, :], aa[:, :, :], AX.X, ALU.max), reads=G, writes=G)
            t.op('dve', lambda e: e.tensor_scalar(bend[:, :], cs[:, :, 63], -1.0, None, ALU.mult), reads=G, writes=G)
            t.op('dve', lambda e: e.tensor_tensor(abe[:, :], amax[:, :], bend[:, :], ALU.add), reads=G, writes=G)
            t.dma('sp', gs1[0, :].rearrange('(q p) -> p q', p=128), bend[:, :], reads=G, writes=['gs1a'], **NC)
            t.dma('sp', gs1[1, :].rearrange('(q p) -> p q', p=128), abe[:, :], reads=G, writes=['gs1b'], **NC)
            t.dma('sp', b4[:, :], gs1[0, :].rearrange('(h c) -> h c', c=64), reads=['gs1a'], writes=['b4'])
            t.dma('sp', a4[:, :], gs1[1, :].rearrange('(h c) -> h c', c=64), reads=['gs1b'], writes=['a4'])
            t.op('dve', lambda e: e.tensor_tensor_scan(mn4[:, :], b4[:, :], a4[:, :], 0.0, ALU.add, ALU.max), reads=['b4', 'a4'], writes=['mn4'])
            t.op('dve', lambda e: e.memset(mp4[:, 0:1], 0.0), reads=[], writes=['mp4'])
            t.op('dve', lambda e: e.tensor_copy(mp4[:, 1:64], mn4[:, 0:63]), reads=['mn4', 'mp4'], writes=['mp4'])
            t.dma('sp', gs2.rearrange('(h c) -> h c', c=64), mp4[:, :], reads=['mp4'], writes=['gs2'])
            t.dma('sp', mp[:, :], gs2.rearrange('(q p) -> p q', p=128), reads=['gs2'], writes=G, **NC)
            t.op('dve', lambda e: e.tensor_tensor(nM[:, :], mp[:, :], amax[:, :], ALU.max), reads=G, writes=G)
            t.op('dve', lambda e: e.tensor_scalar(nM[:, :], nM[:, :], -1.0, None, ALU.mult), reads=G, writes=G)
            t.op('dve', lambda e: e.tensor_scalar(thb[:, :], nM[:, :], -LN_C, None, ALU.add), reads=G, writes=G)
            t.op('dve', lambda e: e.tensor_tensor(dec[:, :], mp[:, :], nM[:, :], ALU.add), reads=G, writes=G)
            t.op('act', lambda e: e.activation(dec[:, :], dec[:, :], AF.Exp), reads=G, writes=G)
            for q in range(2):
                t.op('act', lambda e, q=q: e.activation(wq[:, q, :], aa[:, q, :], AF.Exp, bias=nM[:, q:q + 1]), reads=G, writes=G)
                t.op('act', lambda e, q=q: e.activation(th[:, q, :], cs[:, q, :], AF.Exp, bias=thb[:, q:q + 1]), reads=G, writes=G)
            t.dma('sp', gs3.rearrange('(q p) -> p q', p=128), dec[:, :], reads=G, writes=['gs3'], **NC)
            t.dma('sp', decb[:, :], gs3.partition_broadcast(128), reads=['gs3'], writes=['decb'])
            for q in range(2):
                t.op('pe', lambda e, q=q: e.transpose(PS[0][0:64, q * 128:(q + 1) * 128], wq[:, q, :], identf[:, :]), reads=G, writes=['ps0'])
                t.op('pe', lambda e, q=q: e.transpose(PS[1][0:64, q * 128:(q + 1) * 128], th[:, q, :], identf[:, :]), reads=G, writes=['ps1'])
            t.op('dve', lambda e: e.tensor_copy(wT[:, :], PS[0][0:64, 0:256]), reads=['ps0'], writes=['wT'])
            t.op('dve', lambda e: e.tensor_copy(thT[:, :], PS[1][0:64, 0:256]), reads=['ps1'], writes=['thT'])
            t.barrier()

            qTs = [sbt('qTs%d' % i, [128, 4, 512], BF16) for i in range(2)]
            kTs = [sbt('kTs%d' % i, [128, 4, 512], BF16) for i in range(2)]
            ktm = [sbt('ktm%d' % i, [64, 8, 512], BF16) for i in range(2)]
            vx = [sbt('vx%d' % i, [64, 8, 4, 129], BF16) for i in range(2)]
            og = [sbt('og%d' % i, [64, 8, 512], BF16) for i in range(2)]
            yst = [sbt('yst%d' % i, [128, 4, 512], BF16) for i in range(2)]
            CT = sbt('CT', [128, 4, 129], F32); CTd = sbt('CTd', [128, 4, 129], F32); CTb = sbt('CTb', [128, 4, 129], BF16)
            SmT = [sbt('SmT%d' % i, [64, 256], BF16) for i in range(2)]
            vw = [sbt('vw%d' % i, [64, 4, 129], BF16) for i in range(2)]
            dd = sbt('dd', [64, 4], F32); rr = sbt('rr', [64, 4], F32); s2 = sbt('s2', [64, 4], F32)
            hh = sbt('hh', [64, 4, 128], F32); sq = sbt('sq', [64, 4, 128], F32); hn = sbt('hn', [64, 4, 128], F32)
            yt = [sbt('yt%d' % i, [64, 512], BF16) for i in range(2)]
            for i in range(2):
                t.op('pool', lambda e, i=i: e.memset(vx[i][:, :, :, :], 1.0), writes=[('vx', i)])
            t.op('dve', lambda e: e.memset(CT[:, :, :], 0.0), writes=['CT'])

            def load_sc(sc):
                sl = sc % 2
                c0 = sc * 512
                t.dma('sp', qTs[sl][:, :, :], qmlT[:, c0:c0 + 512].rearrange('(h p) tk -> p h tk', p=128), writes=[('qTs', sl)])
                t.dma('sp', kTs[sl][:, :, :], kmlT[:, c0:c0 + 512].rearrange('(h p) tk -> p h tk', p=128), writes=[('kTs', sl)])
                t.dma('sp', ktm[sl][:, :, :], kml[c0:c0 + 512, :].rearrange('(c l) d -> l c d', l=64), writes=[('ktm', sl)])
                for ci in range(8):
                    t.dma('sp', vx[sl][:, ci, :, 0:128], vml[c0 + ci * 64:c0 + (ci + 1) * 64, :].rearrange('l (h d) -> l h d', h=4),
                          writes=[('vx', sl)])
                t.dma('sp', og[sl][:, :, :], osig[c0:c0 + 512, :].rearrange('(c l) d -> l c d', l=64), writes=[('og', sl)])

            def XB(j):
                return 4 + j

            def YB(j):
                return 6 + j

            def mstage(c, k):
                sc, ci = c // 8, c % 8
                sl = sc % 2
                tk = slice(ci * 64, (ci + 1) * 64)
                par = c % 2
                if k == 0:
                    if ci == 0 and sc + 1 < 8:
                        load_sc(sc + 1)
                    for h in range(4):
                        j, hl = h // 2, h % 2
                        t.op('pe', lambda e, h=h, j=j, hl=hl: e.matmul(PS[XB(j)][0:64, 258 + hl * 64:258 + (hl + 1) * 64], lhsT=kTs[sl][:, h, tk], rhs=qTs[sl][:, h, tk],
                                                                  start=True, stop=True), reads=[('qTs', sl), ('kTs', sl)], writes=['ps%d' % XB(j)])
                    wbc = wT[:, c:256:64].unsqueeze(2).broadcast_to([64, 4, 129])
                    t.op('pool', lambda e: e.tensor_tensor(vw[par][:, :, :], vx[sl][:, ci, :, :], wbc, ALU.mult), reads=[('vx', sl)], writes=[('vw', par)])
                    dbc = decb[:, c:256:64].unsqueeze(2).broadcast_to([128, 4, 129])
                    t.op('pool', lambda e: e.tensor_tensor(CTd[:, :, :], CT[:, :, :], dbc, ALU.mult), reads=['CT'], writes=['CTd'])
                    t.op('pool', lambda e: e.tensor_copy(CTb[:, :, :], CTd[:, :, :]), reads=['CTd'], writes=['CTb'])
                elif k == 1:
                    for j in range(2):
                        t.op('dve', lambda e, j=j: e.tensor_tensor(SmT[par][:, j * 128:(j + 1) * 128], PS[XB(j)][0:64, 258:386], m01[:, 0:128], ALU.mult),
                             reads=['ps%d' % XB(j)], writes=[('SmT', par, j)])
                elif k == 2:
                    for h in range(4):
                        j, off = h // 2, (h % 2) * 129
                        t.op('pe', lambda e, h=h, j=j, off=off: e.matmul(PS[XB(j)][0:64, off:off + 129], lhsT=SmT[par][:, h * 64:(h + 1) * 64], rhs=vw[par][:, h, :],
                                                                    start=True, stop=False), reads=[('SmT', par, j), ('vw', par)], writes=['ps%d' % XB(j)])
                        t.op('pe', lambda e, h=h, j=j, off=off: e.matmul(PS[XB(j)][0:64, off:off + 129], lhsT=qTs[sl][:, h, tk], rhs=CTb[:, h, :],
                                                                    start=False, stop=True), reads=[('qTs', sl), 'CTb'], writes=['ps%d' % XB(j)])
                    for h in range(4):
                        j, off = h // 2, (h % 2) * 129
                        t.op('pe', lambda e, h=h, j=j, off=off: e.matmul(PS[YB(j)][:, off:off + 129], lhsT=ktm[sl][:, ci, h * 128:(h + 1) * 128], rhs=vw[par][:, h, :],
                                                                    start=True, stop=True), reads=[('ktm', sl), ('vw', par)], writes=['ps%d' % YB(j)])
                elif k == 3:
                    for j in range(2):
                        t.op('dve', lambda e, j=j: e.tensor_tensor(CT[:, 2 * j:2 * j + 2, :], CTd[:, 2 * j:2 * j + 2, :],
                                                                   PS[YB(j)][:, 0:258].rearrange('p (a b) -> p a b', a=2), ALU.add),
                             reads=['CTd', 'ps%d' % YB(j)], writes=['CT'])
                    for j in range(2):
                        den = PS[XB(j)][0:64, 128:258:129]
                        t.op('dve', lambda e, j=j, den=den: e.tensor_tensor(dd[:, 2 * j:2 * j + 2], den, thT[:, 2 * j * 64 + c:2 * j * 64 + c + 65:64], ALU.max),
                             reads=['ps%d' % XB(j)], writes=['dd'])
                        t.op('dve', lambda e, j=j, den=den: e.scalar_tensor_tensor(dd[:, 2 * j:2 * j + 2], den, -1.0, dd[:, 2 * j:2 * j + 2], ALU.mult, ALU.max),
                             reads=['ps%d' % XB(j), 'dd'], writes=['dd'])
                    t.op('dve', lambda e: e.reciprocal(rr[:, :], dd[:, :]), reads=['dd'], writes=['rr'])
                    for j in range(2):
                        t.op('dve', lambda e, j=j: e.tensor_tensor(hh[:, 2 * j:2 * j + 2, :], PS[XB(j)][0:64, 0:258].rearrange('p (a b) -> p a b', a=2)[:, :, 0:128],
                                                                   rr[:, 2 * j:2 * j + 2].unsqueeze(2).broadcast_to([64, 2, 128]), ALU.mult),
                             reads=['rr', 'ps%d' % XB(j)], writes=['hh'])
                    t.op('pool', lambda e: e.tensor_tensor(sq[:, :, :], hh[:, :, :], hh[:, :, :], ALU.mult), reads=['hh'], writes=['sq'])
                elif k == 4:
                    t.op('dve', lambda e: e.tensor_reduce(s2[:, :], sq[:, :, :], AX.X, ALU.add), reads=['sq'], writes=['s2'])
                    t.op('act', lambda e: e.activation(s2[:, :], s2[:, :], AF.Ln, scale=1.0 / 128, bias=EPS), reads=['s2'], writes=['s2'])
                    t.op('act', lambda e: e.activation(s2[:, :], s2[:, :], AF.Exp, scale=-0.5), reads=['s2'], writes=['s2'])
                    t.op('dve', lambda e: e.tensor_tensor(hn[:, :, :], hh[:, :, :], s2[:, :].unsqueeze(2).broadcast_to([64, 4, 128]), ALU.mult),
                         reads=['hh', 's2'], writes=['hn'])
                    t.op('pool', lambda e: e.tensor_tensor(yt[par][:, :], hn[:, :, :].rearrange('p a b -> p (a b)'), og[sl][:, ci, :], ALU.mult),
                         reads=['hn', ('og', sl)], writes=[('yt', par)])
                elif k == 5:
                    for h in range(4):
                        j, hl = h // 2, h % 2
                        t.op('pe', lambda e, h=h, j=j, hl=hl: e.transpose(PSb[YB(j)][:, 516 + hl * 64:516 + (hl + 1) * 64], yt[par][:, h * 128:(h + 1) * 128], identb[0:64, 0:64]),
                             reads=[('yt', par)], writes=['ps%d' % YB(j)])
                    for j in range(2):
                        t.op('dve', lambda e, j=j: e.tensor_copy(yst[sl][:, 2 * j:2 * j + 2, tk], PSb[YB(j)][:, 516:644].rearrange('p (h tk) -> p h tk', h=2)),
                             reads=['ps%d' % YB(j)], writes=[('yst', sl)])
                    if ci == 7:
                        t.dma('sp', ymlT[:, sc * 512:(sc + 1) * 512].rearrange('(h p) tk -> p h tk', p=128), yst[sl][:, :, :],
                              reads=[('yst', sl)], writes=[('ymlT', sc)])

            def load_q(g):
                t.dma('sp', Q[g % 2][:, :, :], qsbT[:, g * 512:(g + 1) * 512].rearrange('(j p) tk -> p j tk', p=128), writes=[('Q', g % 2)])

            units = []
            m = 0
            for g in range(NG):
                for h in range(8):
                    kbs = list(range(4 * g + 3, -1, -1))
                    for n_, kb in enumerate(kbs):
                        units.append(dict(g=g, h=h, kb=kb, first=(n_ == 0), last=(n_ == len(kbs) - 1), m=m, newg=(h == 0 and n_ == 0)))
                    m += 1
            for i_, u in enumerate(units):
                u['i'] = i_

            def c0_of(u):
                return max(0, (u['kb'] - 4 * u['g']) * 128)

            def S1(u):
                g, h, kb, i = u['g'], u['h'], u['kb'], u['i']
                if u['newg'] and g + 1 < NG:
                    load_q(g + 1)
                pb = i % 3
                j, r0 = h // 2, (h % 2) * 64
                diag = kb >= 4 * g
                c0 = c0_of(u)
                t.op('pe', lambda e: e.matmul(PS[pb][:, c0:512], lhsT=KT[r0:r0 + 64, j, kb * 128:(kb + 1) * 128], rhs=Q[g % 2][r0:r0 + 64, j, c0:512],
                                              start=True, stop=True), reads=[('Q', g % 2)], writes=['ps%d' % pb])
                if diag:
                    di = kb - 4 * g
                    t.op('pe', lambda e: e.matmul(PS[pb][:, c0:c0 + 128], lhsT=identb[:, :], rhs=negm[:, di, c0:c0 + 128], start=False, stop=True,
                                                  skip_group_check=True), reads=[], writes=['ps%d' % pb])

            def S2a(u):
                i = u['i']
                pb, sl = i % 3, i % 3
                c0 = c0_of(u)
                t.op('act', lambda e: e.activation(E[sl][:, c0:512], PS[pb][:, c0:512], AF.Exp), reads=['ps%d' % pb], writes=[('E', sl)])

            def S2b(u):
                i = u['i']
                pb, sl = i % 3, i % 3
                c0 = c0_of(u)
                t.op('act', lambda e: e.activation(SPt[sl][:, c0:512], E[sl][:, c0:512], AF.Ln, bias=1.0), reads=[('E', sl)], writes=[('SP', sl)])

            def S3(u):
                i, m_ = u['i'], u['m']
                pb, sl, rs = i % 3, i % 3, m_ % 2
                c0 = c0_of(u)
                t.op('pe', lambda e: e.matmul(PS[pb][:, c0:512], lhsT=uneg[:, :], rhs=SPt[sl][:, c0:512], start=False, stop=u['first'], skip_group_check=True),
                     reads=[('SP', sl)], writes=['ps%d' % pb])
                if not u['first']:
                    t.op('pe', lambda e: e.matmul(PS[pb][:, c0:512], lhsT=oneg[:, :], rhs=R[rs][:, c0:512], start=False, stop=True, skip_group_check=True),
                         reads=[('R', rs)], writes=['ps%d' % pb])
                if not u['last']:
                    if u['first']:
                        if c0 > 0:
                            t.op('dve', lambda e: e.memset(R[rs][:, 0:c0], 0.0), reads=[], writes=[('R', rs)])
                        t.op('dve', lambda e: e.tensor_copy(R[rs][:, c0:512], SPt[sl][:, c0:512]), reads=[('SP', sl), ('R', rs)], writes=[('R', rs)])
                    else:
                        t.op('dve', lambda e: e.tensor_tensor(R[rs][:, c0:512], R[rs][:, c0:512], SPt[sl][:, c0:512], ALU.add),
                             reads=[('SP', sl), ('R', rs)], writes=[('R', rs)])

            def S4(u):
                i = u['i']
                pb, sl = i % 3, i % 3
                c0 = c0_of(u)
                t.op('act', lambda e: e.activation(A[sl][:, c0:512], PS[pb][:, c0:512], AF.Exp), reads=['ps%d' % pb], writes=[('A', sl)])

            def S5(u):
                g, h, kb, i, m_ = u['g'], u['h'], u['kb'], u['i'], u['m']
                sl = i % 3
                o0 = (m_ % 2) * 64
                ok = ('ps3', m_ % 2)
                c0 = c0_of(u)
                t.op('pe', lambda e: e.matmul(PS[3][o0:o0 + 64, c0:512], lhsT=V[:, kb, h * 64:(h + 1) * 64], rhs=A[sl][:, c0:512], start=u['first'], stop=u['last'],
                                              skip_group_check=True), reads=[('A', sl)], writes=[ok])
                if u['last']:
                    os_ = ostg[m_ % 2]
                    t.op('dve', lambda e: e.tensor_copy(os_[:, :], PS[3][o0:o0 + 64, :]), reads=[ok], writes=[('ostg', m_ % 2)])
                    t.dma('sp', ysbT[h * 64:(h + 1) * 64, g * 512:(g + 1) * 512], os_[:, :], reads=[('ostg', m_ % 2)], writes=[('ysbT', m_)])

            load_q(0)
            load_sc(0)
            n = len(units)
            GAP = 3
            for i in range(n + 2):
                if i < n:
                    S1(units[i])
                    S2a(units[i])
                    S2b(units[i])
                if 0 <= i - 1 < n:
                    S3(units[i - 1])
                    S4(units[i - 1])
                if 0 <= i - 2 < n:
                    S5(units[i - 2])
                if i % GAP == 0:
                    sidx = i // GAP
                    c, k = sidx // 6, sidx % 6
                    if c < 64:
                        mstage(c, k)
            t.barrier()

    def phase4a():
        with ExitStack() as es:
            def sbt(name, shape, dt):
                return es.enter_context(nc.sbuf_tensor(name, shape, dt))
            Wsb = sbt('Wsb', [128, 4, D], BF16); Wml = sbt('Wml', [128, 4, D], BF16); Wo = sbt('Wo', [128, 8, D], BF16)
            gbc = sbt('gbc', [128, D], F32)
            t.dma('sp', gbc[:, :], g_mix_post.partition_broadcast(128), writes=['gbc'])
            load_bf16_weight(Wsb, Wsb_b, 4, 'wsb')
            load_bf16_weight(Wml, Wml_b, 4, 'wml')
            load_bf16_weight(Wo, Wo_b, 8, 'wo')
            t.barrier()
            ysT = [sbt('ysT%d' % i, [128, 4, 512], BF16) for i in range(2)]
            ymT = [sbt('ymT%d' % i, [128, 4, 512], BF16) for i in range(2)]
            gsx = [sbt('gsx%d' % i, [128, 2048], BF16) for i in range(3)]
            xs = [sbt('xa%d' % i, [128, D], F32) for i in range(3)]
            t1 = sbt('t1', [128, D], F32); t2 = sbt('t2', [128, D], F32)
            mg = [sbt('mg%d' % i, [128, D], BF16) for i in range(2)]
            mT = [sbt('mT%d' % i, [128, 8, 128], BF16) for i in range(2)]
            junk = sbt('junk4', [128, 512], BF16)
            ssq = sbt('ssq4', [128, 4], F32)
            tmp = sbt('tmp4', [128, D], F32)
            xo = [sbt('xo%d' % i, [128, D], F32) for i in range(2)]
            junk2 = sbt('junk4b', [128, D], BF16)
            h2 = [sbt('h2_%d' % i, [128, D], BF16) for i in range(2)]
            h2T = [sbt('h2T_%d' % i, [128, 8, 128], BF16) for i in range(2)]

            def tileB2a(tt):
                sl = tt % 2
                rms_rstd(xo[sl][:, :], [('xo', sl, 0), ('xo', sl, 1)], junk2, ssq[:, 3:4], ('ssq4', 3))
                t.op('dve', lambda e: e.tensor_scalar(h2[sl][:, :], xo[sl][:, :], ssq[:, 3:4], None, ALU.mult),
                     reads=[('xo', sl, 0), ('xo', sl, 1), ('ssq4', 3)], writes=[('h2', sl)])

            def tileB2b(tt):
                sl = tt % 2
                transpose8(h2[sl], ('h2', sl), 8, h2T[sl][:, :, :], ('h2T', sl), 6 + sl, 'dve')
                t.dma('sp', h2T_d[:, tt * 128:(tt + 1) * 128].rearrange('(k p) tk -> p k tk', p=128), h2T[sl][:, :, :],
                      reads=[('h2T', sl)], writes=[('h2T_d', tt)])

            def load_g(g):
                sl = g % 2
                t.dma('sp', ysT[sl][:, :, :], ysbT[:, g * 512:(g + 1) * 512].rearrange('(j p) tk -> p j tk', p=128), writes=[('ysT', sl)])
                t.dma('sp', ymT[sl][:, :, :], ymlT[:, g * 512:(g + 1) * 512].rearrange('(j p) tk -> p j tk', p=128), writes=[('ymT', sl)])

            def load_t(tt):
                sl = tt % 3
                t.dma('sp', gsx[sl][:, :], gsig[tt * 128:(tt + 1) * 128, :], writes=[('gsx', sl)])
                t.dma('sp', xs[sl][:, :], x[tt * 128:(tt + 1) * 128, :], writes=[('xa', sl)])

            def tileA1(tt):
                g, ti, sl = tt // 4, tt % 4, tt % 2
                gl = g % 2
                tks = slice(ti * 128, (ti + 1) * 128)
                for half in range(2):
                    cs_ = slice(half * 512, (half + 1) * 512)
                    for (Ysrc, Wsrc, bk, yk) in ((ysT, Wsb, half, 'ysT'), (ymT, Wml, 2 + half, 'ymT')):
                        for j in range(4):
                            t.op('pe', lambda e, Ysrc=Ysrc, Wsrc=Wsrc, bk=bk, j=j, cs_=cs_: e.matmul(PS[bk][:, :], lhsT=Ysrc[gl][:, j, tks], rhs=Wsrc[:, j, cs_],
                                                                                                start=(j == 0), stop=(j == 3)),
                                 reads=[(yk, gl)], writes=['ps%d' % bk])
                for half in range(2):
                    cs_ = slice(half * 512, (half + 1) * 512)
                    t.op('dve', lambda e, half=half, cs_=cs_: e.tensor_tensor(t1[:, cs_], PS[half][:, :], gsx[tt % 3][:, cs_], ALU.mult),
                         reads=['ps%d' % half, ('gsx', tt % 3)], writes=[('t1', half)])
                    t.op('dve', lambda e, half=half, cs_=cs_: e.tensor_tensor(t2[:, cs_], PS[2 + half][:, :], gsx[tt % 3][:, 1024 + half * 512:1024 + (half + 1) * 512], ALU.mult),
                         reads=['ps%d' % (2 + half), ('gsx', tt % 3)], writes=[('t2', half)])
                    t.op('pool', lambda e, cs_=cs_: e.tensor_tensor(mg[sl][:, cs_], t1[:, cs_], t2[:, cs_], ALU.add),
                         reads=[('t1', half), ('t2', half)], writes=[('mg', sl, half)])

            def tileA2(tt):
                sl = tt % 2
                transpose8(mg[sl], [('mg', sl, 0), ('mg', sl, 1)], 8, mT[sl][:, :, :], ('mT', sl), 6 + sl, 'act')

            def tileB(tt):
                sl = tt % 2
                for half in range(2):
                    cs_ = slice(half * 512, (half + 1) * 512)
                    bk = 4 + half
                    for kc in range(8):
                        t.op('pe', lambda e, kc=kc, bk=bk, cs_=cs_: e.matmul(PS[bk][:, :], lhsT=mT[sl][:, kc, :], rhs=Wo[:, kc, cs_], start=(kc == 0), stop=(kc == 7)),
                             reads=[('mT', sl)], writes=['ps%d' % bk])
                for half in range(2):
                    t.op('act', lambda e, half=half: e.activation(junk[:, :], PS[4 + half][:, :], AF.Square, accum_out=ssq[:, half:half + 1]),
                         reads=['ps%d' % (4 + half)], writes=['junk4', ('ssq4', half)])
                t.op('dve', lambda e: e.tensor_tensor(ssq[:, 2:3], ssq[:, 0:1], ssq[:, 1:2], ALU.add), reads=[('ssq4', 0), ('ssq4', 1)], writes=[('ssq4', 2)])
                t.op('act', lambda e: e.activation(ssq[:, 2:3], ssq[:, 2:3], AF.Sqrt, scale=1.0 / 1024, bias=EPS), reads=[('ssq4', 2)], writes=[('ssq4', 2)])
                t.op('dve', lambda e: e.reciprocal(ssq[:, 2:3], ssq[:, 2:3]), reads=[('ssq4', 2)], writes=[('ssq4', 2)])
                for half in range(2):
                    cs_ = slice(half * 512, (half + 1) * 512)
                    t.op('dve', lambda e, half=half, cs_=cs_: e.tensor_tensor(tmp[:, cs_], PS[4 + half][:, :], gbc[:, cs_], ALU.mult),
                         reads=['ps%d' % (4 + half)], writes=[('tmp4', half)])
                    t.op('dve', lambda e, cs_=cs_: e.scalar_tensor_tensor(xo[sl][:, cs_], tmp[:, cs_], ssq[:, 2:3], xs[tt % 3][:, cs_], ALU.mult, ALU.add),
                         reads=[('tmp4', half), ('ssq4', 2), ('xa', tt % 3)], writes=[('xo', sl, half)])
                t.dma('sp', x1[tt * 128:(tt + 1) * 128, :], xo[sl][:, :], reads=[('xo', sl, 0), ('xo', sl, 1)], writes=[('x1', tt)])

            load_g(0)
            load_t(0)
            load_t(1)
            load_t(2)
            tileA1(0)
            tileA2(0)
            for tt in range(NT):
                if (tt + 1) % 4 == 0 and (tt + 1) // 4 + 1 < NG:
                    load_g((tt + 1) // 4 + 1)
                if tt == 0:
                    load_g(1)
                if tt + 1 < NT:
                    tileA1(tt + 1)
                if tt >= 1:
                    tileB2b(tt - 1)
                tileB(tt)
                tileB2a(tt)
                if tt + 3 < NT:
                    load_t(tt + 3)
                if tt + 1 < NT:
                    tileA2(tt + 1)
            tileB2b(NT - 1)
            t.barrier()

    def phase4b():
        with ExitStack() as es:
            def sbt(name, shape, dt):
                return es.enter_context(nc.sbuf_tensor(name, shape, dt))
            Wup = sbt('Wup', [128, 8, 4096], BF16); Wdn = sbt('Wdn', [128, 32, D], BF16)
            gbc = sbt('gbc5', [128, D], F32)
            t.dma('sp', gbc[:, :], g_mlp_post.partition_broadcast(128), writes=['gbc'])
            hT = [sbt('hT5_%d' % i, [128, 8, 512], BF16) for i in range(2)]
            xr = [sbt('xr%d' % i, [128, D], F32) for i in range(3)]

            def load_h(g):
                t.dma('sp', hT[g % 2][:, :, :], h2T_d[:, g * 512:(g + 1) * 512].rearrange('(k p) tk -> p k tk', p=128), writes=[('hT5', g % 2)])

            def load_xr(tt):
                t.dma('sp', xr[tt % 3][:, :], x1[tt * 128:(tt + 1) * 128, :], writes=[('xr', tt % 3)])

            load_h(0)
            load_bf16_weight(Wup, Wup_b, 8, 'wup')
            load_h(1)
            load_bf16_weight(Wdn, Wdn_b, 32, 'wdn')
            for tt in range(3):
                load_xr(tt)
            UT = sbt('UT', [128, 32, 512], BF16)
            rl = [sbt('rl%d' % i, [128, 512], F32) for i in range(2)]
            junk = sbt('junk5', [128, 512], BF16)
            ssq = sbt('ssq5', [128, 8], F32)
            tmp = sbt('tmp5', [128, 512], F32)
            xo = [sbt('xo5_%d' % i, [128, 512], F32) for i in range(2)]
            rdw_up = [('wup', kc) for kc in range(8)]
            rdw_dn = [('wdn', f) for f in range(32)]

            def group(g):
                hTg = hT[g % 2]
                for f in range(32):
                    bk = f % 4
                    for kc in range(8):
                        t.op('pe', lambda e, kc=kc, bk=bk, f=f: e.matmul(PS[bk][:, :], lhsT=Wup[:, kc, f * 128:(f + 1) * 128], rhs=hTg[:, kc, :], start=(kc == 0), stop=(kc == 7)),
                             reads=[('hT5', g % 2)] + (rdw_up if g == 0 else []), writes=['ps%d' % bk])
                    r = rl[f % 2]
                    t.op('act', lambda e, bk=bk, r=r: e.activation(r[:, :], PS[bk][:, :], AF.Relu), reads=['ps%d' % bk], writes=[('rl', f % 2)])
                    t.op('pool', lambda e, f=f, r=r: e.tensor_tensor(UT[:, f, :], r[:, :], r[:, :], ALU.mult), reads=[('rl', f % 2)], writes=[('UT', f)])
                rdu = [('UT', f) for f in range(32)]
                for ti in range(4):
                    tt = g * 4 + ti
                    xs_ = xr[tt % 3]
                    b0 = 4 + 2 * (ti % 2)
                    for half in range(2):
                        bk = b0 + half
                        cs_ = slice(half * 512, (half + 1) * 512)
                        for f in range(32):
                            t.op('pe', lambda e, f=f, bk=bk, cs_=cs_, ti=ti: e.matmul(PS[bk][:, :], lhsT=UT[:, f, ti * 128:(ti + 1) * 128], rhs=Wdn[:, f, cs_],
                                                                                  start=(f == 0), stop=(f == 31)),
                                 reads=rdu + (rdw_dn if (g == 0 and ti == 0) else []), writes=['ps%d' % bk])
                    c0 = 2 * (ti % 2)
                    for half in range(2):
                        t.op('act', lambda e, half=half: e.activation(junk[:, :], PS[b0 + half][:, :], AF.Square, accum_out=ssq[:, c0 + half:c0 + half + 1]),
                             reads=['ps%d' % (b0 + half)], writes=['junk', ('ssq5', c0 + half)])
                    sc_ = ssq[:, 4 + ti % 2:5 + ti % 2]
                    sk_ = ('ssq5', 4 + ti % 2)
                    t.op('dve', lambda e: e.tensor_tensor(sc_, ssq[:, c0:c0 + 1], ssq[:, c0 + 1:c0 + 2], ALU.add), reads=[('ssq5', c0), ('ssq5', c0 + 1)], writes=[sk_])
                    t.op('act', lambda e: e.activation(sc_, sc_, AF.Sqrt, scale=1.0 / 1024, bias=EPS), reads=[sk_], writes=[sk_])
                    t.op('dve', lambda e: e.reciprocal(sc_, sc_), reads=[sk_], writes=[sk_])
                    for half in range(2):
                        cs_ = slice(half * 512, (half + 1) * 512)
                        t.op('dve', lambda e, half=half, cs_=cs_: e.tensor_tensor(tmp[:, :], PS[b0 + half][:, :], gbc[:, cs_], ALU.mult),
                             reads=['ps%d' % (b0 + half), 'gbc'], writes=['tmp5'])
                        t.op('dve', lambda e, half=half, cs_=cs_: e.scalar_tensor_tensor(xo[half][:, :], tmp[:, :], sc_, xs_[:, cs_], ALU.mult, ALU.add),
                             reads=['tmp5', sk_, ('xr', tt % 3)], writes=[('xo5', half)])
                        t.dma('sp', x2[tt * 128:(tt + 1) * 128, cs_], xo[half][:, :], reads=[('xo5', half)], writes=[('x2', tt, half)])
                    if tt + 3 < NT:
                        load_xr(tt + 3)

            for g in range(NG):
                group(g)
                if g + 2 < NG:
                    load_h(g + 2)
            t.barrier()

    def phase4c():
        with ExitStack() as es:
            def sbt(name, shape, dt):
                return es.enter_context(nc.sbuf_tensor(name, shape, dt))
            Wg = sbt('Wg', [128, 8, D], BF16); Wp = sbt('Wp', [128, 2, D], BF16)
            gbc = sbt('gbc6', [128, D], F32)
            t.dma('sp', gbc[:, :], g_ple_post.partition_broadcast(128), writes=['gbc'])
            load_bf16_weight(Wg, Wg_b, 8, 'wg')
            load_bf16_weight(Wp, Wp_b, 2, 'wp')
            t.barrier()
            xs = [sbt('xc%d' % i, [128, D], F32) for i in range(3)]
            pt = [sbt('pt%d' % i, [128, 256], F32) for i in range(3)]
            pbf = [sbt('pbf%d' % i, [128, 256], BF16) for i in range(2)]
            hb = [sbt('hb6_%d' % i, [128, D], BF16) for i in range(2)]
            hT = [sbt('hT6_%d' % i, [128, 8, 128], BF16) for i in range(2)]
            pT = [sbt('pT6_%d' % i, [128, 2, 128], BF16) for i in range(2)]
            sg = sbt('sg6', [128, D], F32); ee = sbt('ee6', [128, D], F32)
            junk = sbt('junk6', [128, D], BF16)
            ssq = sbt('ssq6', [128, 4], F32)
            tmp = sbt('tmp6', [128, D], F32)
            xo = [sbt('xo6_%d' % i, [128, D], F32) for i in range(2)]

            def load_t(tt):
                sl = tt % 3
                t.dma('sp', xs[tt % 3][:, :], x2[tt * 128:(tt + 1) * 128, :], writes=[('xc', tt % 3)])
                t.dma('sp', pt[tt % 3][:, :], p_in[tt * 128:(tt + 1) * 128, :], writes=[('pt', tt % 3)])

            def tileC12(tt):
                sl = tt % 2
                rms_rstd(xs[tt % 3][:, :], [('xc', tt % 3)], junk, ssq[:, 0:1], ('ssq6', 0))
                t.op('dve', lambda e: e.tensor_scalar(hb[sl][:, :], xs[tt % 3][:, :], ssq[:, 0:1], None, ALU.mult), reads=[('xc', tt % 3), ('ssq6', 0)], writes=[('hb6', sl)])
                t.op('pool', lambda e: e.tensor_copy(pbf[sl][:, :], pt[tt % 3][:, :]), reads=[('pt', tt % 3)], writes=[('pbf', sl)])
                transpose8(hb[sl], ('hb6', sl), 8, hT[sl][:, :, :], ('hT6', sl), 6, 'act')
                transpose8(pbf[sl], ('pbf', sl), 2, pT[sl][:, :, :], ('pT6', sl), 7, 'dve')

            def tileC3(tt):
                sl = tt % 2
                for half in range(2):
                    cs_ = slice(half * 512, (half + 1) * 512)
                    for kc in range(8):
                        t.op('pe', lambda e, kc=kc, half=half, cs_=cs_: e.matmul(PS[half][:, :], lhsT=hT[sl][:, kc, :], rhs=Wg[:, kc, cs_], start=(kc == 0), stop=(kc == 7)),
                             reads=[('hT6', sl)], writes=['ps%d' % half])
                    for j in range(2):
                        t.op('pe', lambda e, j=j, half=half, cs_=cs_: e.matmul(PS[2 + half][:, :], lhsT=pT[sl][:, j, :], rhs=Wp[:, j, cs_], start=(j == 0), stop=(j == 1)),
                             reads=[('pT6', sl)], writes=['ps%d' % (2 + half)])

            def tileC4(tt):
                sl = tt % 2
                for half in range(2):
                    cs_ = slice(half * 512, (half + 1) * 512)
                    t.op('act', lambda e, half=half, cs_=cs_: e.activation(sg[:, cs_], PS[half][:, :], AF.Sigmoid), reads=['ps%d' % half], writes=[('sg6', half)])
                    t.op('dve', lambda e, half=half, cs_=cs_: e.tensor_tensor(ee[:, cs_], PS[2 + half][:, :], sg[:, cs_], ALU.mult),
                         reads=['ps%d' % (2 + half), ('sg6', half)], writes=[('ee6', half)])
                rms_rstd(ee[:, :], [('ee6', 0), ('ee6', 1)], junk, ssq[:, 1:2], ('ssq6', 1))
                t.op('pool', lambda e: e.tensor_tensor(tmp[:, :], ee[:, :], gbc[:, :], ALU.mult), reads=[('ee6', 0), ('ee6', 1)], writes=['tmp6'])
                t.op('dve', lambda e: e.scalar_tensor_tensor(xo[sl][:, :], tmp[:, :], ssq[:, 1:2], xs[tt % 3][:, :], ALU.mult, ALU.add),
                     reads=['tmp6', ('ssq6', 1), ('xc', tt % 3)], writes=[('xo6', sl)])
                t.dma('sp', y[tt * 128:(tt + 1) * 128, :], xo[sl][:, :], reads=[('xo6', sl)], writes=[('y', tt)])

            load_t(0)
            load_t(1)
            load_t(2)
            tileC12(0)
            for tt in range(NT):
                tileC3(tt)
                if tt + 1 < NT:
                    tileC12(tt + 1)
                tileC4(tt)
                if tt + 3 < NT:
                    load_t(tt + 3)
            t.barrier()

    if upto >= 1:
        phase1()
    if upto >= 3:
        phase23()
    if upto >= 4:
        phase4a()
    if upto >= 5:
        phase4b()
    if upto >= 6:
        phase4c()
    t.barrier()
    return nc, t


def make_in_maps(inputs):
    c = host_consts()
    maps = []
    sq = {k: np.ascontiguousarray(np.asarray(v, dtype=np.float32)[0]) for k, v in inputs.items() if k not in ('x',)}
    xx = np.asarray(inputs['x'], dtype=np.float32)
    for b in range(8):
        m = dict(c)
        m['x'] = np.ascontiguousarray(xx[b])
        m['p'] = np.ascontiguousarray(sq['p'][b])
        for k, v in sq.items():
            if k != 'p':
                m[k] = v
        maps.append(m)
    return maps


def kernel(**inputs):
    nc, _ = build(debug=False)
    maps = make_in_maps(inputs)
    res = run_bass_kernel_spmd(nc, maps, core_ids=list(range(8)))
    out = np.stack([np.asarray(r['y'], dtype=np.float32) for r in res.results], axis=0)
    return out
```
